# Optimizing a Trainium2 kernel written in Bass

```python
import jax, jax.numpy as jnp
from jax import lax
import numpy as np

D_MODEL = 1024
BATCH = 32
SEQ = 2048
DEPTH = 2

MLA_HEADS = 8
MLA_NOPE = 64
MLA_ROPE = 32
MLA_V = 64
MLA_Q_RANK = 768
MLA_KV_RANK = 256
DSA_HEADS = 4
DSA_DIM = 64
IDX_HEADS = 8
IDX_DIM = 32
DSA_TOPK_MAX = 256
RET_HEADS = 4
RET_DK = 64
RET_DV = 64
RET_CHUNK = 128
D_FF = 4 * D_MODEL
ROPE_THETA = 10000.0
EPS = 1e-6
Q_BLOCK = 128

MIX_WIDTH = MLA_HEADS * MLA_V + DSA_HEADS * DSA_DIM + RET_HEADS * RET_DV
MLA_COLS = (MLA_Q_RANK, MLA_KV_RANK, MLA_ROPE)
DSA_COLS = (DSA_HEADS * DSA_DIM, DSA_DIM, DSA_DIM, IDX_HEADS * IDX_DIM, IDX_DIM, IDX_HEADS)
RET_COLS = (RET_HEADS * RET_DK, RET_HEADS * RET_DK, RET_HEADS * RET_DV, RET_HEADS * RET_DV)
SPLITS = MLA_COLS + DSA_COLS + RET_COLS
IN_WIDTH = sum(SPLITS)

kernel_name = 'hymba_mla_dsa_retention_block'


def rmsnorm(x, g):
    xf = x.astype(jnp.float32)
    y = xf * lax.rsqrt(jnp.mean(xf * xf, axis=-1, keepdims=True) + EPS)
    return (y * g.astype(jnp.float32)).astype(x.dtype)


def rope_tables(seq, dim):
    pos = jnp.arange(seq, dtype=jnp.float32)
    inv = ROPE_THETA ** (-jnp.arange(0, dim, 2, dtype=jnp.float32) / dim)
    ang = pos[:, None] * inv[None, :]
    return jnp.cos(ang), jnp.sin(ang)


def apply_rope(x, cos, sin):
    d2 = x.shape[-1] // 2
    x1, x2 = x[..., :d2], x[..., d2:]
    c = cos[:, None, :].astype(x.dtype)
    s = sin[:, None, :].astype(x.dtype)
    return jnp.concatenate([x1 * c - x2 * s, x1 * s + x2 * c], axis=-1)


def to_blocks(t):
    b, s = t.shape[0], t.shape[1]
    return t.reshape((b, s // Q_BLOCK, Q_BLOCK) + t.shape[2:]).swapaxes(0, 1)


def from_blocks(t):
    t = t.swapaxes(0, 1)
    return t.reshape(t.shape[0], t.shape[1] * t.shape[2], -1)


def mla_attention(q_nope, q_rope, k_nope, k_rope, v):
    S = q_nope.shape[1]
    nb = S // Q_BLOCK
    scale = (MLA_NOPE + MLA_ROPE) ** -0.5
    key_pos = jnp.arange(S)
    neg = jnp.finfo(jnp.float32).min

    def block(args):
        qn, qr, start = args
        s = jnp.einsum('bqhd,bkhd->bhqk', qn, k_nope) + jnp.einsum('bqhd,bkd->bhqk', qr, k_rope)
        s = s.astype(jnp.float32) * scale
        qpos = start + jnp.arange(Q_BLOCK)
        causal = key_pos[None, :] <= qpos[:, None]
        p = jax.nn.softmax(jnp.where(causal, s, neg), axis=-1).astype(v.dtype)
        return jnp.einsum('bhqk,bkhd->bqhd', p, v)

    starts = jnp.arange(nb, dtype=jnp.int32) * Q_BLOCK
    out = lax.map(block, (to_blocks(q_nope), to_blocks(q_rope), starts))
    return from_blocks(out)


def dsa_attention(q, k, v, q_idx, k_idx, w_idx, topk):
    S = q.shape[1]
    nb = S // Q_BLOCK
    scale = DSA_DIM ** -0.5
    key_pos = jnp.arange(S)
    neg = jnp.finfo(jnp.float32).min
    gather = jax.vmap(lambda table, idx: table[idx])

    def block(args):
        qb, qib, wb, start = args
        qpos = start + jnp.arange(Q_BLOCK)
        causal = key_pos[None, :] <= qpos[:, None]
        rel = jax.nn.relu(jnp.einsum('bqhd,bkd->bqhk', qib, k_idx))
        score = jnp.einsum('bqh,bqhk->bqk', wb, rel).astype(jnp.float32)
        score = jnp.where(causal[None], score, -jnp.inf)
        _, sel = lax.top_k(score, topk)
        valid = sel <= qpos[None, :, None]
        k_sel = gather(k, sel)
        v_sel = gather(v, sel)
        s = jnp.einsum('bqhd,bqjd->bhqj', qb, k_sel).astype(jnp.float32) * scale
        p = jax.nn.softmax(jnp.where(valid[:, None], s, neg), axis=-1).astype(v.dtype)
        return jnp.einsum('bhqj,bqjd->bqhd', p, v_sel)

    starts = jnp.arange(nb, dtype=jnp.int32) * Q_BLOCK
    out = lax.map(block, (to_blocks(q), to_blocks(q_idx), to_blocks(w_idx), starts))
    return from_blocks(out)


def retention(q, k, v, log_gamma):
    B, S, H, dk = q.shape
    dv = v.shape[-1]
    C = RET_CHUNK
    n = S // C
    qc = q.astype(jnp.float32).reshape(B, n, C, H, dk)
    kc = k.astype(jnp.float32).reshape(B, n, C, H, dk)
    vc = v.astype(jnp.float32).reshape(B, n, C, H, dv)
    pos = jnp.arange(C, dtype=jnp.float32)
    rel = pos[:, None] - pos[None, :]
    decay = jnp.where(rel[None] >= 0, jnp.exp(jnp.maximum(rel, 0.0)[None] * log_gamma[:, None, None]), 0.0)
    inner = jnp.einsum('bnqhd,bnchd->bnhqc', qc, kc) * decay[None, None]
    inner_out = jnp.einsum('bnhqc,bnche->bnqhe', inner, vc)
    zeta = jnp.exp((C - 1.0 - pos)[None, :] * log_gamma[:, None])
    chunk_state = jnp.einsum('bnchd,hc,bnche->bnhde', kc, zeta, vc)
    chunk_decay = jnp.exp(C * log_gamma)[None, :, None, None]

    def step(R, s_i):
        return R * chunk_decay + s_i, R

    R0 = jnp.zeros((B, H, dk, dv), jnp.float32)
    _, R_prev = lax.scan(step, R0, chunk_state.swapaxes(0, 1))
    R_prev = R_prev.swapaxes(0, 1)
    xi = jnp.exp((pos + 1.0)[None, :] * log_gamma[:, None])
    cross = jnp.einsum('bnqhd,bnhde,hq->bnqhe', qc, R_prev, xi)
    return (inner_out + cross).reshape(B, S, H, dv)


def head_groupnorm(x, g):
    mu = jnp.mean(x, axis=-1, keepdims=True)
    xc = x - mu
    var = jnp.mean(xc * xc, axis=-1, keepdims=True)
    return xc * lax.rsqrt(var + EPS) * g


def setup_inputs(seed: int = 0) -> dict:
    key = jax.random.key(seed)
    ks = jax.random.split(key, 16)

    def nrm(k, shape, scale):
        return jax.random.normal(k, shape, jnp.float32) * scale

    def gain(k, shape):
        return 1.0 + 0.05 * jax.random.normal(k, shape, jnp.float32)

    return {
        'x': nrm(ks[0], (BATCH, SEQ, D_MODEL), 1.0),
        'g_mix': gain(ks[1], (DEPTH, D_MODEL)),
        'w_in': nrm(ks[2], (DEPTH, D_MODEL, IN_WIDTH), D_MODEL ** -0.5),
        'g_q': gain(ks[3], (DEPTH, MLA_Q_RANK)),
        'w_uq': nrm(ks[4], (DEPTH, MLA_Q_RANK, MLA_HEADS * (MLA_NOPE + MLA_ROPE)), MLA_Q_RANK ** -0.5),
        'g_kv': gain(ks[5], (DEPTH, MLA_KV_RANK)),
        'w_ukv': nrm(ks[6], (DEPTH, MLA_KV_RANK, MLA_HEADS * (MLA_NOPE + MLA_V)), MLA_KV_RANK ** -0.5),
        'g_ret': gain(ks[7], (DEPTH, RET_HEADS * RET_DV)),
        'w_out': nrm(ks[8], (DEPTH, MIX_WIDTH, D_MODEL), MIX_WIDTH ** -0.5),
        'g_mlp': gain(ks[9], (DEPTH, D_MODEL)),
        'w_ff1': nrm(ks[10], (DEPTH, D_MODEL, D_FF), D_MODEL ** -0.5),
        'w_ff2': nrm(ks[11], (DEPTH, D_FF, D_MODEL), D_FF ** -0.5),
        'g_final': gain(ks[12], (D_MODEL,)),
    }


def reference(x, g_mix, w_in, g_q, w_uq, g_kv, w_ukv, g_ret, w_out, g_mlp, w_ff1, w_ff2, g_final):
    B, S, _ = x.shape
    topk = min(DSA_TOPK_MAX, S // 4)
    cos_r, sin_r = rope_tables(S, MLA_ROPE)
    cos_a, sin_a = rope_tables(S, DSA_DIM)
    cos_i, sin_i = rope_tables(S, IDX_DIM)
    cos_t, sin_t = rope_tables(S, RET_DK)
    log_gamma = jnp.log1p(-jnp.exp2(-5.0 - jnp.arange(RET_HEADS, dtype=jnp.float32)))
    split_at = np.cumsum(SPLITS)[:-1].tolist()
    idx_scale = (IDX_HEADS ** -0.5) * (IDX_DIM ** -0.5)

    for l in range(DEPTH):
        h = rmsnorm(x, g_mix[l])
        (cq, ckv, kr, aq, ak, av, qi, ki, wi, rq, rk, rv, rg) = jnp.split(h @ w_in[l], split_at, axis=-1)

        q = (rmsnorm(cq, g_q[l]) @ w_uq[l]).reshape(B, S, MLA_HEADS, MLA_NOPE + MLA_ROPE)
        q_nope = q[..., :MLA_NOPE]
        q_rope = apply_rope(q[..., MLA_NOPE:], cos_r, sin_r)
        kv = (rmsnorm(ckv, g_kv[l]) @ w_ukv[l]).reshape(B, S, MLA_HEADS, MLA_NOPE + MLA_V)
        k_nope, v_mla = kv[..., :MLA_NOPE], kv[..., MLA_NOPE:]
        k_rope = apply_rope(kr[:, :, None, :], cos_r, sin_r)[:, :, 0]
        o_a = mla_attention(q_nope, q_rope, k_nope, k_rope, v_mla)

        q_b = apply_rope(aq.reshape(B, S, DSA_HEADS, DSA_DIM), cos_a, sin_a)
        k_b = apply_rope(ak[:, :, None, :], cos_a, sin_a)[:, :, 0]
        q_i = apply_rope(qi.reshape(B, S, IDX_HEADS, IDX_DIM), cos_i, sin_i)
        k_i = apply_rope(ki[:, :, None, :], cos_i, sin_i)[:, :, 0]
        o_b = dsa_attention(q_b, k_b, av, q_i, k_i, wi * idx_scale, topk)

        q_c = apply_rope(rq.reshape(B, S, RET_HEADS, RET_DK), cos_t, sin_t)
        k_c = apply_rope(rk.reshape(B, S, RET_HEADS, RET_DK), cos_t, sin_t) * (RET_DK ** -0.5)
        ret = retention(q_c, k_c, rv.reshape(B, S, RET_HEADS, RET_DV), log_gamma)
        ret = head_groupnorm(ret, g_ret[l].reshape(RET_HEADS, RET_DV).astype(jnp.float32))
        o_c = (jax.nn.silu(rg.astype(jnp.float32)) * ret.reshape(B, S, RET_HEADS * RET_DV)).astype(x.dtype)

        x = x + jnp.concatenate([o_a, o_b, o_c], axis=-1) @ w_out[l]

        hf = rmsnorm(x, g_mlp[l])
        x = x + jnp.square(jax.nn.relu(hf @ w_ff1[l])) @ w_ff2[l]

    return rmsnorm(x, g_final)
```

```python
import numpy as np
import concourse.bass as bass
import concourse.mybir as mybir
from concourse.bass_utils import run_bass_kernel_spmd

F32 = mybir.dt.float32
BF16 = mybir.dt.bfloat16
AF = mybir.ActivationFunctionType
ALU = mybir.AluOpType
AX = mybir.AxisListType

D = 1024
S = 2048
NCH = 4
CH = 512
NT = 16
INW = 2760
DFF = 4096
EPS = 1e-6
NIT = 16
MLA_SCALE = 96 ** -0.5
DSA_SCALE = 64 ** -0.5
IDX_SCALE = (8 ** -0.5) * (32 ** -0.5)
NEG = -30000.0
NG = 8 + 6 + 2 + 8 + 256


class Tok:
    __slots__ = ("name", "lw", "rds")

    def __init__(self, name=""):
        self.name = name
        self.lw = None
        self.rds = []


class Op:
    __slots__ = ("eng", "fn", "deps", "inc", "val", "dsem", "dval", "is_dma", "ep")


class Sched:
    ENGS = ("pe", "act", "dve", "pool", "sp")

    def __init__(self, n_dma_sems=24):
        self.ops = {e: [] for e in self.ENGS}
        self.n_dma_sems = n_dma_sems
        self.dma_last = [None] * n_dma_sems
        self.dma_cnt = [0] * n_dma_sems
        self.dma_rr = 0
        self.pending = {e: [] for e in self.ENGS}
        self.epoch = 0
        self.nsets = 12

    def barrier(self):
        lasts = []
        for e in self.ENGS:
            for op in reversed(self.ops[e]):
                if not op.is_dma:
                    lasts.append(op)
                    break
        for d in self.dma_last:
            if d is not None:
                lasts.append(d)
        for e in self.ENGS:
            self.pending[e] = list(lasts)
        self.epoch += 1

    def _rec(self, eng, fn, rd, wr, is_dma=False):
        op = Op()
        op.eng = eng
        op.fn = fn
        op.inc = False
        op.val = None
        op.is_dma = is_dma
        op.ep = self.epoch % self.nsets
        op.dsem = None
        op.dval = None
        deps = list(self.pending[eng])
        self.pending[eng] = []
        for t in rd:
            if t.lw is not None:
                deps.append(t.lw)
        for t in wr:
            if t.lw is not None:
                deps.append(t.lw)
            deps.extend(t.rds)
        if is_dma:
            k = self.dma_rr % self.n_dma_sems
            self.dma_rr += 1
            if self.dma_last[k] is not None:
                deps.append(self.dma_last[k])
            self.dma_last[k] = op
            self.dma_cnt[k] += 16
            op.dsem = k
            op.dval = self.dma_cnt[k]
        fd = []
        seen = set()
        for d in deps:
            if id(d) in seen or d is op:
                continue
            seen.add(id(d))
            if (not d.is_dma) and d.eng == "pe" and eng == "pe":
                continue
            if not d.is_dma:
                d.inc = True
            fd.append(d)
        op.deps = fd
        for t in rd:
            t.rds.append(op)
        for t in wr:
            t.lw = op
            t.rds = []
        self.ops[eng].append(op)
        return op

    def op(self, eng, fn, rd=(), wr=()):
        return self._rec(eng, fn, rd, wr)

    def dma(self, eng, out, in_, rd=(), wr=()):
        return self._rec(eng, lambda e: e.dma_start(out=out, in_=in_), rd, wr, is_dma=True)

    def mm(self, out, lhsT, rhs, start, stop, rd, wr):
        return self.op("pe", lambda e: e.matmul(out, lhsT, rhs, start=start, stop=stop), rd, wr)

    def tr(self, out, in_, ident, rd, wr):
        return self.op("pe", lambda e: e.transpose(out, in_, ident), rd, wr)

    def act(self, out, in_, func, rd, wr, scale=None, bias=None, accum=None):
        kw = {}
        if scale is not None:
            kw["scale"] = scale
        if bias is not None:
            kw["bias"] = bias
        if accum is not None:
            kw["accum_out"] = accum
        return self.op("act", lambda e: e.activation(out=out, in_=in_, func=func, **kw), rd, wr)

    def tt(self, eng, out, in0, in1, op, rd, wr):
        return self.op(eng, lambda e: e.tensor_tensor(out=out, in0=in0, in1=in1, op=op), rd, wr)

    def ts(self, eng, out, in0, s1, s2, op0, op1, rd, wr, accum=None):
        if op1 is None:
            return self.op(eng, lambda e: e.tensor_scalar(out=out, in0=in0, scalar1=s1, scalar2=None, op0=op0), rd, wr)
        if accum is not None:
            return self.op(eng, lambda e: e.tensor_scalar(out=out, in0=in0, scalar1=s1, scalar2=s2, op0=op0,
                                                          op1=op1, accum_out=accum), rd, wr)
        return self.op(eng, lambda e: e.tensor_scalar(out=out, in0=in0, scalar1=s1, scalar2=s2, op0=op0, op1=op1),
                       rd, wr)

    def stt(self, out, in0, scalar, in1, op0, op1, rd, wr):
        return self.op("dve", lambda e: e.scalar_tensor_tensor(out=out, in0=in0, scalar=scalar, in1=in1,
                                                               op0=op0, op1=op1), rd, wr)

    def copy(self, eng, out, in_, rd, wr):
        if eng == "act":
            return self.op("act", lambda e: e.activation(out=out, in_=in_, func=AF.Copy), rd, wr)
        return self.op(eng, lambda e: e.tensor_copy(out=out, in_=in_), rd, wr)

    def memset(self, eng, out, val, wr):
        return self.op(eng, lambda e: e.memset(out, val), (), wr)

    def emit(self, nc, block, sems, dsems, final_waits):
        for eng in self.ENGS:
            c = [0] * self.nsets
            for op in self.ops[eng]:
                if op.inc and not op.is_dma:
                    c[op.ep] += 1
                    op.val = c[op.ep]
        engobj = {"pe": block.tensor, "act": block.scalar, "dve": block.vector, "pool": block.gpsimd,
                  "sp": block.sync}

        def make(eng):
            ops = self.ops[eng]

            def body(e):
                seen = {}
                for op in ops:
                    for d in op.deps:
                        if d.is_dma:
                            key = ("d", d.dsem)
                            sem = dsems[d.dsem]
                            val = d.dval
                        else:
                            key = ("c", d.eng, d.ep)
                            sem = sems[d.ep][d.eng]
                            val = d.val
                        if seen.get(key, 0) >= val:
                            continue
                        seen[key] = val
                        e.wait_ge(sem, val)
                    ins = op.fn(e)
                    if op.is_dma:
                        ins.then_inc(dsems[op.dsem], 16)
                    elif op.inc:
                        ins.then_inc(sems[op.ep][eng], 1)
                if eng == "sp":
                    for (k, v) in final_waits:
                        e.wait_ge(dsems[k], v)
            return body

        for eng in self.ENGS:
            engobj[eng](make(eng))


def _consts():
    c = {}
    pos = np.arange(S, dtype=np.float64)

    def tab(dim):
        inv = 10000.0 ** (-np.arange(0, dim, 2, dtype=np.float64) / dim)
        C = np.zeros((128, S), np.float32)
        Sg = np.zeros((128, S), np.float32)
        P = np.zeros((128, 128), np.float32)
        for p in range(128):
            i = p % dim
            j = i % (dim // 2)
            ang = (pos.astype(np.float32) * np.float32(inv[j])).astype(np.float32)
            C[p] = np.cos(ang)
            Sg[p] = (-np.sin(ang)) if i < dim // 2 else np.sin(ang)
            src = (p // dim) * dim + (i + dim // 2) % dim
            P[src, p] = 1.0
        return C, Sg, P
    c["C32"], c["S32"], c["P32"] = tab(32)
    c["C64"], c["S64"], c["P64"] = tab(64)
    cm = np.zeros((128, 4, 512), np.float32)
    k = np.arange(128)[:, None]
    q = np.arange(512)[None, :]
    for i in range(4):
        cm[:, i, :] = np.where(i * 128 + k > q, NEG, 0.0)
    c["cmask"] = cm
    qq = np.arange(128)[:, None]
    kk = np.arange(128)[None, :]
    c["cb"] = np.where(kk <= qq, 0.0, -1e30).astype(np.float32)
    lg = np.log1p(-np.exp2(-5.0 - np.arange(4, dtype=np.float64)))
    dec = np.zeros((128, 4, 128), np.float32)
    cc = np.arange(128)[:, None]
    q1 = np.arange(128)[None, :]
    for h in range(4):
        dec[:, (h % 2) * 2 + h // 2, :] = np.where(q1 >= cc, 0.125 * np.exp(np.maximum(q1 - cc, 0) * lg[h]), 0.0)
    c["decT"] = dec
    xi = np.zeros((128, 2, 512), np.float32)
    for i in range(2):
        for r in range(128):
            h = 2 * i + r // 64
            xi[r, i, :] = np.tile(np.exp((np.arange(128) + 1.0) * lg[h]), 4)
    c["xiT"] = xi
    zb = np.zeros((128, 256), np.float32)
    cdb = np.zeros((64, 256), np.float32)
    for h in range(4):
        zb[:, h * 64:(h + 1) * 64] = (0.125 * np.exp((127.0 - np.arange(128)) * lg[h]))[:, None]
        cdb[:, h * 64:(h + 1) * 64] = np.exp(128.0 * lg[h])
    c["zetab"] = zb
    c["cdb"] = cdb
    c["ctab"] = np.broadcast_to((2.0 ** -(np.arange(NIT + 1) + 1.0)).astype(np.float32)[None, :], (128, NIT + 1)).copy()
    c["ident"] = np.eye(128, dtype=np.float32)
    c["ones"] = np.ones((128, 128), np.float32)
    return c


CONST_SHAPES = {
    "C32": [128, S], "S32": [128, S], "P32": [128, 128], "C64": [128, S], "S64": [128, S], "P64": [128, 128],
    "cmask": [128, 4, 512], "cb": [128, 128], "decT": [128, 4, 128], "xiT": [128, 2, 512], "zetab": [128, 256],
    "cdb": [64, 256], "ctab": [128, NIT + 1], "ident": [128, 128], "ones": [128, 128],
}


def build_nc(nseq, nlayers, final_norm, dbg=None):
    from contextlib import ExitStack
    nc = bass.Bass("TRN2", target_bir_lowering=False)
    dr = {}
    dr["x"] = nc.dram_tensor("x", [nseq * S, D], F32, kind="ExternalInput").ap()
    dr["y"] = nc.dram_tensor("y", [nseq * S, D], F32, kind="ExternalOutput").ap()
    dr["w_in"] = nc.dram_tensor("w_in", [nlayers, D, INW], F32, kind="ExternalInput").ap()
    dr["w_uq"] = nc.dram_tensor("w_uq", [nlayers, 768, 768], F32, kind="ExternalInput").ap()
    dr["w_ukv"] = nc.dram_tensor("w_ukv", [nlayers, 256, 1024], F32, kind="ExternalInput").ap()
    dr["w_out"] = nc.dram_tensor("w_out", [nlayers, D, D], F32, kind="ExternalInput").ap()
    dr["w_ff1"] = nc.dram_tensor("w_ff1", [nlayers, D, DFF], F32, kind="ExternalInput").ap()
    dr["w_ff2"] = nc.dram_tensor("w_ff2", [nlayers, DFF, D], F32, kind="ExternalInput").ap()
    dr["gpack"] = nc.dram_tensor("gpack", [nlayers, 128, NG], F32, kind="ExternalInput").ap()
    dr["gfin"] = nc.dram_tensor("gfin", [128, 8], F32, kind="ExternalInput").ap()
    for k, shp in CONST_SHAPES.items():
        dr[k] = nc.dram_tensor("c_" + k, shp, F32, kind="ExternalInput").ap()
    if dbg:
        dr["dbg"] = nc.dram_tensor("dbg", [128, 8, S], F32, kind="ExternalOutput").ap()

    stop = None
    if dbg and ":" in dbg:
        dbg, stop = dbg.split(":")
    sch = Sched()
    with ExitStack() as st:
        ARW = 53000
        arena = st.enter_context(nc.sbuf_tensor("arena", [128, ARW], F32))
        psf = [st.enter_context(nc.psum_tensor("ps%d" % i, [128, 512], F32))[:, :] for i in range(7)]
        psb = st.enter_context(nc.psum_tensor("psb", [128, 1024], BF16))[:, :]
        sems = [{e: st.enter_context(nc.semaphore("sem%d_%s" % (k, e))) for e in Sched.ENGS} for k in range(sch.nsets)]
        dsems = [st.enter_context(nc.semaphore("dsem%d" % i)) for i in range(sch.n_dma_sems)]
        block = st.enter_context(nc.Block())

        class Carver:
            def __init__(self, base=0):
                self.off = base

            def take(self, nbytes):
                o = self.off
                self.off += (nbytes + 3) // 4 * 4
                assert self.off <= ARW * 4, ("SBUF arena overflow", self.off, ARW * 4)
                self.peak = max(getattr(self, "peak", 0), self.off)
                return o

        def view(off, dtype, shape):
            n = 1
            for s_ in shape[1:]:
                n *= s_
            esz = 4 if dtype == F32 else 2
            w0 = off // 4
            nw = (n * esz + 3) // 4
            ap = arena[0:shape[0], w0:w0 + nw]
            if dtype != F32:
                ap = ap.bitcast(dtype)
            if len(shape) == 3:
                ap = ap.rearrange("p (a b) -> p a b", a=shape[1])
            elif len(shape) == 4:
                ap = ap.rearrange("p (a b c) -> p a b c", a=shape[1], b=shape[2])
            return ap

        def alloc(C, dtype, shape):
            n = 1
            for s_ in shape[1:]:
                n *= s_
            return view(C.take(n * (4 if dtype == F32 else 2)), dtype, shape)

        G = Carver()
        xT = alloc(G, F32, [128, 8, S])
        OA = alloc(G, BF16, [128, 4, S])
        ident_b = alloc(G, BF16, [128, 128])
        ident_f = alloc(G, F32, [128, 128])
        ones_b = alloc(G, BF16, [128, 128])
        ones_f = alloc(G, F32, [128, 128])
        P32 = alloc(G, BF16, [128, 128])
        P64 = alloc(G, BF16, [128, 128])
        gp = alloc(G, F32, [128, nlayers, NG])
        gfin = alloc(G, F32, [128, 8])
        PHASE_BASE = G.off

        t_xT = [Tok("xT%d" % c) for c in range(NCH)]
        t_OA = [Tok("OA%d" % c) for c in range(NCH)]
        t_const = Tok("const")
        t_ps = [Tok("ps%d" % i) for i in range(7)]
        t_psb = Tok("psb")

        sch.dma("pool", ident_b, dr["ident"], (), [t_const])
        sch.dma("sp", ident_f, dr["ident"], (), [t_const])
        sch.dma("pool", ones_b, dr["ones"], (), [t_const])
        sch.dma("sp", ones_f, dr["ones"], (), [t_const])
        sch.dma("pool", P32, dr["P32"], (), [t_const])
        sch.dma("pool", P64, dr["P64"], (), [t_const])
        for l in range(nlayers):
            sch.dma("sp", gp[:, l, :], dr["gpack"][l], (), [t_const])
        sch.dma("sp", gfin, dr["gfin"], (), [t_const])

        def gvec(l, which, j):
            base = {"mix": 0, "q": 8, "kv": 14, "mlp": 16}[which]
            return gp[:, l, base + j:base + j + 1]

        def chunk_norm(c, gfun, hT_dst, t_h, sq, t_sq, rb, t_rb, pi):
            cols = slice(c * CH, (c + 1) * CH)
            for kc in range(8):
                sch.act(sq[:, kc % 2, :], xT[:, kc, cols], AF.Square, [t_xT[c]], [t_sq[kc % 2]])
                sch.mm(psf[pi], ones_b, sq[:, kc % 2, :], kc == 0, kc == 7, [t_sq[kc % 2], t_const], [t_ps[pi]])
            sch.act(rb, psf[pi], AF.Sqrt, [t_ps[pi]], [t_rb], scale=1.0 / D, bias=EPS)
            sch.op("dve", lambda e: e.reciprocal(out=rb, in_=rb), [t_rb], [t_rb])
            for kc in range(8):
                sch.stt(hT_dst[:, kc, :], xT[:, kc, cols], gfun(kc), rb, ALU.mult, ALU.mult,
                        [t_xT[c], t_rb, t_const], [t_h])

        def rope(rows, pin, pr, Ctab, Stab, t_tab, Pm, xb, t_xb, t1, t_t1, t2, t_t2, dst, t_dst,
                 rstd=None, t_rstd=None):
            if rstd is None:
                sch.copy("act", xb[0:rows, :], psf[pin][0:rows, :], [t_ps[pin]], [t_xb])
            else:
                sch.tt("dve", xb[0:rows, :], psf[pin][0:rows, :], rstd[0:rows, :], ALU.mult, [t_ps[pin], t_rstd], [t_xb])
            sch.mm(psf[pr][0:rows, :], Pm[0:rows, 0:rows], xb[0:rows, :], True, True, [t_xb, t_const], [t_ps[pr]])
            sch.tt("dve", t1[0:rows, :], xb[0:rows, :], Ctab[0:rows, :], ALU.mult, [t_xb, t_tab], [t_t1])
            sch.tt("dve", t2[0:rows, :], psf[pr][0:rows, :], Stab[0:rows, :], ALU.mult, [t_ps[pr], t_tab], [t_t2])
            sch.tt("dve", dst, t1[0:rows, :], t2[0:rows, :], ALU.add, [t_t1, t_t2], [t_dst])

        for sq_i in range(nseq):
            sch.barrier()
            Lc = Carver(PHASE_BASE)
            xin = [alloc(Lc, F32, [128, D]) for _ in range(2)]
            t_xin = [Tok("xin0"), Tok("xin1")]
            for t in range(NT):
                b = t % 2
                sch.dma("sp", xin[b], dr["x"][sq_i * S + t * 128: sq_i * S + (t + 1) * 128, :], (), [t_xin[b]])
                for half in range(2):
                    for k4 in range(4):
                        kc = half * 4 + k4
                        sch.tr(psf[half][:, k4 * 128:(k4 + 1) * 128], xin[b][:, kc * 128:(kc + 1) * 128], ident_f,
                               [t_xin[b], t_const], [t_ps[half]])
                    for k4 in range(4):
                        kc = half * 4 + k4
                        sch.copy("act" if half == 0 else "dve", xT[:, kc, t * 128:(t + 1) * 128],
                                 psf[half][:, k4 * 128:(k4 + 1) * 128], [t_ps[half]], [t_xT[t // 4]])

            for l in range(nlayers):
                sch.barrier()
                A = Carver(PHASE_BASE)
                Win = alloc(A, BF16, [128, 8, 1056])
                WuqN = alloc(A, BF16, [128, 6, 512])
                WuqR = alloc(A, BF16, [128, 6, 256])
                WukvK = alloc(A, BF16, [128, 2, 512])
                WukvV = alloc(A, BF16, [128, 2, 512])
                KN = alloc(A, BF16, [128, 4, S])
                KR3 = alloc(A, BF16, [128, S])
                VC = alloc(A, BF16, [128, NT, 8, 65])
                hT = alloc(A, BF16, [128, 8, CH])
                sqb = alloc(A, BF16, [128, 2, CH])
                rb = alloc(A, F32, [128, CH])
                rq_b = alloc(A, F32, [128, CH])
                rkv_b = alloc(A, F32, [128, CH])
                cqn = alloc(A, BF16, [128, 6, CH])
                ckvn = alloc(A, BF16, [128, 2, CH])
                tabC = alloc(A, BF16, [128, CH])
                tabS = alloc(A, BF16, [128, CH])
                xb = alloc(A, BF16, [128, CH])
                t1 = alloc(A, BF16, [128, CH])
                t2 = alloc(A, BF16, [128, CH])
                QN = alloc(A, BF16, [128, 4, CH])
                QR = alloc(A, BF16, [128, 3, CH])
                PT = [alloc(A, BF16, [128, CH]) for _ in range(3)]
                cmask = alloc(A, BF16, [128, 4, CH])
                rec = alloc(A, F32, [128, CH])
                pbs = alloc(A, F32, [128, CH])
                rtok = alloc(A, F32, [128, 8])
                t_W = Tok("W1")
                t_KN = [Tok() for _ in range(NCH)]
                t_KR = [Tok() for _ in range(NCH)]
                t_VC = [Tok() for _ in range(NCH)]
                t_h, t_rb, t_rq, t_rkv, t_cqn, t_ckvn = Tok(), Tok(), Tok(), Tok(), Tok(), Tok()
                t_sq = [Tok(), Tok()]
                t_tab, t_xb, t_t1, t_t2, t_QN, t_QR = Tok(), Tok(), Tok(), Tok(), Tok(), Tok()
                t_PT = [Tok(), Tok(), Tok()]
                t_cm, t_rec, t_pbs, t_rtok = Tok(), Tok(), Tok(), Tok()

                for kc in range(8):
                    sch.dma("pool", Win[:, kc, :], dr["w_in"][l, kc * 128:(kc + 1) * 128, 0:1056], (), [t_W])
                for kc in range(6):
                    srcq = dr["w_uq"][l, kc * 128:(kc + 1) * 128, :].rearrange("p (h d) -> p h d", h=8)
                    sch.dma("pool", WuqN[:, kc, :].rearrange("p (h d) -> p h d", h=8), srcq[:, :, 0:64], (), [t_W])
                    sch.dma("pool", WuqR[:, kc, :].rearrange("p (h d) -> p h d", h=8), srcq[:, :, 64:96], (), [t_W])
                for kc in range(2):
                    srck = dr["w_ukv"][l, kc * 128:(kc + 1) * 128, :].rearrange("p (h d) -> p h d", h=8)
                    sch.dma("pool", WukvK[:, kc, :].rearrange("p (h d) -> p h d", h=8), srck[:, :, 0:64], (), [t_W])
                    sch.dma("pool", WukvV[:, kc, :].rearrange("p (h d) -> p h d", h=8), srck[:, :, 64:128], (), [t_W])
                sch.dma("pool", cmask, dr["cmask"], (), [t_cm])
                sch.memset("pool", VC[:, :, :, 64:65], 1.0, [t_VC[c] for c in range(NCH)])

                for c in range(NCH):
                    cols = slice(c * CH, (c + 1) * CH)
                    sch.dma("pool", tabC, dr["C32"][:, cols], (), [t_tab])
                    sch.dma("pool", tabS, dr["S32"][:, cols], (), [t_tab])
                    chunk_norm(c, lambda kc: gvec(l, "mix", kc), hT, t_h, sqb, t_sq, rb, t_rb, 0)
                    for j in range(6):
                        pi = j % 2
                        for kc in range(8):
                            sch.mm(psf[pi], Win[:, kc, j * 128:(j + 1) * 128], hT[:, kc, :], kc == 0, kc == 7,
                                   [t_W, t_h], [t_ps[pi]])
                        sch.act(cqn[:, j, :], psf[pi], AF.Copy, [t_ps[pi], t_const], [t_cqn], scale=gvec(l, "q", j))
                        sch.act(sqb[:, j % 2, :], psf[pi], AF.Square, [t_ps[pi]], [t_sq[j % 2]])
                        sch.mm(psf[2], ones_b, sqb[:, j % 2, :], j == 0, j == 5, [t_sq[j % 2], t_const], [t_ps[2]])
                    sch.act(rq_b, psf[2], AF.Sqrt, [t_ps[2]], [t_rq], scale=1.0 / 768, bias=EPS)
                    sch.op("dve", lambda e: e.reciprocal(out=rq_b, in_=rq_b), [t_rq], [t_rq])
                    for j in range(2):
                        pi = j % 2
                        for kc in range(8):
                            sch.mm(psf[pi], Win[:, kc, 768 + j * 128:768 + (j + 1) * 128], hT[:, kc, :], kc == 0, kc == 7,
                                   [t_W, t_h], [t_ps[pi]])
                        sch.act(ckvn[:, j, :], psf[pi], AF.Copy, [t_ps[pi], t_const], [t_ckvn], scale=gvec(l, "kv", j))
                        sch.act(sqb[:, j % 2, :], psf[pi], AF.Square, [t_ps[pi]], [t_sq[j % 2]])
                        sch.mm(psf[2], ones_b, sqb[:, j % 2, :], j == 0, j == 1, [t_sq[j % 2], t_const], [t_ps[2]])
                    for t in range(4):
                        for j in range(2):
                            sch.mm(psf[3][:, 2 * t:2 * t + 2], sqb[:, j, t * 128:(t + 1) * 128], ones_b[:, 0:2], j == 0, j == 1,
                                   [t_sq[j], t_const], [t_ps[3]])
                    sch.act(rkv_b, psf[2], AF.Sqrt, [t_ps[2]], [t_rkv], scale=1.0 / 256, bias=EPS)
                    sch.op("dve", lambda e: e.reciprocal(out=rkv_b, in_=rkv_b), [t_rkv], [t_rkv])
                    sch.act(rtok[:, 0:4], psf[3][:, 0:8].rearrange("p (t two) -> p t two", two=2)[:, :, 0], AF.Sqrt, [t_ps[3]], [t_rtok], scale=1.0 / 256, bias=EPS)
                    sch.op("dve", lambda e: e.reciprocal(out=rtok[:, 0:4], in_=rtok[:, 0:4]), [t_rtok], [t_rtok])
                    for kc in range(8):
                        sch.mm(psf[0][0:32, :], Win[:, kc, 1024:1056], hT[:, kc, :], kc == 0, kc == 7, [t_W, t_h], [t_ps[0]])
                    rope(32, 0, 1, tabC, tabS, t_tab, P32, xb, t_xb, t1, t_t1, t2, t_t2, KR3[0:32, cols], t_KR[c])
                    sch.copy("pool", KR3[32:64, cols], KR3[0:32, cols], [t_KR[c]], [t_KR[c]])
                    sch.copy("pool", KR3[64:96, cols], KR3[0:32, cols], [t_KR[c]], [t_KR[c]])
                    for i in range(4):
                        pi = i % 2
                        for kc in range(6):
                            sch.mm(psf[pi], WuqN[:, kc, i * 128:(i + 1) * 128], cqn[:, kc, :], kc == 0, kc == 5,
                                   [t_W, t_cqn], [t_ps[pi]])
                        sch.tt("dve", QN[:, i, :], psf[pi], rq_b, ALU.mult, [t_ps[pi], t_rq], [t_QN])
                    for g in range(3):
                        nh = 3 if g < 2 else 2
                        rows = nh * 32
                        for kc in range(6):
                            sch.mm(psf[0][0:rows, :], WuqR[:, kc, g * 96:g * 96 + rows], cqn[:, kc, :], kc == 0, kc == 5,
                                   [t_W, t_cqn], [t_ps[0]])
                        rope(rows, 0, 1, tabC, tabS, t_tab, P32, xb, t_xb, t1, t_t1, t2, t_t2, QR[0:rows, g, :], t_QR,
                             rstd=rq_b, t_rstd=t_rq)
                    for i in range(4):
                        pi = i % 2
                        for kc in range(2):
                            sch.mm(psf[pi], WukvK[:, kc, i * 128:(i + 1) * 128], ckvn[:, kc, :], kc == 0, kc == 1,
                                   [t_W, t_ckvn], [t_ps[pi]])
                        sch.tt("dve", KN[:, i, cols], psf[pi], rkv_b, ALU.mult, [t_ps[pi], t_rkv], [t_KN[c]])
                    for t in range(4):
                        pi = t % 2
                        for kc in range(2):
                            sch.mm(psf[pi], ckvn[:, kc, t * 128:(t + 1) * 128], WukvV[:, kc, :], kc == 0, kc == 1,
                                   [t_W, t_ckvn], [t_ps[pi]])
                        sch.ts("dve", VC[:, c * 4 + t, :, 0:64], psf[pi].rearrange("p (h d) -> p h d", h=8),
                               rtok[:, t:t + 1], None, ALU.mult, None, [t_ps[pi], t_rtok], [t_VC[c]])
                    nkb = 4 * (c + 1)
                    steps = [(h, kb) for h in range(8) for kb in range(nkb)]

                    def mla_qk(si):
                        h, kb = steps[si]
                        i, off = h // 2, (h % 2) * 64
                        g, goff = h // 3, (h % 3) * 32
                        pi = 2 + (si % 2)
                        kc_ = kb // 4
                        kcols = slice(kb * 128, (kb + 1) * 128)
                        diag = kb >= 4 * c
                        sch.mm(psf[pi], KN[off:off + 64, i, kcols], QN[off:off + 64, i, :], True, False,
                               [t_KN[kc_], t_QN], [t_ps[pi]])
                        sch.mm(psf[pi], KR3[goff:goff + 32, kcols], QR[goff:goff + 32, g, :], False, not diag,
                               [t_KR[kc_], t_QR], [t_ps[pi]])
                        if diag:
                            sch.mm(psf[pi], ident_b, cmask[:, kb - 4 * c, :], False, True, [t_cm, t_const], [t_ps[pi]])
                        pt = si % 3
                        sch.act(PT[pt], psf[pi], AF.Exp, [t_ps[pi]], [t_PT[pt]], scale=MLA_SCALE)

                    def mla_pv(si):
                        h, kb = steps[si]
                        po = 4 + (h % 2)
                        pt = si % 3
                        sch.mm(psf[po][0:65, :], VC[:, kb, h, 0:65], PT[pt], kb == 0, kb == nkb - 1,
                               [t_VC[kb // 4], t_PT[pt]], [t_ps[po]])

                    def mla_norm_a(h):
                        po = 4 + (h % 2)
                        sch.op("dve", lambda e, po=po: e.reciprocal(out=rec[64:65, :], in_=psf[po][64:65, :]),
                               [t_ps[po]], [t_rec])

                    def mla_norm_b(h):
                        i, off = h // 2, (h % 2) * 64
                        po = 4 + (h % 2)
                        sch.mm(psf[6][0:64, :], ones_f[64:65, 0:64], rec[64:65, :], True, True, [t_rec, t_const], [t_ps[6]])
                        sch.copy("act", pbs[0:64, :], psf[6][0:64, :], [t_ps[6]], [t_pbs])
                        sch.tt("dve", OA[off:off + 64, i, cols], psf[po][0:64, :], pbs[0:64, :], ALU.mult,
                               [t_ps[po], t_pbs], [t_OA[c]])

                    mla_qk(0)
                    pend = None
                    for si in range(len(steps)):
                        if si + 1 < len(steps):
                            mla_qk(si + 1)
                        mla_pv(si)
                        if pend is not None:
                            pend[1] -= 1
                            if pend[1] == 0:
                                mla_norm_b(pend[0])
                                pend = None
                        h, kb = steps[si]
                        if kb == nkb - 1:
                            if pend is not None:
                                mla_norm_b(pend[0])
                            mla_norm_a(h)
                            pend = [h, 2]
                    if pend is not None:
                        mla_norm_b(pend[0])

                if dbg == "p1":
                    break
                sch.barrier()
                A = Carver(PHASE_BASE)
                Vbf = alloc(A, BF16, [128, 256])
                Vz = alloc(A, BF16, [128, 256])
                Gs = alloc(A, F32, [128, 256])
                Ktok = alloc(A, BF16, [128, 256])
                AT = alloc(A, BF16, [128, 4, 128])
                rsb = alloc(A, F32, [128, 256])
                oct_ = alloc(A, BF16, [128, 256])
                bst = alloc(A, F32, [128, 4, 6])
                bag = alloc(A, F32, [128, 4, 2])
                Win = alloc(A, BF16, [128, 8, 1704])
                Wout = alloc(A, BF16, [128, 8, D])
                KB2 = alloc(A, BF16, [128, S])
                KI3 = alloc(A, BF16, [128, S])
                VB = alloc(A, BF16, [128, NT, 65])
                hT = alloc(A, BF16, [128, 8, CH])
                rb = alloc(A, F32, [128, CH])
                tC32 = alloc(A, BF16, [128, CH])
                tS32 = alloc(A, BF16, [128, CH])
                tC64 = alloc(A, BF16, [128, CH])
                tS64 = alloc(A, BF16, [128, CH])
                xb = alloc(A, BF16, [128, CH])
                t1 = alloc(A, BF16, [128, CH])
                t2 = alloc(A, BF16, [128, CH])
                QB = alloc(A, BF16, [128, 2, CH])
                QI = alloc(A, BF16, [128, 3, CH])
                WI = alloc(A, F32, [128, 4, 8])
                RQ = alloc(A, BF16, [128, 2, CH])
                RQX = alloc(A, BF16, [128, 2, CH])
                RK = alloc(A, BF16, [128, 2, CH])
                xiT = alloc(A, F32, [128, 2, 128])
                decT = alloc(A, F32, [128, 4, 128])
                zetab = alloc(A, F32, [128, 256])
                cdb = alloc(A, F32, [128, 256])
                cb = alloc(A, F32, [128, 128])
                ctab = alloc(A, F32, [128, NIT + 1])
                gret = gp[:, l, 24:280]
                OT = alloc(A, BF16, [128, 4, CH])
                Ibuf = alloc(A, F32, [128, S])
                Rtmp2 = alloc(A, BF16, [128, 2, CH])
                Rtmp = [Rtmp2[:, 0, :], Rtmp2[:, 1, :]]
                sqb = Rtmp2
                Mb = alloc(A, BF16, [128, S])
                junk = Mb
                MbT = [alloc(A, BF16, [128, NT, 128]) for _ in range(2)]
                PTd = [alloc(A, BF16, [128, 4, 128]) for _ in range(2)]
                smA = alloc(A, F32, [128, 2])
                smR = alloc(A, F32, [128, 4])
                OBt = alloc(A, BF16, [128, 256])
                sm = alloc(A, F32, [128, 64])
                wtab = alloc(A, F32, [128, NIT + 1])
                Rst = alloc(A, F32, [128, 256])
                Rbf = alloc(A, BF16, [128, 256])
                t_W, t_Wo, t_cst = Tok(), Tok(), Tok()
                t_KB = [Tok() for _ in range(NCH)]
                t_KI = [Tok() for _ in range(NCH)]
                t_VB = [Tok() for _ in range(NCH)]
                t_h, t_rb = Tok(), Tok()
                t_tab, t_xb, t_t1, t_t2 = Tok(), Tok(), Tok(), Tok()
                t_QB, t_QI, t_WI, t_RQ, t_RQX, t_RK, t_OT = Tok(), Tok(), Tok(), Tok(), Tok(), Tok(), Tok()
                t_I, t_junk, t_Mb, t_OBt, t_sm, t_wtab = Tok(), Tok(), Tok(), Tok(), Tok(), Tok()
                t_MbT = [Tok(), Tok()]
                t_smA, t_smR = Tok(), Tok()
                t_Rtmp = [Tok(), Tok()]
                t_sq = t_Rtmp
                t_junk = t_Mb
                t_PTd = [Tok(), Tok()]
                t_Rst, t_Rbf, t_Vbf, t_Vz, t_Gs, t_Ktok, t_AT, t_rsb, t_oct, t_bst = (Tok() for _ in range(10))

                for kc in range(8):
                    sch.dma("pool", Win[:, kc, :], dr["w_in"][l, kc * 128:(kc + 1) * 128, 1056:2760], (), [t_W])
                for kc in range(8):
                    sch.dma("pool", Wout[:, kc, :], dr["w_out"][l, kc * 128:(kc + 1) * 128, :], (), [t_Wo])
                sch.dma("sp", xiT, dr["xiT"][:, :, 0:128], (), [t_cst])
                sch.dma("sp", decT, dr["decT"], (), [t_cst])
                sch.dma("sp", zetab, dr["zetab"], (), [t_cst])
                sch.dma("sp", cdb[0:64, :], dr["cdb"], (), [t_cst])
                sch.dma("sp", cb, dr["cb"], (), [t_cst])
                sch.dma("sp", ctab, dr["ctab"], (), [t_cst])
                sch.memset("pool", VB[:, :, 64:65], 1.0, [t_VB[c] for c in range(NCH)])
                sch.memset("pool", Rst[:, :], 0.0, [t_Rst])
                sch.memset("pool", Rbf[:, :], 0.0, [t_Rbf])

                def wc(a, b):
                    return slice(a - 1056, b - 1056)

                for c in range(NCH):
                    cols = slice(c * CH, (c + 1) * CH)
                    sch.dma("pool", tC32, dr["C32"][:, cols], (), [t_tab])
                    sch.dma("pool", tS32, dr["S32"][:, cols], (), [t_tab])
                    sch.dma("pool", tC64, dr["C64"][:, cols], (), [t_tab])
                    sch.dma("pool", tS64, dr["S64"][:, cols], (), [t_tab])
                    chunk_norm(c, lambda kc: gvec(l, "mix", kc), hT, t_h, sqb, t_sq, rb, t_rb, 0)

                    def proj(pi, c0, c1, rows):
                        for kc in range(8):
                            sch.mm(psf[pi][0:rows, :], Win[:, kc, wc(c0, c1)], hT[:, kc, :], kc == 0, kc == 7,
                                   [t_W, t_h], [t_ps[pi]])
                    if stop == "setup":
                        break
                    for i in range(2):
                        proj(0, 1056 + 128 * i, 1056 + 128 * (i + 1), 128)
                        rope(128, 0, 1, tC64, tS64, t_tab, P64, xb, t_xb, t1, t_t1, t2, t_t2, QB[:, i, :], t_QB)
                    proj(0, 1312, 1376, 64)
                    rope(64, 0, 1, tC64, tS64, t_tab, P64, xb, t_xb, t1, t_t1, t2, t_t2, KB2[0:64, cols], t_KB[c])
                    sch.copy("pool", KB2[64:128, cols], KB2[0:64, cols], [t_KB[c]], [t_KB[c]])
                    for g in range(3):
                        nh = 3 if g < 2 else 2
                        proj(0, 1440 + 96 * g, 1440 + 96 * g + 32 * nh, 32 * nh)
                        rope(32 * nh, 0, 1, tC32, tS32, t_tab, P32, xb, t_xb, t1, t_t1, t2, t_t2, QI[0:32 * nh, g, :], t_QI)
                    proj(0, 1696, 1728, 32)
                    rope(32, 0, 1, tC32, tS32, t_tab, P32, xb, t_xb, t1, t_t1, t2, t_t2, KI3[0:32, cols], t_KI[c])
                    sch.copy("pool", KI3[32:64, cols], KI3[0:32, cols], [t_KI[c]], [t_KI[c]])
                    sch.copy("pool", KI3[64:96, cols], KI3[0:32, cols], [t_KI[c]], [t_KI[c]])
                    if stop == "proj1":
                        break
                    for i in range(2):
                        proj(0, 1736 + 128 * i, 1736 + 128 * (i + 1), 128)
                        rope(128, 0, 1, tC64, tS64, t_tab, P64, xb, t_xb, t1, t_t1, t2, t_t2, RQ[:, i, :], t_RQ)
                        proj(0, 1992 + 128 * i, 1992 + 128 * (i + 1), 128)
                        rope(128, 0, 1, tC64, tS64, t_tab, P64, xb, t_xb, t1, t_t1, t2, t_t2, RK[:, i, :], t_RK)
                    if stop == "proj2":
                        break
                    for t in range(4):
                        tcols = slice(t * 128, (t + 1) * 128)
                        for kc in range(8):
                            sch.mm(psf[2][:, 0:64], hT[:, kc, tcols], Win[:, kc, wc(1376, 1440)], kc == 0, kc == 7,
                                   [t_W, t_h], [t_ps[2]])
                        for kc in range(8):
                            sch.mm(psf[2][:, 64:128], hT[:, kc, tcols], Win[:, kc, wc(1728, 1792)], kc == 0, kc == 7,
                                   [t_W, t_h], [t_ps[2]])
                        sch.copy("act", VB[:, c * 4 + t, 0:64], psf[2][:, 0:64], [t_ps[2]], [t_VB[c]])
                        sch.act(WI[:, t, :], psf[2][:, 64:72], AF.Copy, [t_ps[2]], [t_WI], scale=IDX_SCALE)

                    if stop == "proj":
                        break
                    def dsa_idx(j):
                        jt = c * 4 + j
                        qcols = slice(j * 128, (j + 1) * 128)
                        W = 128 * (jt + 1)
                        ngrp = (W + 511) // 512
                        n = 0
                        for h in range(8):
                            g, goff = h // 3, (h % 3) * 32
                            for kg in range(ngrp):
                                k0, k1 = kg * 512, min(W, kg * 512 + 512)
                                pi = 2 + n % 2
                                rt = n % 2
                                n += 1
                                sch.mm(psf[pi][:, 0:k1 - k0], QI[goff:goff + 32, g, qcols], KI3[goff:goff + 32, k0:k1], True, True,
                                       [t_QI] + [t_KI[k0 // 512]], [t_ps[pi]])
                                sch.act(Rtmp[rt][:, 0:k1 - k0], psf[pi][:, 0:k1 - k0], AF.Relu, [t_ps[pi]], [t_Rtmp[rt]])
                                if h == 0:
                                    sch.ts("dve", Ibuf[:, k0:k1], Rtmp[rt][:, 0:k1 - k0], WI[:, j, 0:1], None, ALU.mult, None,
                                           [t_Rtmp[rt], t_WI], [t_I])
                                else:
                                    sch.stt(Ibuf[:, k0:k1], Rtmp[rt][:, 0:k1 - k0], WI[:, j, h:h + 1], Ibuf[:, k0:k1],
                                            ALU.mult, ALU.add, [t_Rtmp[rt], t_WI, t_I], [t_I])

                    def dsa_bis(j):
                        jt = c * 4 + j
                        W = 128 * (jt + 1)
                        sch.op("dve", lambda e, W=W: e.tensor_reduce(out=sm[:, 0:1], in_=Ibuf[:, 0:W], axis=AX.X, op=ALU.max,
                                                                      apply_absolute_value=True), [t_I], [t_sm])
                        sch.ts("dve", sm[:, 1:2], sm[:, 0:1], 2.0, 2.0, ALU.mult, ALU.add, [t_sm], [t_sm])
                        sch.ts("dve", wtab[:, :], ctab[:, :], sm[:, 1:2], None, ALU.mult, None, [t_sm, t_cst], [t_wtab])
                        sch.tt("dve", Ibuf[:, W - 128:W], Ibuf[:, W - 128:W], cb[:, :], ALU.add, [t_I, t_cst], [t_I])
                        sch.memset("dve", sm[:, 2:3], 0.0, [t_sm])
                        for it in range(NIT):
                            sch.ts("dve", junk[:, 0:W], Ibuf[:, 0:W], sm[:, 2:3], 0.0, ALU.is_ge, ALU.add, [t_I, t_sm], [t_junk, t_sm],
                                   accum=sm[:, 3:4])
                            sch.ts("dve", sm[:, 4:5], sm[:, 3:4], 255.5, 0.5, ALU.is_ge, ALU.subtract, [t_sm], [t_sm])
                            sch.stt(sm[:, 2:3], sm[:, 4:5], wtab[:, it:it + 1], sm[:, 2:3], ALU.mult, ALU.add, [t_sm, t_wtab], [t_sm])
                        sch.tt("dve", sm[:, 5:6], sm[:, 2:3], wtab[:, NIT:NIT + 1], ALU.subtract, [t_sm, t_wtab], [t_sm])
                        sch.ts("dve", Mb[:, 0:W], Ibuf[:, 0:W], sm[:, 5:6], NEG, ALU.is_lt, ALU.mult, [t_I, t_sm], [t_Mb])

                    def dsa_tr(j):
                        jt = c * 4 + j
                        mb = jt % 2
                        for kb0 in range(0, jt + 1, 8):
                            nb = min(8, jt + 1 - kb0)
                            for ii in range(nb):
                                kb = kb0 + ii
                                sch.tr(psb[:, ii * 128:(ii + 1) * 128], Mb[:, kb * 128:(kb + 1) * 128], ident_b,
                                       [t_Mb, t_const], [t_psb])
                            sch.copy("act", MbT[mb][:, kb0:kb0 + nb, :], psb[:, 0:nb * 128].rearrange("p (a b) -> p a b", a=nb),
                                     [t_psb], [t_MbT[mb]])

                    def dsa_attn(j):
                        jt = c * 4 + j
                        mb = jt % 2
                        qcols = slice(j * 128, (j + 1) * 128)
                        steps = [(h, kb0) for h in range(4) for kb0 in range(0, jt + 1, 4)]

                        def qk(si):
                            h, kb0 = steps[si]
                            i, off = h // 2, (h % 2) * 64
                            nb = min(4, jt + 1 - kb0)
                            pi = si % 2
                            for ii in range(nb):
                                kb = kb0 + ii
                                sch.mm(psf[pi][:, ii * 128:(ii + 1) * 128], KB2[off:off + 64, kb * 128:(kb + 1) * 128],
                                       QB[off:off + 64, i, qcols], True, False, [t_KB[kb // 4], t_QB], [t_ps[pi]])
                                sch.mm(psf[pi][:, ii * 128:(ii + 1) * 128], ident_b, MbT[mb][:, kb, :], False, True,
                                       [t_MbT[mb], t_const], [t_ps[pi]])
                            sch.act(PTd[pi][:, 0:nb, :], psf[pi][:, 0:nb * 128].rearrange("p (a b) -> p a b", a=nb), AF.Exp,
                                    [t_ps[pi]], [t_PTd[pi]], scale=DSA_SCALE)

                        def pv(si):
                            h, kb0 = steps[si]
                            nb = min(4, jt + 1 - kb0)
                            pi = si % 2
                            po = 4 + (h % 2)
                            for ii in range(nb):
                                kb = kb0 + ii
                                sch.mm(psf[po][:, 0:65], PTd[pi][:, ii, :], VB[:, kb, 0:65], kb == 0, kb == jt,
                                       [t_PTd[pi], t_VB[kb // 4]], [t_ps[po]])
                            if kb0 + nb == jt + 1:
                                sch.op("dve", lambda e, po=po: e.reciprocal(out=smA[:, 0:1], in_=psf[po][:, 64:65]), [t_ps[po]], [t_smA])
                                sch.ts("dve", OBt[:, h * 64:(h + 1) * 64], psf[po][:, 0:64], smA[:, 0:1], None, ALU.mult, None,
                                       [t_ps[po], t_smA], [t_OBt])

                        qk(0)
                        for si in range(len(steps)):
                            if si + 1 < len(steps):
                                qk(si + 1)
                            pv(si)
                        for i in range(2):
                            sch.tr(psb[:, i * 128:(i + 1) * 128], OBt[:, i * 128:(i + 1) * 128], ident_b, [t_OBt, t_const], [t_psb])
                        sch.copy("act", OT[:, 0:2, qcols], psb[:, 0:256].rearrange("p (a b) -> p a b", a=2), [t_psb], [t_OT])

                    def ret_tile(j):
                        qcols = slice(j * 128, (j + 1) * 128)
                        for i in range(2):
                            sch.tt("pool", RQX[:, i, qcols], RQ[:, i, qcols], xiT[:, i, :], ALU.mult, [t_RQ, t_cst], [t_RQX])
                        for kc in range(8):
                            sch.mm(psf[6], hT[:, kc, qcols], Win[:, kc, wc(2248, 2760)], kc == 0, kc == 7, [t_W, t_h], [t_ps[6]])
                        sch.copy("act", Vbf[:, :], psf[6][:, 0:256], [t_ps[6]], [t_Vbf])
                        sch.tt("pool", Vz[:, :], Vbf[:, :], zetab[:, :], ALU.mult, [t_Vbf, t_cst], [t_Vz])
                        sch.act(Gs[:, :], psf[6][:, 256:512], AF.Exp, [t_ps[6]], [t_Gs], scale=-1.0)
                        sch.ts("pool", Gs[:, :], Gs[:, :], 1.0, None, ALU.add, None, [t_Gs], [t_Gs])
                        sch.op("dve", lambda e: e.reciprocal(out=Gs[:, :], in_=Gs[:, :]), [t_Gs], [t_Gs])
                        sch.tt("dve", Gs[:, :], Gs[:, :], psf[6][:, 256:512], ALU.mult, [t_Gs, t_ps[6]], [t_Gs])
                        for i in range(2):
                            sch.tr(psb[:, i * 128:(i + 1) * 128], RK[:, i, qcols], ident_b, [t_RK, t_const], [t_psb])
                        sch.copy("act", Ktok[:, :], psb[:, 0:256], [t_psb], [t_Ktok])
                        for h in range(4):
                            i, off = h // 2, (h % 2) * 64
                            sch.mm(psf[2 + h % 2][:, (h // 2) * 128:(h // 2 + 1) * 128], RK[off:off + 64, i, qcols],
                                   RQ[off:off + 64, i, qcols], True, True, [t_RK, t_RQ], [t_ps[2 + h % 2]])
                        for par in range(2):
                            sch.tt("dve", AT[:, 2 * par:2 * par + 2, :], psf[2 + par][:, 0:256].rearrange("p (a b) -> p a b", a=2),
                                   decT[:, 2 * par:2 * par + 2, :], ALU.mult, [t_ps[2 + par], t_cst], [t_AT])
                        for h in range(4):
                            i, off = h // 2, (h % 2) * 64
                            sch.mm(psf[3][:, h * 64:(h + 1) * 64], AT[:, (h % 2) * 2 + h // 2, :], Vbf[:, h * 64:(h + 1) * 64], True, False,
                                   [t_AT, t_Vbf], [t_ps[3]])
                            sch.mm(psf[3][:, h * 64:(h + 1) * 64], RQX[off:off + 64, i, qcols], Rbf[off:off + 64, h * 64:(h + 1) * 64],
                                   False, True, [t_RQX, t_Rbf], [t_ps[3]])
                        for h in range(4):
                            sch.mm(psf[6][0:64, h * 64:(h + 1) * 64], Ktok[:, h * 64:(h + 1) * 64], Vz[:, h * 64:(h + 1) * 64], True, True,
                                   [t_Ktok, t_Vz], [t_ps[6]])
                        sch.tt("pool", Rst[0:64, :], Rst[0:64, :], cdb[0:64, :], ALU.mult, [t_Rst, t_cst], [t_Rst])
                        sch.tt("dve", Rst[0:64, :], Rst[0:64, :], psf[6][0:64, 0:256], ALU.add, [t_Rst, t_ps[6]], [t_Rst])
                        sch.copy("act", Rbf[0:64, :], Rst[0:64, :], [t_Rst], [t_Rbf])
                        sch.copy("act", Rbf[64:128, :], Rst[0:64, :], [t_Rst], [t_Rbf])
                        sch.copy("act", rsb[:, :], psf[3][:, 0:256], [t_ps[3]], [t_rsb])
                        for h in range(4):
                            sch.op("dve", lambda e, h=h: e.bn_stats(out=bst[:, h, :], in_=rsb[:, h * 64:(h + 1) * 64]), [t_rsb], [t_bst])
                            sch.op("dve", lambda e, h=h: e.bn_aggr(out=bag[:, h, :], in_=bst[:, h, :]), [t_bst], [t_bst])
                        sch.act(smR[:, 0:4], bag[:, :, 1], AF.Sqrt, [t_bst], [t_smR], bias=EPS)
                        sch.op("dve", lambda e: e.reciprocal(out=smR[:, 0:4], in_=smR[:, 0:4]), [t_smR], [t_smR])
                        for h in range(4):
                            sch.ts("dve", rsb[:, h * 64:(h + 1) * 64], rsb[:, h * 64:(h + 1) * 64], bag[:, h, 0:1], smR[:, h:h + 1],
                                   ALU.subtract, ALU.mult, [t_rsb, t_bst, t_smR], [t_rsb])
                        sch.tt("pool", rsb[:, :], rsb[:, :], gret, ALU.mult, [t_rsb, t_const], [t_rsb])
                        sch.tt("pool", oct_[:, :], rsb[:, :], Gs[:, :], ALU.mult, [t_rsb, t_Gs], [t_oct])
                        for i in range(2):
                            sch.tr(psb[:, i * 128:(i + 1) * 128], oct_[:, i * 128:(i + 1) * 128], ident_b, [t_oct, t_const], [t_psb])
                        sch.copy("act", OT[:, 2:4, qcols], psb[:, 0:256].rearrange("p (a b) -> p a b", a=2), [t_psb], [t_OT])

                    prev = None
                    for j in range(4):
                        dsa_idx(j)
                        dsa_bis(j)
                        if prev is not None:
                            dsa_attn(prev)
                        ret_tile(j)
                        dsa_tr(j)
                        prev = j
                    dsa_attn(prev)

                    for oc in range(8):
                        pi = oc % 2
                        for mc in range(8):
                            if mc < 4:
                                sch.mm(psf[pi], Wout[:, mc, oc * 128:(oc + 1) * 128], OA[:, mc, cols], mc == 0, False,
                                       [t_Wo, t_OA[c]], [t_ps[pi]])
                            else:
                                sch.mm(psf[pi], Wout[:, mc, oc * 128:(oc + 1) * 128], OT[:, mc - 4, :], False, mc == 7,
                                       [t_Wo, t_OT], [t_ps[pi]])
                        sch.tt("dve", xT[:, oc, cols], xT[:, oc, cols], psf[pi], ALU.add, [t_xT[c], t_ps[pi]], [t_xT[c]])

                if dbg == "p2":
                    break
                sch.barrier()
                A = Carver(PHASE_BASE)
                hfT = alloc(A, BF16, [128, 8, S])
                W1 = [alloc(A, BF16, [128, 8, 1024]) for _ in range(2)]
                W2 = [alloc(A, BF16, [128, 8, D]) for _ in range(2)]
                hid = [alloc(A, BF16, [128, 8, CH]) for _ in range(2)]
                rl = [alloc(A, BF16, [128, CH]) for _ in range(2)]
                sqb = alloc(A, BF16, [128, 2, CH])
                rb = alloc(A, F32, [128, CH])
                t_hf = [Tok() for _ in range(NCH)]
                t_W1 = [Tok(), Tok()]
                t_W2 = [Tok(), Tok()]
                t_hid = [Tok(), Tok()]
                t_rl = [Tok(), Tok()]
                t_sq = [Tok(), Tok()]
                t_rb = Tok()

                def load_q(qr):
                    b = qr % 2
                    for kc in range(8):
                        sch.dma("pool", W1[b][:, kc, :], dr["w_ff1"][l, kc * 128:(kc + 1) * 128, qr * 1024:(qr + 1) * 1024], (), [t_W1[b]])
                    for fc in range(8):
                        sch.dma("pool", W2[b][:, fc, :], dr["w_ff2"][l, qr * 1024 + fc * 128: qr * 1024 + (fc + 1) * 128, :], (), [t_W2[b]])

                load_q(0)
                for c in range(NCH):
                    chunk_norm(c, lambda kc: gvec(l, "mlp", kc), hfT[:, :, c * CH:(c + 1) * CH], t_hf[c], sqb, t_sq, rb, t_rb, 0)
                for qr in range(4):
                    b = qr % 2
                    if qr + 1 < 4:
                        load_q(qr + 1)
                    for c in range(NCH):
                        cols = slice(c * CH, (c + 1) * CH)
                        hb = c % 2
                        for fc in range(8):
                            pi = fc % 2
                            for kc in range(8):
                                sch.mm(psf[pi], W1[b][:, kc, fc * 128:(fc + 1) * 128], hfT[:, kc, cols], kc == 0, kc == 7,
                                       [t_W1[b], t_hf[c]], [t_ps[pi]])
                            sch.act(rl[pi], psf[pi], AF.Relu, [t_ps[pi]], [t_rl[pi]])
                            sch.tt("pool", hid[hb][:, fc, :], rl[pi], rl[pi], ALU.mult, [t_rl[pi]], [t_hid[hb]])
                        for oc in range(8):
                            pi = 2 + oc % 2
                            for fc in range(8):
                                sch.mm(psf[pi], W2[b][:, fc, oc * 128:(oc + 1) * 128], hid[hb][:, fc, :], fc == 0, fc == 7,
                                       [t_W2[b], t_hid[hb]], [t_ps[pi]])
                            sch.tt("dve", xT[:, oc, cols], xT[:, oc, cols], psf[pi], ALU.add, [t_xT[c], t_ps[pi]], [t_xT[c]])

            sch.barrier()
            Lc = Carver(PHASE_BASE)
            yT = alloc(Lc, F32, [128, 8, CH])
            yo = [alloc(Lc, F32, [128, D]) for _ in range(2)]
            sqb = alloc(Lc, BF16, [128, 2, CH])
            rb = alloc(Lc, F32, [128, CH])
            t_y, t_rb = Tok(), Tok()
            t_sq = [Tok(), Tok()]
            t_yo = [Tok(), Tok()]
            for c in range(NCH):
                cols = slice(c * CH, (c + 1) * CH)
                if dbg:
                    src = None
                if final_norm:
                    for kc in range(8):
                        sch.act(sqb[:, kc % 2, :], xT[:, kc, cols], AF.Square, [t_xT[c]], [t_sq[kc % 2]])
                        sch.mm(psf[0], ones_b, sqb[:, kc % 2, :], kc == 0, kc == 7, [t_sq[kc % 2], t_const], [t_ps[0]])
                    sch.act(rb, psf[0], AF.Sqrt, [t_ps[0]], [t_rb], scale=1.0 / D, bias=EPS)
                    sch.op("dve", lambda e: e.reciprocal(out=rb, in_=rb), [t_rb], [t_rb])
                    for kc in range(8):
                        sch.stt(yT[:, kc, :], xT[:, kc, cols], gfin[:, kc:kc + 1], rb, ALU.mult, ALU.mult,
                                [t_xT[c], t_rb, t_const], [t_y])
                    srcT = lambda kc, t: yT[:, kc, t * 128:(t + 1) * 128]
                    t_src = t_y
                else:
                    srcT = lambda kc, t, c=c: xT[:, kc, c * CH + t * 128: c * CH + (t + 1) * 128]
                    t_src = t_xT[c]
                for t in range(4):
                    tt_ = c * 4 + t
                    b = tt_ % 2
                    for half in range(2):
                        pi = 1 + half
                        for k4 in range(4):
                            kc = half * 4 + k4
                            sch.tr(psf[pi][:, k4 * 128:(k4 + 1) * 128], srcT(kc, t), ident_f, [t_src, t_const], [t_ps[pi]])
                        sch.copy("act" if half == 0 else "dve", yo[b][:, half * 512:(half + 1) * 512], psf[pi], [t_ps[pi]], [t_yo[b]])
                    sch.dma("sp", dr["y"][sq_i * S + tt_ * 128: sq_i * S + (tt_ + 1) * 128, :], yo[b], [t_yo[b]], [t_yo[b]])

        if dbg:
            sch.barrier()
            if dbg == "p1":
                Lc = Carver(PHASE_BASE)
                tmp = alloc(Lc, F32, [128, 4, S])
                tk = Tok()
                for i in range(4):
                    sch.copy("dve", tmp[:, i, :], OA[:, i, :], [t_OA[c] for c in range(NCH)], [tk])
                sch.dma("sp", dr["dbg"][:, 0:4, :], tmp, [tk], [tk])
            else:
                sch.dma("sp", dr["dbg"], xT, [t_xT[c] for c in range(NCH)], [Tok()])
        final_waits = [(k, sch.dma_cnt[k]) for k in range(sch.n_dma_sems) if sch.dma_cnt[k] > 0]
        sch.emit(nc, block, sems, dsems, final_waits)
    return nc


NCORES = 8
_CACHE = {}


def _gpack(g_mix, g_q, g_kv, g_mlp, g_ret):
    L = g_mix.shape[0]
    out = np.zeros((L, 128, NG), np.float32)
    for l in range(L):
        out[l, :, 0:8] = np.asarray(g_mix[l], np.float32).reshape(8, 128).T
        out[l, :, 8:14] = np.asarray(g_q[l], np.float32).reshape(6, 128).T
        out[l, :, 14:16] = np.asarray(g_kv[l], np.float32).reshape(2, 128).T
        out[l, :, 16:24] = np.asarray(g_mlp[l], np.float32).reshape(8, 128).T
        out[l, :, 24:280] = np.broadcast_to(np.asarray(g_ret[l], np.float32)[None, :], (128, 256))
    return out


def run_layers(x, layers, final_norm, weights, g_final, ncores=NCORES, nseq=None, dbg=None):
    B = x.shape[0]
    nseq = B // ncores if nseq is None else nseq
    L = len(layers)
    key = (nseq, L, final_norm, dbg)
    if key not in _CACHE:
        _CACHE[key] = build_nc(nseq, L, final_norm, dbg)
    nc = _CACHE[key]
    consts = _consts()
    common = {"c_" + k: np.ascontiguousarray(v, dtype=np.float32) for k, v in consts.items()}
    for nm in ("w_in", "w_uq", "w_ukv", "w_out", "w_ff1", "w_ff2"):
        common[nm] = np.ascontiguousarray(np.asarray(weights[nm], np.float32)[layers])
    common["gpack"] = _gpack(*[np.asarray(weights[n])[layers] for n in ("g_mix", "g_q", "g_kv", "g_mlp", "g_ret")])
    common["gfin"] = np.ascontiguousarray(np.asarray(g_final, np.float32).reshape(8, 128).T)
    in_maps = []
    for ci in range(ncores):
        m = dict(common)
        m["x"] = np.ascontiguousarray(np.asarray(x[ci * nseq:(ci + 1) * nseq], np.float32).reshape(nseq * S, D))
        in_maps.append(m)
    res = run_bass_kernel_spmd(nc, in_maps, core_ids=list(range(ncores)))
    y = np.concatenate([r["y"].reshape(nseq, S, D) for r in res.results], axis=0)
    if dbg:
        return y, [r["dbg"] for r in res.results]
    return y


FUSED = True


def kernel(x, g_mix, w_in, g_q, w_uq, g_kv, w_ukv, g_ret, w_out, g_mlp, w_ff1, w_ff2, g_final):
    weights = dict(g_mix=g_mix, w_in=w_in, g_q=g_q, w_uq=w_uq, g_kv=g_kv, w_ukv=w_ukv, g_ret=g_ret, w_out=w_out,
                   g_mlp=g_mlp, w_ff1=w_ff1, w_ff2=w_ff2)
    x = np.asarray(x, np.float32)
    depth = np.asarray(w_in).shape[0]
    if FUSED:
        return run_layers(x, list(range(depth)), True, weights, g_final).astype(np.float32)
    y = x
    for l in range(depth):
        y = run_layers(y, [l], l == depth - 1, weights, g_final)
    return y.astype(np.float32)
```

```python
import numpy as np
import concourse.bass as bass
import concourse.mybir as mybir
from concourse.bass_utils import run_bass_kernel_spmd

F32 = mybir.dt.float32
BF16 = mybir.dt.bfloat16
AF = mybir.ActivationFunctionType
ALU = mybir.AluOpType
AX = mybir.AxisListType

D = 1024
S = 2048
NCH = 4
CH = 512
NT = 16
INW = 2760
DFF = 4096
EPS = 1e-6
NIT = 16
MLA_SCALE = 96 ** -0.5
DSA_SCALE = 64 ** -0.5
IDX_SCALE = (8 ** -0.5) * (32 ** -0.5)
NEG = -30000.0
NG = 8 + 6 + 2 + 8 + 256


class Tok:
    __slots__ = ("name", "lw", "rds")

    def __init__(self, name=""):
        self.name = name
        self.lw = None
        self.rds = []


class Op:
    __slots__ = ("eng", "fn", "deps", "inc", "val", "dsem", "dval", "is_dma", "ep")


class Sched:
    ENGS = ("pe", "act", "dve", "pool", "sp")

    def __init__(self, n_dma_sems=24):
        self.ops = {e: [] for e in self.ENGS}
        self.n_dma_sems = n_dma_sems
        self.dma_last = [None] * n_dma_sems
        self.dma_cnt = [0] * n_dma_sems
        self.dma_rr = 0
        self.pending = {e: [] for e in self.ENGS}
        self.epoch = 0
        self.nsets = 12

    def barrier(self):
        lasts = []
        for e in self.ENGS:
            for op in reversed(self.ops[e]):
                if not op.is_dma:
                    lasts.append(op)
                    break
        for d in self.dma_last:
            if d is not None:
                lasts.append(d)
        for e in self.ENGS:
            self.pending[e] = list(lasts)
        self.epoch += 1

    def _rec(self, eng, fn, rd, wr, is_dma=False):
        op = Op()
        op.eng = eng
        op.fn = fn
        op.inc = False
        op.val = None
        op.is_dma = is_dma
        op.ep = self.epoch % self.nsets
        op.dsem = None
        op.dval = None
        deps = list(self.pending[eng])
        self.pending[eng] = []
        for t in rd:
            if t.lw is not None:
                deps.append(t.lw)
        for t in wr:
            if t.lw is not None:
                deps.append(t.lw)
            deps.extend(t.rds)
        if is_dma:
            k = self.dma_rr % self.n_dma_sems
            self.dma_rr += 1
            if self.dma_last[k] is not None:
                deps.append(self.dma_last[k])
            self.dma_last[k] = op
            self.dma_cnt[k] += 16
            op.dsem = k
            op.dval = self.dma_cnt[k]
        fd = []
        seen = set()
        for d in deps:
            if id(d) in seen or d is op:
                continue
            seen.add(id(d))
            if (not d.is_dma) and d.eng == "pe" and eng == "pe":
                continue
            if not d.is_dma:
                d.inc = True
            fd.append(d)
        op.deps = fd
        for t in rd:
            t.rds.append(op)
        for t in wr:
            t.lw = op
            t.rds = []
        self.ops[eng].append(op)
        return op

    def op(self, eng, fn, rd=(), wr=()):
        return self._rec(eng, fn, rd, wr)

    def dma(self, eng, out, in_, rd=(), wr=()):
        return self._rec(eng, lambda e: e.dma_start(out=out, in_=in_), rd, wr, is_dma=True)

    def mm(self, out, lhsT, rhs, start, stop, rd, wr):
        return self.op("pe", lambda e: e.matmul(out, lhsT, rhs, start=start, stop=stop), rd, wr)

    def tr(self, out, in_, ident, rd, wr):
        return self.op("pe", lambda e: e.transpose(out, in_, ident), rd, wr)

    def act(self, out, in_, func, rd, wr, scale=None, bias=None, accum=None):
        kw = {}
        if scale is not None:
            kw["scale"] = scale
        if bias is not None:
            kw["bias"] = bias
        if accum is not None:
            kw["accum_out"] = accum
        return self.op("act", lambda e: e.activation(out=out, in_=in_, func=func, **kw), rd, wr)

    def tt(self, eng, out, in0, in1, op, rd, wr):
        return self.op(eng, lambda e: e.tensor_tensor(out=out, in0=in0, in1=in1, op=op), rd, wr)

    def ts(self, eng, out, in0, s1, s2, op0, op1, rd, wr, accum=None):
        if op1 is None:
            return self.op(eng, lambda e: e.tensor_scalar(out=out, in0=in0, scalar1=s1, scalar2=None, op0=op0), rd, wr)
        if accum is not None:
            return self.op(eng, lambda e: e.tensor_scalar(out=out, in0=in0, scalar1=s1, scalar2=s2, op0=op0,
                                                          op1=op1, accum_out=accum), rd, wr)
        return self.op(eng, lambda e: e.tensor_scalar(out=out, in0=in0, scalar1=s1, scalar2=s2, op0=op0, op1=op1),
                       rd, wr)

    def stt(self, out, in0, scalar, in1, op0, op1, rd, wr):
        return self.op("dve", lambda e: e.scalar_tensor_tensor(out=out, in0=in0, scalar=scalar, in1=in1,
                                                               op0=op0, op1=op1), rd, wr)

    def copy(self, eng, out, in_, rd, wr):
        if eng == "act":
            return self.op("act", lambda e: e.activation(out=out, in_=in_, func=AF.Copy), rd, wr)
        return self.op(eng, lambda e: e.tensor_copy(out=out, in_=in_), rd, wr)

    def memset(self, eng, out, val, wr):
        return self.op(eng, lambda e: e.memset(out, val), (), wr)

    def emit(self, nc, block, sems, dsems, final_waits):
        for eng in self.ENGS:
            c = [0] * self.nsets
            for op in self.ops[eng]:
                if op.inc and not op.is_dma:
                    c[op.ep] += 1
                    op.val = c[op.ep]
        engobj = {"pe": block.tensor, "act": block.scalar, "dve": block.vector, "pool": block.gpsimd,
                  "sp": block.sync}

        def make(eng):
            ops = self.ops[eng]

            def body(e):
                seen = {}
                for op in ops:
                    for d in op.deps:
                        if d.is_dma:
                            key = ("d", d.dsem)
                            sem = dsems[d.dsem]
                            val = d.dval
                        else:
                            key = ("c", d.eng, d.ep)
                            sem = sems[d.ep][d.eng]
                            val = d.val
                        if seen.get(key, 0) >= val:
                            continue
                        seen[key] = val
                        e.wait_ge(sem, val)
                    ins = op.fn(e)
                    if op.is_dma:
                        ins.then_inc(dsems[op.dsem], 16)
                    elif op.inc:
                        ins.then_inc(sems[op.ep][eng], 1)
                if eng == "sp":
                    for (k, v) in final_waits:
                        e.wait_ge(dsems[k], v)
            return body

        for eng in self.ENGS:
            engobj[eng](make(eng))


def _consts():
    c = {}
    pos = np.arange(S, dtype=np.float64)

    def tab(dim):
        inv = 10000.0 ** (-np.arange(0, dim, 2, dtype=np.float64) / dim)
        C = np.zeros((128, S), np.float32)
        Sg = np.zeros((128, S), np.float32)
        P = np.zeros((128, 128), np.float32)
        for p in range(128):
            i = p % dim
            j = i % (dim // 2)
            ang = (pos.astype(np.float32) * np.float32(inv[j])).astype(np.float32)
            C[p] = np.cos(ang)
            Sg[p] = (-np.sin(ang)) if i < dim // 2 else np.sin(ang)
            src = (p // dim) * dim + (i + dim // 2) % dim
            P[src, p] = 1.0
        return C, Sg, P
    c["C32"], c["S32"], c["P32"] = tab(32)
    c["C64"], c["S64"], c["P64"] = tab(64)
    cm = np.zeros((128, 4, 512), np.float32)
    k = np.arange(128)[:, None]
    q = np.arange(512)[None, :]
    for i in range(4):
        cm[:, i, :] = np.where(i * 128 + k > q, NEG, 0.0)
    c["cmask"] = cm
    qq = np.arange(128)[:, None]
    kk = np.arange(128)[None, :]
    c["cb"] = np.where(kk <= qq, 0.0, -1e30).astype(np.float32)
    lg = np.log1p(-np.exp2(-5.0 - np.arange(4, dtype=np.float64)))
    dec = np.zeros((128, 4, 128), np.float32)
    cc = np.arange(128)[:, None]
    q1 = np.arange(128)[None, :]
    for h in range(4):
        dec[:, (h % 2) * 2 + h // 2, :] = np.where(q1 >= cc, 0.125 * np.exp(np.maximum(q1 - cc, 0) * lg[h]), 0.0)
    c["decT"] = dec
    xi = np.zeros((128, 2, 512), np.float32)
    for i in range(2):
        for r in range(128):
            h = 2 * i + r // 64
            xi[r, i, :] = np.tile(np.exp((np.arange(128) + 1.0) * lg[h]), 4)
    c["xiT"] = xi
    zb = np.zeros((128, 256), np.float32)
    cdb = np.zeros((64, 256), np.float32)
    for h in range(4):
        zb[:, h * 64:(h + 1) * 64] = (0.125 * np.exp((127.0 - np.arange(128)) * lg[h]))[:, None]
        cdb[:, h * 64:(h + 1) * 64] = np.exp(128.0 * lg[h])
    c["zetab"] = zb
    c["cdb"] = cdb
    c["ctab"] = np.broadcast_to((2.0 ** -(np.arange(NIT + 1) + 1.0)).astype(np.float32)[None, :], (128, NIT + 1)).copy()
    c["ident"] = np.eye(128, dtype=np.float32)
    c["ones"] = np.ones((128, 128), np.float32)
    return c


CONST_SHAPES = {
    "C32": [128, S], "S32": [128, S], "P32": [128, 128], "C64": [128, S], "S64": [128, S], "P64": [128, 128],
    "cmask": [128, 4, 512], "cb": [128, 128], "decT": [128, 4, 128], "xiT": [128, 2, 512], "zetab": [128, 256],
    "cdb": [64, 256], "ctab": [128, NIT + 1], "ident": [128, 128], "ones": [128, 128],
}


def build_nc(nseq, nlayers, final_norm, dbg=None):
    from contextlib import ExitStack
    nc = bass.Bass("TRN2", target_bir_lowering=False)
    dr = {}
    dr["x"] = nc.dram_tensor("x", [nseq * S, D], F32, kind="ExternalInput").ap()
    dr["y"] = nc.dram_tensor("y", [nseq * S, D], F32, kind="ExternalOutput").ap()
    dr["w_in"] = nc.dram_tensor("w_in", [nlayers, D, INW], F32, kind="ExternalInput").ap()
    dr["w_uq"] = nc.dram_tensor("w_uq", [nlayers, 768, 768], F32, kind="ExternalInput").ap()
    dr["w_ukv"] = nc.dram_tensor("w_ukv", [nlayers, 256, 1024], F32, kind="ExternalInput").ap()
    dr["w_out"] = nc.dram_tensor("w_out", [nlayers, D, D], F32, kind="ExternalInput").ap()
    dr["w_ff1"] = nc.dram_tensor("w_ff1", [nlayers, D, DFF], F32, kind="ExternalInput").ap()
    dr["w_ff2"] = nc.dram_tensor("w_ff2", [nlayers, DFF, D], F32, kind="ExternalInput").ap()
    dr["gpack"] = nc.dram_tensor("gpack", [nlayers, 128, NG], F32, kind="ExternalInput").ap()
    dr["gfin"] = nc.dram_tensor("gfin", [128, 8], F32, kind="ExternalInput").ap()
    for k, shp in CONST_SHAPES.items():
        dr[k] = nc.dram_tensor("c_" + k, shp, F32, kind="ExternalInput").ap()
    if dbg:
        dr["dbg"] = nc.dram_tensor("dbg", [128, 8, S], F32, kind="ExternalOutput").ap()

    stop = None
    if dbg and ":" in dbg:
        dbg, stop = dbg.split(":")
    sch = Sched()
    with ExitStack() as st:
        ARW = 53000
        arena = st.enter_context(nc.sbuf_tensor("arena", [128, ARW], F32))
        psf = [st.enter_context(nc.psum_tensor("ps%d" % i, [128, 512], F32))[:, :] for i in range(7)]
        psb = st.enter_context(nc.psum_tensor("psb", [128, 1024], BF16))[:, :]
        sems = [{e: st.enter_context(nc.semaphore("sem%d_%s" % (k, e))) for e in Sched.ENGS} for k in range(sch.nsets)]
        dsems = [st.enter_context(nc.semaphore("dsem%d" % i)) for i in range(sch.n_dma_sems)]
        block = st.enter_context(nc.Block())

        class Carver:
            def __init__(self, base=0):
                self.off = base

            def take(self, nbytes):
                o = self.off
                self.off += (nbytes + 3) // 4 * 4
                assert self.off <= ARW * 4, ("SBUF arena overflow", self.off, ARW * 4)
                self.peak = max(getattr(self, "peak", 0), self.off)
                return o

        def view(off, dtype, shape):
            n = 1
            for s_ in shape[1:]:
                n *= s_
            esz = 4 if dtype == F32 else 2
            w0 = off // 4
            nw = (n * esz + 3) // 4
            ap = arena[0:shape[0], w0:w0 + nw]
            if dtype != F32:
                ap = ap.bitcast(dtype)
            if len(shape) == 3:
                ap = ap.rearrange("p (a b) -> p a b", a=shape[1])
            elif len(shape) == 4:
                ap = ap.rearrange("p (a b c) -> p a b c", a=shape[1], b=shape[2])
            return ap

        def alloc(C, dtype, shape):
            n = 1
            for s_ in shape[1:]:
                n *= s_
            return view(C.take(n * (4 if dtype == F32 else 2)), dtype, shape)

        G = Carver()
        xT = alloc(G, F32, [128, 8, S])
        OA = alloc(G, BF16, [128, 4, S])
        ident_b = alloc(G, BF16, [128, 128])
        ident_f = alloc(G, F32, [128, 128])
        ones_b = alloc(G, BF16, [128, 128])
        ones_f = alloc(G, F32, [128, 128])
        P32 = alloc(G, BF16, [128, 128])
        P64 = alloc(G, BF16, [128, 128])
        gp = alloc(G, F32, [128, nlayers, NG])
        gfin = alloc(G, F32, [128, 8])
        PHASE_BASE = G.off

        t_xT = [Tok("xT%d" % c) for c in range(NCH)]
        t_OA = [Tok("OA%d" % c) for c in range(NCH)]
        t_const = Tok("const")
        t_ps = [Tok("ps%d" % i) for i in range(7)]
        t_psb = Tok("psb")

        sch.dma("pool", ident_b, dr["ident"], (), [t_const])
        sch.dma("sp", ident_f, dr["ident"], (), [t_const])
        sch.dma("pool", ones_b, dr["ones"], (), [t_const])
        sch.dma("sp", ones_f, dr["ones"], (), [t_const])
        sch.dma("pool", P32, dr["P32"], (), [t_const])
        sch.dma("pool", P64, dr["P64"], (), [t_const])
        for l in range(nlayers):
            sch.dma("sp", gp[:, l, :], dr["gpack"][l], (), [t_const])
        sch.dma("sp", gfin, dr["gfin"], (), [t_const])

        def gvec(l, which, j):
            base = {"mix": 0, "q": 8, "kv": 14, "mlp": 16}[which]
            return gp[:, l, base + j:base + j + 1]

        def chunk_norm(c, gfun, hT_dst, t_h, sq, t_sq, rb, t_rb, pi):
            cols = slice(c * CH, (c + 1) * CH)
            for kc in range(8):
                sch.act(sq[:, kc % 2, :], xT[:, kc, cols], AF.Square, [t_xT[c]], [t_sq[kc % 2]])
                sch.mm(psf[pi], ones_b, sq[:, kc % 2, :], kc == 0, kc == 7, [t_sq[kc % 2], t_const], [t_ps[pi]])
            sch.act(rb, psf[pi], AF.Sqrt, [t_ps[pi]], [t_rb], scale=1.0 / D, bias=EPS)
            sch.op("dve", lambda e: e.reciprocal(out=rb, in_=rb), [t_rb], [t_rb])
            for kc in range(8):
                sch.stt(hT_dst[:, kc, :], xT[:, kc, cols], gfun(kc), rb, ALU.mult, ALU.mult,
                        [t_xT[c], t_rb, t_const], [t_h])

        def rope(rows, pin, pr, Ctab, Stab, t_tab, Pm, xb, t_xb, t1, t_t1, t2, t_t2, dst, t_dst,
                 rstd=None, t_rstd=None):
            if rstd is None:
                sch.copy("act", xb[0:rows, :], psf[pin][0:rows, :], [t_ps[pin]], [t_xb])
            else:
                sch.tt("dve", xb[0:rows, :], psf[pin][0:rows, :], rstd[0:rows, :], ALU.mult, [t_ps[pin], t_rstd], [t_xb])
            sch.mm(psf[pr][0:rows, :], Pm[0:rows, 0:rows], xb[0:rows, :], True, True, [t_xb, t_const], [t_ps[pr]])
            sch.tt("dve", t1[0:rows, :], xb[0:rows, :], Ctab[0:rows, :], ALU.mult, [t_xb, t_tab], [t_t1])
            sch.tt("dve", t2[0:rows, :], psf[pr][0:rows, :], Stab[0:rows, :], ALU.mult, [t_ps[pr], t_tab], [t_t2])
            if isinstance(dst, list):
                for (d_ap, r0) in dst:
                    sch.tt("dve", d_ap, t1[r0:r0 + 32, :], t2[r0:r0 + 32, :], ALU.add, [t_t1, t_t2], [t_dst])
            else:
                sch.tt("dve", dst, t1[0:rows, :], t2[0:rows, :], ALU.add, [t_t1, t_t2], [t_dst])

        for sq_i in range(nseq):
            sch.barrier()
            Lc = Carver(PHASE_BASE)
            xin = [alloc(Lc, F32, [128, D]) for _ in range(2)]
            t_xin = [Tok("xin0"), Tok("xin1")]
            for t in range(NT):
                b = t % 2
                sch.dma("sp", xin[b], dr["x"][sq_i * S + t * 128: sq_i * S + (t + 1) * 128, :], (), [t_xin[b]])
                for half in range(2):
                    for k4 in range(4):
                        kc = half * 4 + k4
                        sch.tr(psf[half][:, k4 * 128:(k4 + 1) * 128], xin[b][:, kc * 128:(kc + 1) * 128], ident_f,
                               [t_xin[b], t_const], [t_ps[half]])
                    for k4 in range(4):
                        kc = half * 4 + k4
                        sch.copy("act" if half == 0 else "dve", xT[:, kc, t * 128:(t + 1) * 128],
                                 psf[half][:, k4 * 128:(k4 + 1) * 128], [t_ps[half]], [t_xT[t // 4]])

            for l in range(nlayers):
                sch.barrier()
                A = Carver(PHASE_BASE)
                Win = alloc(A, BF16, [128, 8, 1056])
                WuqN = alloc(A, BF16, [128, 6, 512])
                WuqR = alloc(A, BF16, [128, 6, 256])
                WukvK = alloc(A, BF16, [128, 2, 512])
                WukvV = alloc(A, BF16, [128, 2, 512])
                KN = alloc(A, BF16, [128, 4, S])
                KR3 = alloc(A, BF16, [128, S])
                VC = alloc(A, BF16, [128, NT, 8, 65])
                hT = alloc(A, BF16, [128, 8, CH])
                sqb = alloc(A, BF16, [128, 2, CH])
                rb = alloc(A, F32, [128, CH])
                rq_b = alloc(A, F32, [128, CH])
                rkv_b = alloc(A, F32, [128, CH])
                cqn = alloc(A, BF16, [128, 6, CH])
                ckvn = alloc(A, BF16, [128, 2, CH])
                tabC = alloc(A, BF16, [128, CH])
                tabS = alloc(A, BF16, [128, CH])
                xb = alloc(A, BF16, [128, CH])
                t1 = alloc(A, BF16, [128, CH])
                t2 = alloc(A, BF16, [128, CH])
                QN = alloc(A, BF16, [128, 8, CH])
                QR = alloc(A, BF16, [128, 8, CH])
                PT = [alloc(A, BF16, [128, CH]) for _ in range(3)]
                cmask = alloc(A, BF16, [128, 4, CH])
                rec = alloc(A, F32, [128, CH])
                pbs = alloc(A, F32, [128, CH])
                rtok = alloc(A, F32, [128, 8])
                t_W = Tok("W1")
                t_KN = [Tok() for _ in range(NCH)]
                t_KR = [Tok() for _ in range(NCH)]
                t_VC = [Tok() for _ in range(NCH)]
                t_h, t_rb, t_rq, t_rkv, t_cqn, t_ckvn = Tok(), Tok(), Tok(), Tok(), Tok(), Tok()
                t_sq = [Tok(), Tok()]
                t_tab, t_xb, t_t1, t_t2, t_QN, t_QR = Tok(), Tok(), Tok(), Tok(), Tok(), Tok()
                t_PT = [Tok(), Tok(), Tok()]
                t_cm, t_rec, t_pbs, t_rtok = Tok(), Tok(), Tok(), Tok()

                for kc in range(8):
                    sch.dma("pool", Win[:, kc, :], dr["w_in"][l, kc * 128:(kc + 1) * 128, 0:1056], (), [t_W])
                for kc in range(6):
                    srcq = dr["w_uq"][l, kc * 128:(kc + 1) * 128, :].rearrange("p (h d) -> p h d", h=8)
                    sch.dma("pool", WuqN[:, kc, :].rearrange("p (h d) -> p h d", h=8), srcq[:, :, 0:64], (), [t_W])
                    sch.dma("pool", WuqR[:, kc, :].rearrange("p (h d) -> p h d", h=8), srcq[:, :, 64:96], (), [t_W])
                for kc in range(2):
                    srck = dr["w_ukv"][l, kc * 128:(kc + 1) * 128, :].rearrange("p (h d) -> p h d", h=8)
                    sch.dma("pool", WukvK[:, kc, :].rearrange("p (h d) -> p h d", h=8), srck[:, :, 0:64], (), [t_W])
                    sch.dma("pool", WukvV[:, kc, :].rearrange("p (h d) -> p h d", h=8), srck[:, :, 64:128], (), [t_W])
                sch.dma("pool", cmask, dr["cmask"], (), [t_cm])
                sch.memset("pool", QN[:, :, :], 0.0, [t_QN])
                sch.memset("pool", QR[:, :, :], 0.0, [t_QR])
                sch.memset("pool", VC[:, :, :, 64:65], 1.0, [t_VC[c] for c in range(NCH)])

                for c in range(NCH):
                    cols = slice(c * CH, (c + 1) * CH)
                    sch.dma("pool", tabC, dr["C32"][:, cols], (), [t_tab])
                    sch.dma("pool", tabS, dr["S32"][:, cols], (), [t_tab])
                    chunk_norm(c, lambda kc: gvec(l, "mix", kc), hT, t_h, sqb, t_sq, rb, t_rb, 0)
                    for j in range(6):
                        pi = j % 2
                        for kc in range(8):
                            sch.mm(psf[pi], Win[:, kc, j * 128:(j + 1) * 128], hT[:, kc, :], kc == 0, kc == 7,
                                   [t_W, t_h], [t_ps[pi]])
                        sch.act(cqn[:, j, :], psf[pi], AF.Copy, [t_ps[pi], t_const], [t_cqn], scale=gvec(l, "q", j))
                        sch.act(sqb[:, j % 2, :], psf[pi], AF.Square, [t_ps[pi]], [t_sq[j % 2]])
                        sch.mm(psf[2], ones_b, sqb[:, j % 2, :], j == 0, j == 5, [t_sq[j % 2], t_const], [t_ps[2]])
                    sch.act(rq_b, psf[2], AF.Sqrt, [t_ps[2]], [t_rq], scale=1.0 / 768, bias=EPS)
                    sch.op("dve", lambda e: e.reciprocal(out=rq_b, in_=rq_b), [t_rq], [t_rq])
                    for j in range(2):
                        pi = j % 2
                        for kc in range(8):
                            sch.mm(psf[pi], Win[:, kc, 768 + j * 128:768 + (j + 1) * 128], hT[:, kc, :], kc == 0, kc == 7,
                                   [t_W, t_h], [t_ps[pi]])
                        sch.act(ckvn[:, j, :], psf[pi], AF.Copy, [t_ps[pi], t_const], [t_ckvn], scale=gvec(l, "kv", j))
                        sch.act(sqb[:, j % 2, :], psf[pi], AF.Square, [t_ps[pi]], [t_sq[j % 2]])
                        sch.mm(psf[2], ones_b, sqb[:, j % 2, :], j == 0, j == 1, [t_sq[j % 2], t_const], [t_ps[2]])
                    for t in range(4):
                        for j in range(2):
                            sch.mm(psf[3][:, 2 * t:2 * t + 2], sqb[:, j, t * 128:(t + 1) * 128], ones_b[:, 0:2], j == 0, j == 1,
                                   [t_sq[j], t_const], [t_ps[3]])
                    sch.act(rkv_b, psf[2], AF.Sqrt, [t_ps[2]], [t_rkv], scale=1.0 / 256, bias=EPS)
                    sch.op("dve", lambda e: e.reciprocal(out=rkv_b, in_=rkv_b), [t_rkv], [t_rkv])
                    sch.act(rtok[:, 0:4], psf[3][:, 0:8].rearrange("p (t two) -> p t two", two=2)[:, :, 0], AF.Sqrt, [t_ps[3]], [t_rtok], scale=1.0 / 256, bias=EPS)
                    sch.op("dve", lambda e: e.reciprocal(out=rtok[:, 0:4], in_=rtok[:, 0:4]), [t_rtok], [t_rtok])
                    for kc in range(8):
                        sch.mm(psf[0][0:32, :], Win[:, kc, 1024:1056], hT[:, kc, :], kc == 0, kc == 7, [t_W, t_h], [t_ps[0]])
                    rope(32, 0, 1, tabC, tabS, t_tab, P32, xb, t_xb, t1, t_t1, t2, t_t2, KR3[0:32, cols], t_KR[c])
                    sch.copy("pool", KR3[32:64, cols], KR3[0:32, cols], [t_KR[c]], [t_KR[c]])
                    sch.copy("pool", KR3[64:96, cols], KR3[0:32, cols], [t_KR[c]], [t_KR[c]])
                    for i in range(4):
                        pi = i % 2
                        for kc in range(6):
                            sch.mm(psf[pi], WuqN[:, kc, i * 128:(i + 1) * 128], cqn[:, kc, :], kc == 0, kc == 5,
                                   [t_W, t_cqn], [t_ps[pi]])
                        sch.tt("dve", QN[0:64, 2 * i, :], psf[pi][0:64, :], rq_b[0:64, :], ALU.mult, [t_ps[pi], t_rq], [t_QN])
                        sch.tt("dve", QN[64:128, 2 * i + 1, :], psf[pi][64:128, :], rq_b[64:128, :], ALU.mult, [t_ps[pi], t_rq], [t_QN])
                    for g in range(3):
                        nh = 3 if g < 2 else 2
                        rows = nh * 32
                        for kc in range(6):
                            sch.mm(psf[0][0:rows, :], WuqR[:, kc, g * 96:g * 96 + rows], cqn[:, kc, :], kc == 0, kc == 5,
                                   [t_W, t_cqn], [t_ps[0]])
                        rope(rows, 0, 1, tabC, tabS, t_tab, P32, xb, t_xb, t1, t_t1, t2, t_t2,
                             [(QR[32 * k:32 * k + 32, 3 * g + k, :], 32 * k) for k in range(nh)], t_QR,
                             rstd=rq_b, t_rstd=t_rq)
                    for i in range(4):
                        pi = i % 2
                        for kc in range(2):
                            sch.mm(psf[pi], WukvK[:, kc, i * 128:(i + 1) * 128], ckvn[:, kc, :], kc == 0, kc == 1,
                                   [t_W, t_ckvn], [t_ps[pi]])
                        sch.tt("dve", KN[:, i, cols], psf[pi], rkv_b, ALU.mult, [t_ps[pi], t_rkv], [t_KN[c]])
                    for t in range(4):
                        pi = t % 2
                        for kc in range(2):
                            sch.mm(psf[pi], ckvn[:, kc, t * 128:(t + 1) * 128], WukvV[:, kc, :], kc == 0, kc == 1,
                                   [t_W, t_ckvn], [t_ps[pi]])
                        sch.ts("dve", VC[:, c * 4 + t, :, 0:64], psf[pi].rearrange("p (h d) -> p h d", h=8),
                               rtok[:, t:t + 1], None, ALU.mult, None, [t_ps[pi], t_rtok], [t_VC[c]])
                    nkb = 4 * (c + 1)
                    steps = [(h, kb) for h in range(8) for kb in range(nkb)]

                    def mla_qk(si):
                        h, kb = steps[si]
                        i, off = h // 2, (h % 2) * 64
                        g, goff = h // 3, (h % 3) * 32
                        pi = 2 + (si % 2)
                        kc_ = kb // 4
                        kcols = slice(kb * 128, (kb + 1) * 128)
                        diag = kb >= 4 * c
                        sch.mm(psf[pi], KN[:, i, kcols], QN[:, h, :], True, False,
                               [t_KN[kc_], t_QN], [t_ps[pi]])
                        sch.mm(psf[pi], KR3[0:96, kcols], QR[0:96, h, :], False, not diag,
                               [t_KR[kc_], t_QR], [t_ps[pi]])
                        if diag:
                            sch.mm(psf[pi], ident_b, cmask[:, kb - 4 * c, :], False, True, [t_cm, t_const], [t_ps[pi]])
                        pt = si % 3
                        sch.act(PT[pt], psf[pi], AF.Exp, [t_ps[pi]], [t_PT[pt]], scale=MLA_SCALE)

                    def mla_pv(si):
                        h, kb = steps[si]
                        po = 4 + (h % 2)
                        pt = si % 3
                        sch.mm(psf[po][0:65, :], VC[:, kb, h, 0:65], PT[pt], kb == 0, kb == nkb - 1,
                               [t_VC[kb // 4], t_PT[pt]], [t_ps[po]])

                    def mla_norm_a(h):
                        po = 4 + (h % 2)
                        sch.op("dve", lambda e, po=po: e.reciprocal(out=rec[64:65, :], in_=psf[po][64:65, :]),
                               [t_ps[po]], [t_rec])

                    def mla_norm_b(h):
                        i, off = h // 2, (h % 2) * 64
                        po = 4 + (h % 2)
                        sch.mm(psf[6][0:64, :], ones_f[64:65, 0:64], rec[64:65, :], True, True, [t_rec, t_const], [t_ps[6]])
                        sch.copy("act", pbs[0:64, :], psf[6][0:64, :], [t_ps[6]], [t_pbs])
                        sch.tt("dve", OA[off:off + 64, i, cols], psf[po][0:64, :], pbs[0:64, :], ALU.mult,
                               [t_ps[po], t_pbs], [t_OA[c]])

                    mla_qk(0)
                    pend = None
                    for si in range(len(steps)):
                        if si + 1 < len(steps):
                            mla_qk(si + 1)
                        mla_pv(si)
                        if pend is not None:
                            pend[1] -= 1
                            if pend[1] == 0:
                                mla_norm_b(pend[0])
                                pend = None
                        h, kb = steps[si]
                        if kb == nkb - 1:
                            if pend is not None:
                                mla_norm_b(pend[0])
                            mla_norm_a(h)
                            pend = [h, 2]
                    if pend is not None:
                        mla_norm_b(pend[0])

                if dbg == "p1":
                    break
                sch.barrier()
                A = Carver(PHASE_BASE)
                Vbf = alloc(A, BF16, [128, 256])
                Vz = alloc(A, BF16, [128, 256])
                Gs = alloc(A, F32, [128, 256])
                Ktok = alloc(A, BF16, [128, 256])
                AT = alloc(A, BF16, [128, 4, 128])
                rsb = alloc(A, F32, [128, 256])
                oct_ = alloc(A, BF16, [128, 256])
                bst = alloc(A, F32, [128, 4, 6])
                bag = alloc(A, F32, [128, 4, 2])
                Win = alloc(A, BF16, [128, 8, 1704])
                Wout = alloc(A, BF16, [128, 8, D])
                KB2 = alloc(A, BF16, [128, S])
                KI3 = alloc(A, BF16, [128, S])
                VB = alloc(A, BF16, [128, NT, 65])
                hT = alloc(A, BF16, [128, 8, CH])
                rb = alloc(A, F32, [128, CH])
                tC32 = alloc(A, BF16, [128, CH])
                tS32 = alloc(A, BF16, [128, CH])
                tC64 = alloc(A, BF16, [128, CH])
                tS64 = alloc(A, BF16, [128, CH])
                xb = alloc(A, BF16, [128, CH])
                t1 = alloc(A, BF16, [128, CH])
                t2 = alloc(A, BF16, [128, CH])
                QB = alloc(A, BF16, [128, 2, CH])
                QI = alloc(A, BF16, [128, 3, CH])
                WI = alloc(A, F32, [128, 4, 8])
                RQ = alloc(A, BF16, [128, 2, CH])
                RQX = alloc(A, BF16, [128, 2, CH])
                RK = alloc(A, BF16, [128, 2, CH])
                xiT = alloc(A, F32, [128, 2, 128])
                decT = alloc(A, F32, [128, 4, 128])
                zetab = alloc(A, F32, [128, 256])
                cdb = alloc(A, F32, [128, 256])
                cb = alloc(A, F32, [128, 128])
                ctab = alloc(A, F32, [128, NIT + 1])
                gret = gp[:, l, 24:280]
                OT = alloc(A, BF16, [128, 4, CH])
                Ibuf = alloc(A, F32, [128, S])
                Rtmp2 = alloc(A, BF16, [128, 2, CH])
                Rtmp = [Rtmp2[:, 0, :], Rtmp2[:, 1, :]]
                sqb = Rtmp2
                Mb = alloc(A, BF16, [128, S])
                junk = Mb
                MbT = [alloc(A, BF16, [128, NT, 128]) for _ in range(2)]
                PTd = [alloc(A, BF16, [128, 4, 128]) for _ in range(2)]
                smA = alloc(A, F32, [128, 2])
                smR = alloc(A, F32, [128, 4])
                OBt = alloc(A, BF16, [128, 256])
                sm = alloc(A, F32, [128, 64])
                wtab = alloc(A, F32, [128, NIT + 1])
                Rst = alloc(A, F32, [128, 256])
                Rbf = alloc(A, BF16, [128, 256])
                t_W, t_Wo, t_cst = Tok(), Tok(), Tok()
                t_KB = [Tok() for _ in range(NCH)]
                t_KI = [Tok() for _ in range(NCH)]
                t_VB = [Tok() for _ in range(NCH)]
                t_h, t_rb = Tok(), Tok()
                t_tab, t_xb, t_t1, t_t2 = Tok(), Tok(), Tok(), Tok()
                t_QB, t_QI, t_WI, t_RQ, t_RQX, t_RK, t_OT = Tok(), Tok(), Tok(), Tok(), Tok(), Tok(), Tok()
                t_I, t_junk, t_Mb, t_OBt, t_sm, t_wtab = Tok(), Tok(), Tok(), Tok(), Tok(), Tok()
                t_MbT = [Tok(), Tok()]
                t_smA, t_smR = Tok(), Tok()
                t_Rtmp = [Tok(), Tok()]
                t_sq = t_Rtmp
                t_junk = t_Mb
                t_PTd = [Tok(), Tok()]
                t_Rst, t_Rbf, t_Vbf, t_Vz, t_Gs, t_Ktok, t_AT, t_rsb, t_oct, t_bst = (Tok() for _ in range(10))

                for kc in range(8):
                    sch.dma("pool", Win[:, kc, :], dr["w_in"][l, kc * 128:(kc + 1) * 128, 1056:2760], (), [t_W])
                for kc in range(8):
                    sch.dma("pool", Wout[:, kc, :], dr["w_out"][l, kc * 128:(kc + 1) * 128, :], (), [t_Wo])
                sch.dma("sp", xiT, dr["xiT"][:, :, 0:128], (), [t_cst])
                sch.dma("sp", decT, dr["decT"], (), [t_cst])
                sch.dma("sp", zetab, dr["zetab"], (), [t_cst])
                sch.dma("sp", cdb[0:64, :], dr["cdb"], (), [t_cst])
                sch.dma("sp", cb, dr["cb"], (), [t_cst])
                sch.dma("sp", ctab, dr["ctab"], (), [t_cst])
                sch.memset("pool", VB[:, :, 64:65], 1.0, [t_VB[c] for c in range(NCH)])
                sch.memset("pool", Rst[:, :], 0.0, [t_Rst])
                sch.memset("pool", Rbf[:, :], 0.0, [t_Rbf])

                def wc(a, b):
                    return slice(a - 1056, b - 1056)

                for c in range(NCH):
                    cols = slice(c * CH, (c + 1) * CH)
                    sch.dma("pool", tC32, dr["C32"][:, cols], (), [t_tab])
                    sch.dma("pool", tS32, dr["S32"][:, cols], (), [t_tab])
                    sch.dma("pool", tC64, dr["C64"][:, cols], (), [t_tab])
                    sch.dma("pool", tS64, dr["S64"][:, cols], (), [t_tab])
                    chunk_norm(c, lambda kc: gvec(l, "mix", kc), hT, t_h, sqb, t_sq, rb, t_rb, 0)

                    def proj(pi, c0, c1, rows):
                        for kc in range(8):
                            sch.mm(psf[pi][0:rows, :], Win[:, kc, wc(c0, c1)], hT[:, kc, :], kc == 0, kc == 7,
                                   [t_W, t_h], [t_ps[pi]])
                    if stop == "setup":
                        break
                    for i in range(2):
                        proj(0, 1056 + 128 * i, 1056 + 128 * (i + 1), 128)
                        rope(128, 0, 1, tC64, tS64, t_tab, P64, xb, t_xb, t1, t_t1, t2, t_t2, QB[:, i, :], t_QB)
                    proj(0, 1312, 1376, 64)
                    rope(64, 0, 1, tC64, tS64, t_tab, P64, xb, t_xb, t1, t_t1, t2, t_t2, KB2[0:64, cols], t_KB[c])
                    sch.copy("pool", KB2[64:128, cols], KB2[0:64, cols], [t_KB[c]], [t_KB[c]])
                    for g in range(3):
                        nh = 3 if g < 2 else 2
                        proj(0, 1440 + 96 * g, 1440 + 96 * g + 32 * nh, 32 * nh)
                        rope(32 * nh, 0, 1, tC32, tS32, t_tab, P32, xb, t_xb, t1, t_t1, t2, t_t2, QI[0:32 * nh, g, :], t_QI)
                    proj(0, 1696, 1728, 32)
                    rope(32, 0, 1, tC32, tS32, t_tab, P32, xb, t_xb, t1, t_t1, t2, t_t2, KI3[0:32, cols], t_KI[c])
                    sch.copy("pool", KI3[32:64, cols], KI3[0:32, cols], [t_KI[c]], [t_KI[c]])
                    sch.copy("pool", KI3[64:96, cols], KI3[0:32, cols], [t_KI[c]], [t_KI[c]])
                    if stop == "proj1":
                        break
                    for i in range(2):
                        proj(0, 1736 + 128 * i, 1736 + 128 * (i + 1), 128)
                        rope(128, 0, 1, tC64, tS64, t_tab, P64, xb, t_xb, t1, t_t1, t2, t_t2, RQ[:, i, :], t_RQ)
                        proj(0, 1992 + 128 * i, 1992 + 128 * (i + 1), 128)
                        rope(128, 0, 1, tC64, tS64, t_tab, P64, xb, t_xb, t1, t_t1, t2, t_t2, RK[:, i, :], t_RK)
                    if stop == "proj2":
                        break
                    for t in range(4):
                        tcols = slice(t * 128, (t + 1) * 128)
                        for kc in range(8):
                            sch.mm(psf[2][:, 0:64], hT[:, kc, tcols], Win[:, kc, wc(1376, 1440)], kc == 0, kc == 7,
                                   [t_W, t_h], [t_ps[2]])
                        for kc in range(8):
                            sch.mm(psf[2][:, 64:128], hT[:, kc, tcols], Win[:, kc, wc(1728, 1792)], kc == 0, kc == 7,
                                   [t_W, t_h], [t_ps[2]])
                        sch.copy("act", VB[:, c * 4 + t, 0:64], psf[2][:, 0:64], [t_ps[2]], [t_VB[c]])
                        sch.act(WI[:, t, :], psf[2][:, 64:72], AF.Copy, [t_ps[2]], [t_WI], scale=IDX_SCALE)

                    if stop == "proj":
                        break
                    def dsa_idx(j):
                        jt = c * 4 + j
                        qcols = slice(j * 128, (j + 1) * 128)
                        W = 128 * (jt + 1)
                        ngrp = (W + 511) // 512
                        n = 0
                        for h in range(8):
                            g, goff = h // 3, (h % 3) * 32
                            for kg in range(ngrp):
                                k0, k1 = kg * 512, min(W, kg * 512 + 512)
                                pi = 2 + n % 2
                                rt = n % 2
                                n += 1
                                sch.mm(psf[pi][:, 0:k1 - k0], QI[goff:goff + 32, g, qcols], KI3[goff:goff + 32, k0:k1], True, True,
                                       [t_QI] + [t_KI[k0 // 512]], [t_ps[pi]])
                                sch.act(Rtmp[rt][:, 0:k1 - k0], psf[pi][:, 0:k1 - k0], AF.Relu, [t_ps[pi]], [t_Rtmp[rt]])
                                if h == 0:
                                    sch.ts("dve", Ibuf[:, k0:k1], Rtmp[rt][:, 0:k1 - k0], WI[:, j, 0:1], None, ALU.mult, None,
                                           [t_Rtmp[rt], t_WI], [t_I])
                                else:
                                    sch.stt(Ibuf[:, k0:k1], Rtmp[rt][:, 0:k1 - k0], WI[:, j, h:h + 1], Ibuf[:, k0:k1],
                                            ALU.mult, ALU.add, [t_Rtmp[rt], t_WI, t_I], [t_I])

                    def dsa_bis(j):
                        jt = c * 4 + j
                        W = 128 * (jt + 1)
                        sch.op("dve", lambda e, W=W: e.tensor_reduce(out=sm[:, 0:1], in_=Ibuf[:, 0:W], axis=AX.X, op=ALU.max,
                                                                      apply_absolute_value=True), [t_I], [t_sm])
                        sch.ts("dve", sm[:, 1:2], sm[:, 0:1], 2.0, 2.0, ALU.mult, ALU.add, [t_sm], [t_sm])
                        sch.ts("dve", wtab[:, :], ctab[:, :], sm[:, 1:2], None, ALU.mult, None, [t_sm, t_cst], [t_wtab])
                        sch.tt("dve", Ibuf[:, W - 128:W], Ibuf[:, W - 128:W], cb[:, :], ALU.add, [t_I, t_cst], [t_I])
                        sch.memset("dve", sm[:, 2:3], 0.0, [t_sm])
                        for it in range(NIT):
                            sch.ts("dve", junk[:, 0:W], Ibuf[:, 0:W], sm[:, 2:3], 0.0, ALU.is_ge, ALU.add, [t_I, t_sm], [t_junk, t_sm],
                                   accum=sm[:, 3:4])
                            sch.ts("dve", sm[:, 4:5], sm[:, 3:4], 255.5, 0.5, ALU.is_ge, ALU.subtract, [t_sm], [t_sm])
                            sch.stt(sm[:, 2:3], sm[:, 4:5], wtab[:, it:it + 1], sm[:, 2:3], ALU.mult, ALU.add, [t_sm, t_wtab], [t_sm])
                        sch.tt("dve", sm[:, 5:6], sm[:, 2:3], wtab[:, NIT:NIT + 1], ALU.subtract, [t_sm, t_wtab], [t_sm])
                        sch.ts("dve", Mb[:, 0:W], Ibuf[:, 0:W], sm[:, 5:6], NEG, ALU.is_lt, ALU.mult, [t_I, t_sm], [t_Mb])

                    def dsa_tr(j):
                        jt = c * 4 + j
                        mb = jt % 2
                        for kb0 in range(0, jt + 1, 8):
                            nb = min(8, jt + 1 - kb0)
                            for ii in range(nb):
                                kb = kb0 + ii
                                sch.tr(psb[:, ii * 128:(ii + 1) * 128], Mb[:, kb * 128:(kb + 1) * 128], ident_b,
                                       [t_Mb, t_const], [t_psb])
                            sch.copy("act", MbT[mb][:, kb0:kb0 + nb, :], psb[:, 0:nb * 128].rearrange("p (a b) -> p a b", a=nb),
                                     [t_psb], [t_MbT[mb]])

                    def dsa_attn(j):
                        jt = c * 4 + j
                        mb = jt % 2
                        qcols = slice(j * 128, (j + 1) * 128)
                        steps = [(h, kb0) for h in range(4) for kb0 in range(0, jt + 1, 4)]

                        def qk(si):
                            h, kb0 = steps[si]
                            i, off = h // 2, (h % 2) * 64
                            nb = min(4, jt + 1 - kb0)
                            pi = si % 2
                            for ii in range(nb):
                                kb = kb0 + ii
                                sch.mm(psf[pi][:, ii * 128:(ii + 1) * 128], KB2[off:off + 64, kb * 128:(kb + 1) * 128],
                                       QB[off:off + 64, i, qcols], True, False, [t_KB[kb // 4], t_QB], [t_ps[pi]])
                                sch.mm(psf[pi][:, ii * 128:(ii + 1) * 128], ident_b, MbT[mb][:, kb, :], False, True,
                                       [t_MbT[mb], t_const], [t_ps[pi]])
                            sch.act(PTd[pi][:, 0:nb, :], psf[pi][:, 0:nb * 128].rearrange("p (a b) -> p a b", a=nb), AF.Exp,
                                    [t_ps[pi]], [t_PTd[pi]], scale=DSA_SCALE)

                        def pv(si):
                            h, kb0 = steps[si]
                            nb = min(4, jt + 1 - kb0)
                            pi = si % 2
                            po = 4 + (h % 2)
                            for ii in range(nb):
                                kb = kb0 + ii
                                sch.mm(psf[po][:, 0:65], PTd[pi][:, ii, :], VB[:, kb, 0:65], kb == 0, kb == jt,
                                       [t_PTd[pi], t_VB[kb // 4]], [t_ps[po]])
                            if kb0 + nb == jt + 1:
                                sch.op("dve", lambda e, po=po: e.reciprocal(out=smA[:, 0:1], in_=psf[po][:, 64:65]), [t_ps[po]], [t_smA])
                                sch.ts("dve", OBt[:, h * 64:(h + 1) * 64], psf[po][:, 0:64], smA[:, 0:1], None, ALU.mult, None,
                                       [t_ps[po], t_smA], [t_OBt])

                        qk(0)
                        for si in range(len(steps)):
                            if si + 1 < len(steps):
                                qk(si + 1)
                            pv(si)
                        for i in range(2):
                            sch.tr(psb[:, i * 128:(i + 1) * 128], OBt[:, i * 128:(i + 1) * 128], ident_b, [t_OBt, t_const], [t_psb])
                        sch.copy("act", OT[:, 0:2, qcols], psb[:, 0:256].rearrange("p (a b) -> p a b", a=2), [t_psb], [t_OT])

                    def ret_tile(j):
                        qcols = slice(j * 128, (j + 1) * 128)
                        for i in range(2):
                            sch.tt("pool", RQX[:, i, qcols], RQ[:, i, qcols], xiT[:, i, :], ALU.mult, [t_RQ, t_cst], [t_RQX])
                        for kc in range(8):
                            sch.mm(psf[6], hT[:, kc, qcols], Win[:, kc, wc(2248, 2760)], kc == 0, kc == 7, [t_W, t_h], [t_ps[6]])
                        sch.copy("act", Vbf[:, :], psf[6][:, 0:256], [t_ps[6]], [t_Vbf])
                        sch.tt("pool", Vz[:, :], Vbf[:, :], zetab[:, :], ALU.mult, [t_Vbf, t_cst], [t_Vz])
                        sch.act(Gs[:, :], psf[6][:, 256:512], AF.Exp, [t_ps[6]], [t_Gs], scale=-1.0)
                        sch.ts("pool", Gs[:, :], Gs[:, :], 1.0, None, ALU.add, None, [t_Gs], [t_Gs])
                        sch.op("dve", lambda e: e.reciprocal(out=Gs[:, :], in_=Gs[:, :]), [t_Gs], [t_Gs])
                        sch.tt("dve", Gs[:, :], Gs[:, :], psf[6][:, 256:512], ALU.mult, [t_Gs, t_ps[6]], [t_Gs])
                        for i in range(2):
                            sch.tr(psb[:, i * 128:(i + 1) * 128], RK[:, i, qcols], ident_b, [t_RK, t_const], [t_psb])
                        sch.copy("act", Ktok[:, :], psb[:, 0:256], [t_psb], [t_Ktok])
                        for h in range(4):
                            i, off = h // 2, (h % 2) * 64
                            sch.mm(psf[2 + h % 2][:, (h // 2) * 128:(h // 2 + 1) * 128], RK[off:off + 64, i, qcols],
                                   RQ[off:off + 64, i, qcols], True, True, [t_RK, t_RQ], [t_ps[2 + h % 2]])
                        for par in range(2):
                            sch.tt("dve", AT[:, 2 * par:2 * par + 2, :], psf[2 + par][:, 0:256].rearrange("p (a b) -> p a b", a=2),
                                   decT[:, 2 * par:2 * par + 2, :], ALU.mult, [t_ps[2 + par], t_cst], [t_AT])
                        for h in range(4):
                            i, off = h // 2, (h % 2) * 64
                            sch.mm(psf[3][:, h * 64:(h + 1) * 64], AT[:, (h % 2) * 2 + h // 2, :], Vbf[:, h * 64:(h + 1) * 64], True, False,
                                   [t_AT, t_Vbf], [t_ps[3]])
                            sch.mm(psf[3][:, h * 64:(h + 1) * 64], RQX[off:off + 64, i, qcols], Rbf[off:off + 64, h * 64:(h + 1) * 64],
                                   False, True, [t_RQX, t_Rbf], [t_ps[3]])
                        for h in range(4):
                            sch.mm(psf[6][0:64, h * 64:(h + 1) * 64], Ktok[:, h * 64:(h + 1) * 64], Vz[:, h * 64:(h + 1) * 64], True, True,
                                   [t_Ktok, t_Vz], [t_ps[6]])
                        sch.tt("pool", Rst[0:64, :], Rst[0:64, :], cdb[0:64, :], ALU.mult, [t_Rst, t_cst], [t_Rst])
                        sch.tt("dve", Rst[0:64, :], Rst[0:64, :], psf[6][0:64, 0:256], ALU.add, [t_Rst, t_ps[6]], [t_Rst])
                        sch.copy("act", Rbf[0:64, :], Rst[0:64, :], [t_Rst], [t_Rbf])
                        sch.copy("act", Rbf[64:128, :], Rst[0:64, :], [t_Rst], [t_Rbf])
                        sch.copy("act", rsb[:, :], psf[3][:, 0:256], [t_ps[3]], [t_rsb])
                        for h in range(4):
                            sch.op("dve", lambda e, h=h: e.bn_stats(out=bst[:, h, :], in_=rsb[:, h * 64:(h + 1) * 64]), [t_rsb], [t_bst])
                            sch.op("dve", lambda e, h=h: e.bn_aggr(out=bag[:, h, :], in_=bst[:, h, :]), [t_bst], [t_bst])
                        sch.act(smR[:, 0:4], bag[:, :, 1], AF.Sqrt, [t_bst], [t_smR], bias=EPS)
                        sch.op("dve", lambda e: e.reciprocal(out=smR[:, 0:4], in_=smR[:, 0:4]), [t_smR], [t_smR])
                        for h in range(4):
                            sch.ts("dve", rsb[:, h * 64:(h + 1) * 64], rsb[:, h * 64:(h + 1) * 64], bag[:, h, 0:1], smR[:, h:h + 1],
                                   ALU.subtract, ALU.mult, [t_rsb, t_bst, t_smR], [t_rsb])
                        sch.tt("pool", rsb[:, :], rsb[:, :], gret, ALU.mult, [t_rsb, t_const], [t_rsb])
                        sch.tt("pool", oct_[:, :], rsb[:, :], Gs[:, :], ALU.mult, [t_rsb, t_Gs], [t_oct])
                        for i in range(2):
                            sch.tr(psb[:, i * 128:(i + 1) * 128], oct_[:, i * 128:(i + 1) * 128], ident_b, [t_oct, t_const], [t_psb])
                        sch.copy("act", OT[:, 2:4, qcols], psb[:, 0:256].rearrange("p (a b) -> p a b", a=2), [t_psb], [t_OT])

                    prev = None
                    for j in range(4):
                        dsa_idx(j)
                        dsa_bis(j)
                        if prev is not None:
                            dsa_attn(prev)
                        ret_tile(j)
                        dsa_tr(j)
                        prev = j
                    dsa_attn(prev)

                    for oc in range(8):
                        pi = oc % 2
                        for mc in range(8):
                            if mc < 4:
                                sch.mm(psf[pi], Wout[:, mc, oc * 128:(oc + 1) * 128], OA[:, mc, cols], mc == 0, False,
                                       [t_Wo, t_OA[c]], [t_ps[pi]])
                            else:
                                sch.mm(psf[pi], Wout[:, mc, oc * 128:(oc + 1) * 128], OT[:, mc - 4, :], False, mc == 7,
                                       [t_Wo, t_OT], [t_ps[pi]])
                        sch.tt("dve", xT[:, oc, cols], xT[:, oc, cols], psf[pi], ALU.add, [t_xT[c], t_ps[pi]], [t_xT[c]])

                if dbg == "p2":
                    break
                sch.barrier()
                A = Carver(PHASE_BASE)
                hfT = alloc(A, BF16, [128, 8, S])
                W1 = [alloc(A, BF16, [128, 8, 1024]) for _ in range(2)]
                W2 = [alloc(A, BF16, [128, 8, D]) for _ in range(2)]
                hid = [alloc(A, BF16, [128, 8, CH]) for _ in range(2)]
                rl = [alloc(A, BF16, [128, CH]) for _ in range(2)]
                sqb = alloc(A, BF16, [128, 2, CH])
                rb = alloc(A, F32, [128, CH])
                t_hf = [Tok() for _ in range(NCH)]
                t_W1 = [Tok(), Tok()]
                t_W2 = [Tok(), Tok()]
                t_hid = [Tok(), Tok()]
                t_rl = [Tok(), Tok()]
                t_sq = [Tok(), Tok()]
                t_rb = Tok()

                def load_q(qr):
                    b = qr % 2
                    for kc in range(8):
                        sch.dma("pool", W1[b][:, kc, :], dr["w_ff1"][l, kc * 128:(kc + 1) * 128, qr * 1024:(qr + 1) * 1024], (), [t_W1[b]])
                    for fc in range(8):
                        sch.dma("pool", W2[b][:, fc, :], dr["w_ff2"][l, qr * 1024 + fc * 128: qr * 1024 + (fc + 1) * 128, :], (), [t_W2[b]])

                load_q(0)
                for c in range(NCH):
                    chunk_norm(c, lambda kc: gvec(l, "mlp", kc), hfT[:, :, c * CH:(c + 1) * CH], t_hf[c], sqb, t_sq, rb, t_rb, 0)
                for qr in range(4):
                    b = qr % 2
                    if qr + 1 < 4:
                        load_q(qr + 1)
                    for c in range(NCH):
                        cols = slice(c * CH, (c + 1) * CH)
                        hb = c % 2
                        for fc in range(8):
                            pi = fc % 2
                            for kc in range(8):
                                sch.mm(psf[pi], W1[b][:, kc, fc * 128:(fc + 1) * 128], hfT[:, kc, cols], kc == 0, kc == 7,
                                       [t_W1[b], t_hf[c]], [t_ps[pi]])
                            sch.act(rl[pi], psf[pi], AF.Relu, [t_ps[pi]], [t_rl[pi]])
                            sch.tt("pool", hid[hb][:, fc, :], rl[pi], rl[pi], ALU.mult, [t_rl[pi]], [t_hid[hb]])
                        for oc in range(8):
                            pi = 2 + oc % 2
                            for fc in range(8):
                                sch.mm(psf[pi], W2[b][:, fc, oc * 128:(oc + 1) * 128], hid[hb][:, fc, :], fc == 0, fc == 7,
                                       [t_W2[b], t_hid[hb]], [t_ps[pi]])
                            sch.tt("dve", xT[:, oc, cols], xT[:, oc, cols], psf[pi], ALU.add, [t_xT[c], t_ps[pi]], [t_xT[c]])

            sch.barrier()
            Lc = Carver(PHASE_BASE)
            yT = alloc(Lc, F32, [128, 8, CH])
            yo = [alloc(Lc, F32, [128, D]) for _ in range(2)]
            sqb = alloc(Lc, BF16, [128, 2, CH])
            rb = alloc(Lc, F32, [128, CH])
            t_y, t_rb = Tok(), Tok()
            t_sq = [Tok(), Tok()]
            t_yo = [Tok(), Tok()]
            for c in range(NCH):
                cols = slice(c * CH, (c + 1) * CH)
                if dbg:
                    src = None
                if final_norm:
                    for kc in range(8):
                        sch.act(sqb[:, kc % 2, :], xT[:, kc, cols], AF.Square, [t_xT[c]], [t_sq[kc % 2]])
                        sch.mm(psf[0], ones_b, sqb[:, kc % 2, :], kc == 0, kc == 7, [t_sq[kc % 2], t_const], [t_ps[0]])
                    sch.act(rb, psf[0], AF.Sqrt, [t_ps[0]], [t_rb], scale=1.0 / D, bias=EPS)
                    sch.op("dve", lambda e: e.reciprocal(out=rb, in_=rb), [t_rb], [t_rb])
                    for kc in range(8):
                        sch.stt(yT[:, kc, :], xT[:, kc, cols], gfin[:, kc:kc + 1], rb, ALU.mult, ALU.mult,
                                [t_xT[c], t_rb, t_const], [t_y])
                    srcT = lambda kc, t: yT[:, kc, t * 128:(t + 1) * 128]
                    t_src = t_y
                else:
                    srcT = lambda kc, t, c=c: xT[:, kc, c * CH + t * 128: c * CH + (t + 1) * 128]
                    t_src = t_xT[c]
                for t in range(4):
                    tt_ = c * 4 + t
                    b = tt_ % 2
                    for half in range(2):
                        pi = 1 + half
                        for k4 in range(4):
                            kc = half * 4 + k4
                            sch.tr(psf[pi][:, k4 * 128:(k4 + 1) * 128], srcT(kc, t), ident_f, [t_src, t_const], [t_ps[pi]])
                        sch.copy("act" if half == 0 else "dve", yo[b][:, half * 512:(half + 1) * 512], psf[pi], [t_ps[pi]], [t_yo[b]])
                    sch.dma("sp", dr["y"][sq_i * S + tt_ * 128: sq_i * S + (tt_ + 1) * 128, :], yo[b], [t_yo[b]], [t_yo[b]])

        if dbg:
            sch.barrier()
            if dbg == "p1":
                Lc = Carver(PHASE_BASE)
                tmp = alloc(Lc, F32, [128, 4, S])
                tk = Tok()
                for i in range(4):
                    sch.copy("dve", tmp[:, i, :], OA[:, i, :], [t_OA[c] for c in range(NCH)], [tk])
                sch.dma("sp", dr["dbg"][:, 0:4, :], tmp, [tk], [tk])
            else:
                sch.dma("sp", dr["dbg"], xT, [t_xT[c] for c in range(NCH)], [Tok()])
        final_waits = [(k, sch.dma_cnt[k]) for k in range(sch.n_dma_sems) if sch.dma_cnt[k] > 0]
        sch.emit(nc, block, sems, dsems, final_waits)
    return nc


NCORES = 8
_CACHE = {}


def _gpack(g_mix, g_q, g_kv, g_mlp, g_ret):
    L = g_mix.shape[0]
    out = np.zeros((L, 128, NG), np.float32)
    for l in range(L):
        out[l, :, 0:8] = np.asarray(g_mix[l], np.float32).reshape(8, 128).T
        out[l, :, 8:14] = np.asarray(g_q[l], np.float32).reshape(6, 128).T
        out[l, :, 14:16] = np.asarray(g_kv[l], np.float32).reshape(2, 128).T
        out[l, :, 16:24] = np.asarray(g_mlp[l], np.float32).reshape(8, 128).T
        out[l, :, 24:280] = np.broadcast_to(np.asarray(g_ret[l], np.float32)[None, :], (128, 256))
    return out


def run_layers(x, layers, final_norm, weights, g_final, ncores=NCORES, nseq=None, dbg=None):
    B = x.shape[0]
    nseq = B // ncores if nseq is None else nseq
    L = len(layers)
    key = (nseq, L, final_norm, dbg)
    if key not in _CACHE:
        _CACHE[key] = build_nc(nseq, L, final_norm, dbg)
    nc = _CACHE[key]
    consts = _consts()
    common = {"c_" + k: np.ascontiguousarray(v, dtype=np.float32) for k, v in consts.items()}
    for nm in ("w_in", "w_uq", "w_ukv", "w_out", "w_ff1", "w_ff2"):
        common[nm] = np.ascontiguousarray(np.asarray(weights[nm], np.float32)[layers])
    common["gpack"] = _gpack(*[np.asarray(weights[n])[layers] for n in ("g_mix", "g_q", "g_kv", "g_mlp", "g_ret")])
    common["gfin"] = np.ascontiguousarray(np.asarray(g_final, np.float32).reshape(8, 128).T)
    in_maps = []
    for ci in range(ncores):
        m = dict(common)
        m["x"] = np.ascontiguousarray(np.asarray(x[ci * nseq:(ci + 1) * nseq], np.float32).reshape(nseq * S, D))
        in_maps.append(m)
    res = run_bass_kernel_spmd(nc, in_maps, core_ids=list(range(ncores)))
    y = np.concatenate([r["y"].reshape(nseq, S, D) for r in res.results], axis=0)
    if dbg:
        return y, [r["dbg"] for r in res.results]
    return y


FUSED = True


def kernel(x, g_mix, w_in, g_q, w_uq, g_kv, w_ukv, g_ret, w_out, g_mlp, w_ff1, w_ff2, g_final):
    weights = dict(g_mix=g_mix, w_in=w_in, g_q=g_q, w_uq=w_uq, g_kv=g_kv, w_ukv=w_ukv, g_ret=g_ret, w_out=w_out,
                   g_mlp=g_mlp, w_ff1=w_ff1, w_ff2=w_ff2)
    x = np.asarray(x, np.float32)
    depth = np.asarray(w_in).shape[0]
    if FUSED:
        return run_layers(x, list(range(depth)), True, weights, g_final).astype(np.float32)
    y = x
    for l in range(depth):
        y = run_layers(y, [l], l == depth - 1, weights, g_final)
    return y.astype(np.float32)
```

```python
import numpy as np
import concourse.bass as bass
import concourse.mybir as mybir
from concourse.bass_utils import run_bass_kernel_spmd

F32 = mybir.dt.float32
BF16 = mybir.dt.bfloat16
AF = mybir.ActivationFunctionType
ALU = mybir.AluOpType
AX = mybir.AxisListType

D = 1024
S = 2048
NCH = 4
CH = 512
NT = 16
INW = 2760
DFF = 4096
EPS = 1e-6
NIT = 16
MLA_SCALE = 96 ** -0.5
DSA_SCALE = 64 ** -0.5
IDX_SCALE = (8 ** -0.5) * (32 ** -0.5)
NEG = -30000.0
NG = 8 + 6 + 2 + 8 + 256


class Tok:
    __slots__ = ("name", "lw", "rds")

    def __init__(self, name=""):
        self.name = name
        self.lw = None
        self.rds = []


class Op:
    __slots__ = ("eng", "fn", "deps", "inc", "val", "dsem", "dval", "is_dma", "ep")


class Sched:
    ENGS = ("pe", "act", "dve", "pool", "sp")

    def __init__(self, n_dma_sems=24):
        self.ops = {e: [] for e in self.ENGS}
        self.n_dma_sems = n_dma_sems
        self.dma_last = [None] * n_dma_sems
        self.dma_cnt = [0] * n_dma_sems
        self.dma_rr = 0
        self.pending = {e: [] for e in self.ENGS}
        self.epoch = 0
        self.nsets = 12

    def barrier(self):
        lasts = []
        for e in self.ENGS:
            for op in reversed(self.ops[e]):
                if not op.is_dma:
                    lasts.append(op)
                    break
        for d in self.dma_last:
            if d is not None:
                lasts.append(d)
        for e in self.ENGS:
            self.pending[e] = list(lasts)
        self.epoch += 1

    def _rec(self, eng, fn, rd, wr, is_dma=False):
        op = Op()
        op.eng = eng
        op.fn = fn
        op.inc = False
        op.val = None
        op.is_dma = is_dma
        op.ep = self.epoch % self.nsets
        op.dsem = None
        op.dval = None
        deps = list(self.pending[eng])
        self.pending[eng] = []
        for t in rd:
            if t.lw is not None:
                deps.append(t.lw)
        for t in wr:
            if t.lw is not None:
                deps.append(t.lw)
            deps.extend(t.rds)
        if is_dma:
            k = self.dma_rr % self.n_dma_sems
            self.dma_rr += 1
            if self.dma_last[k] is not None:
                deps.append(self.dma_last[k])
            self.dma_last[k] = op
            self.dma_cnt[k] += 16
            op.dsem = k
            op.dval = self.dma_cnt[k]
        fd = []
        seen = set()
        for d in deps:
            if id(d) in seen or d is op:
                continue
            seen.add(id(d))
            if (not d.is_dma) and d.eng == "pe" and eng == "pe":
                continue
            if not d.is_dma:
                d.inc = True
            fd.append(d)
        op.deps = fd
        for t in rd:
            t.rds.append(op)
        for t in wr:
            t.lw = op
            t.rds = []
        self.ops[eng].append(op)
        return op

    def op(self, eng, fn, rd=(), wr=()):
        return self._rec(eng, fn, rd, wr)

    def dma(self, eng, out, in_, rd=(), wr=()):
        return self._rec(eng, lambda e: e.dma_start(out=out, in_=in_), rd, wr, is_dma=True)

    def mm(self, out, lhsT, rhs, start, stop, rd, wr):
        return self.op("pe", lambda e: e.matmul(out, lhsT, rhs, start=start, stop=stop), rd, wr)

    def tr(self, out, in_, ident, rd, wr):
        return self.op("pe", lambda e: e.transpose(out, in_, ident), rd, wr)

    def act(self, out, in_, func, rd, wr, scale=None, bias=None, accum=None):
        kw = {}
        if scale is not None:
            kw["scale"] = scale
        if bias is not None:
            kw["bias"] = bias
        if accum is not None:
            kw["accum_out"] = accum
        return self.op("act", lambda e: e.activation(out=out, in_=in_, func=func, **kw), rd, wr)

    def tt(self, eng, out, in0, in1, op, rd, wr):
        return self.op(eng, lambda e: e.tensor_tensor(out=out, in0=in0, in1=in1, op=op), rd, wr)

    def ts(self, eng, out, in0, s1, s2, op0, op1, rd, wr, accum=None):
        if op1 is None:
            return self.op(eng, lambda e: e.tensor_scalar(out=out, in0=in0, scalar1=s1, scalar2=None, op0=op0), rd, wr)
        if accum is not None:
            return self.op(eng, lambda e: e.tensor_scalar(out=out, in0=in0, scalar1=s1, scalar2=s2, op0=op0,
                                                          op1=op1, accum_out=accum), rd, wr)
        return self.op(eng, lambda e: e.tensor_scalar(out=out, in0=in0, scalar1=s1, scalar2=s2, op0=op0, op1=op1),
                       rd, wr)

    def stt(self, out, in0, scalar, in1, op0, op1, rd, wr):
        return self.op("dve", lambda e: e.scalar_tensor_tensor(out=out, in0=in0, scalar=scalar, in1=in1,
                                                               op0=op0, op1=op1), rd, wr)

    def copy(self, eng, out, in_, rd, wr):
        if eng == "act":
            return self.op("act", lambda e: e.activation(out=out, in_=in_, func=AF.Copy), rd, wr)
        return self.op(eng, lambda e: e.tensor_copy(out=out, in_=in_), rd, wr)

    def memset(self, eng, out, val, wr):
        return self.op(eng, lambda e: e.memset(out, val), (), wr)

    def emit(self, nc, block, sems, dsems, final_waits):
        for eng in self.ENGS:
            c = [0] * self.nsets
            for op in self.ops[eng]:
                if op.inc and not op.is_dma:
                    c[op.ep] += 1
                    op.val = c[op.ep]
        engobj = {"pe": block.tensor, "act": block.scalar, "dve": block.vector, "pool": block.gpsimd,
                  "sp": block.sync}

        def make(eng):
            ops = self.ops[eng]

            def body(e):
                seen = {}
                for op in ops:
                    for d in op.deps:
                        if d.is_dma:
                            key = ("d", d.dsem)
                            sem = dsems[d.dsem]
                            val = d.dval
                        else:
                            key = ("c", d.eng, d.ep)
                            sem = sems[d.ep][d.eng]
                            val = d.val
                        if seen.get(key, 0) >= val:
                            continue
                        seen[key] = val
                        e.wait_ge(sem, val)
                    ins = op.fn(e)
                    if op.is_dma:
                        ins.then_inc(dsems[op.dsem], 16)
                    elif op.inc:
                        ins.then_inc(sems[op.ep][eng], 1)
                if eng == "sp":
                    for (k, v) in final_waits:
                        e.wait_ge(dsems[k], v)
            return body

        for eng in self.ENGS:
            engobj[eng](make(eng))


def _consts():
    c = {}
    pos = np.arange(S, dtype=np.float64)

    def tab(dim):
        inv = 10000.0 ** (-np.arange(0, dim, 2, dtype=np.float64) / dim)
        C = np.zeros((128, S), np.float32)
        Sg = np.zeros((128, S), np.float32)
        P = np.zeros((128, 128), np.float32)
        for p in range(128):
            i = p % dim
            j = i % (dim // 2)
            ang = (pos.astype(np.float32) * np.float32(inv[j])).astype(np.float32)
            C[p] = np.cos(ang)
            Sg[p] = (-np.sin(ang)) if i < dim // 2 else np.sin(ang)
            src = (p // dim) * dim + (i + dim // 2) % dim
            P[src, p] = 1.0
        return C, Sg, P
    c["C32"], c["S32"], c["P32"] = tab(32)
    c["C64"], c["S64"], c["P64"] = tab(64)
    cm = np.zeros((128, 4, 512), np.float32)
    k = np.arange(128)[:, None]
    q = np.arange(512)[None, :]
    for i in range(4):
        cm[:, i, :] = np.where(i * 128 + k > q, NEG, 0.0)
    c["cmask"] = cm
    qq = np.arange(128)[:, None]
    kk = np.arange(128)[None, :]
    c["cb"] = np.where(kk <= qq, 0.0, -1e30).astype(np.float32)
    lg = np.log1p(-np.exp2(-5.0 - np.arange(4, dtype=np.float64)))
    dec = np.zeros((128, 4, 128), np.float32)
    cc = np.arange(128)[:, None]
    q1 = np.arange(128)[None, :]
    for h in range(4):
        dec[:, (h % 2) * 2 + h // 2, :] = np.where(q1 >= cc, 0.125 * np.exp(np.maximum(q1 - cc, 0) * lg[h]), 0.0)
    c["decT"] = dec
    xi = np.zeros((128, 2, 512), np.float32)
    for i in range(2):
        for r in range(128):
            h = 2 * i + r // 64
            xi[r, i, :] = np.tile(np.exp((np.arange(128) + 1.0) * lg[h]), 4)
    c["xiT"] = xi
    zb = np.zeros((128, 256), np.float32)
    cdb = np.zeros((64, 256), np.float32)
    for h in range(4):
        zb[:, h * 64:(h + 1) * 64] = (0.125 * np.exp((127.0 - np.arange(128)) * lg[h]))[:, None]
        cdb[:, h * 64:(h + 1) * 64] = np.exp(128.0 * lg[h])
    c["zetab"] = zb
    c["cdb"] = cdb
    c["ctab"] = np.broadcast_to((2.0 ** -(np.arange(NIT + 1) + 1.0)).astype(np.float32)[None, :], (128, NIT + 1)).copy()
    c["ident"] = np.eye(128, dtype=np.float32)
    c["ones"] = np.ones((128, 128), np.float32)
    return c


CONST_SHAPES = {
    "C32": [128, S], "S32": [128, S], "P32": [128, 128], "C64": [128, S], "S64": [128, S], "P64": [128, 128],
    "cmask": [128, 4, 512], "cb": [128, 128], "decT": [128, 4, 128], "xiT": [128, 2, 512], "zetab": [128, 256],
    "cdb": [64, 256], "ctab": [128, NIT + 1], "ident": [128, 128], "ones": [128, 128],
}


def build_nc(nseq, nlayers, final_norm, dbg=None):
    from contextlib import ExitStack
    nc = bass.Bass("TRN2", target_bir_lowering=False)
    dr = {}
    dr["x"] = nc.dram_tensor("x", [nseq * S, D], F32, kind="ExternalInput").ap()
    dr["y"] = nc.dram_tensor("y", [nseq * S, D], F32, kind="ExternalOutput").ap()
    dr["w_in"] = nc.dram_tensor("w_in", [nlayers, D, INW], F32, kind="ExternalInput").ap()
    dr["w_uq"] = nc.dram_tensor("w_uq", [nlayers, 768, 768], F32, kind="ExternalInput").ap()
    dr["w_ukv"] = nc.dram_tensor("w_ukv", [nlayers, 256, 1024], F32, kind="ExternalInput").ap()
    dr["w_out"] = nc.dram_tensor("w_out", [nlayers, D, D], F32, kind="ExternalInput").ap()
    dr["w_ff1"] = nc.dram_tensor("w_ff1", [nlayers, D, DFF], F32, kind="ExternalInput").ap()
    dr["w_ff2"] = nc.dram_tensor("w_ff2", [nlayers, DFF, D], F32, kind="ExternalInput").ap()
    dr["gpack"] = nc.dram_tensor("gpack", [nlayers, 128, NG], F32, kind="ExternalInput").ap()
    dr["gfin"] = nc.dram_tensor("gfin", [128, 8], F32, kind="ExternalInput").ap()
    for k, shp in CONST_SHAPES.items():
        dr[k] = nc.dram_tensor("c_" + k, shp, F32, kind="ExternalInput").ap()
    if dbg:
        dr["dbg"] = nc.dram_tensor("dbg", [128, 8, S], F32, kind="ExternalOutput").ap()

    stop = None
    if dbg and ":" in dbg:
        dbg, stop = dbg.split(":")
    sch = Sched()
    with ExitStack() as st:
        ARW = 53000
        arena = st.enter_context(nc.sbuf_tensor("arena", [128, ARW], F32))
        psf = [st.enter_context(nc.psum_tensor("ps%d" % i, [128, 512], F32))[:, :] for i in range(7)]
        psb = st.enter_context(nc.psum_tensor("psb", [128, 1024], BF16))[:, :]
        sems = [{e: st.enter_context(nc.semaphore("sem%d_%s" % (k, e))) for e in Sched.ENGS} for k in range(sch.nsets)]
        dsems = [st.enter_context(nc.semaphore("dsem%d" % i)) for i in range(sch.n_dma_sems)]
        block = st.enter_context(nc.Block())

        class Carver:
            def __init__(self, base=0):
                self.off = base

            def take(self, nbytes):
                o = self.off
                self.off += (nbytes + 3) // 4 * 4
                assert self.off <= ARW * 4, ("SBUF arena overflow", self.off, ARW * 4)
                self.peak = max(getattr(self, "peak", 0), self.off)
                return o

        def view(off, dtype, shape):
            n = 1
            for s_ in shape[1:]:
                n *= s_
            esz = 4 if dtype == F32 else 2
            w0 = off // 4
            nw = (n * esz + 3) // 4
            ap = arena[0:shape[0], w0:w0 + nw]
            if dtype != F32:
                ap = ap.bitcast(dtype)
            if len(shape) == 3:
                ap = ap.rearrange("p (a b) -> p a b", a=shape[1])
            elif len(shape) == 4:
                ap = ap.rearrange("p (a b c) -> p a b c", a=shape[1], b=shape[2])
            return ap

        def alloc(C, dtype, shape):
            n = 1
            for s_ in shape[1:]:
                n *= s_
            return view(C.take(n * (4 if dtype == F32 else 2)), dtype, shape)

        G = Carver()
        xT = alloc(G, F32, [128, 8, S])
        OA = alloc(G, BF16, [128, 4, S])
        ident_b = alloc(G, BF16, [128, 128])
        ident_f = alloc(G, F32, [128, 128])
        ones_b = alloc(G, BF16, [128, 128])
        ones_f = alloc(G, F32, [128, 128])
        P32 = alloc(G, BF16, [128, 128])
        P64 = alloc(G, BF16, [128, 128])
        gp = alloc(G, F32, [128, nlayers, NG])
        gfin = alloc(G, F32, [128, 8])
        PHASE_BASE = G.off

        t_xT = [Tok("xT%d" % c) for c in range(NCH)]
        t_OA = [Tok("OA%d" % c) for c in range(NCH)]
        t_const = Tok("const")
        t_ps = [Tok("ps%d" % i) for i in range(7)]
        t_psb = Tok("psb")

        sch.dma("pool", ident_b, dr["ident"], (), [t_const])
        sch.dma("sp", ident_f, dr["ident"], (), [t_const])
        sch.dma("pool", ones_b, dr["ones"], (), [t_const])
        sch.dma("sp", ones_f, dr["ones"], (), [t_const])
        sch.dma("pool", P32, dr["P32"], (), [t_const])
        sch.dma("pool", P64, dr["P64"], (), [t_const])
        for l in range(nlayers):
            sch.dma("sp", gp[:, l, :], dr["gpack"][l], (), [t_const])
        sch.dma("sp", gfin, dr["gfin"], (), [t_const])

        def gvec(l, which, j):
            base = {"mix": 0, "q": 8, "kv": 14, "mlp": 16}[which]
            return gp[:, l, base + j:base + j + 1]

        def chunk_norm(c, gfun, hT_dst, t_h, sq, t_sq, rb, t_rb, pi):
            cols = slice(c * CH, (c + 1) * CH)
            for kc in range(8):
                sch.act(sq[:, kc % 2, :], xT[:, kc, cols], AF.Square, [t_xT[c]], [t_sq[kc % 2]])
                sch.mm(psf[pi], ones_b, sq[:, kc % 2, :], kc == 0, kc == 7, [t_sq[kc % 2], t_const], [t_ps[pi]])
            sch.act(rb, psf[pi], AF.Ln, [t_ps[pi]], [t_rb], scale=1.0 / D, bias=EPS)
            sch.act(rb, rb, AF.Exp, [t_rb], [t_rb], scale=-0.5)
            for kc in range(8):
                sch.stt(hT_dst[:, kc, :], xT[:, kc, cols], gfun(kc), rb, ALU.mult, ALU.mult,
                        [t_xT[c], t_rb, t_const], [t_h])

        def rope(rows, pin, pr, Ctab, Stab, t_tab, Pm, xb, t_xb, t1, t_t1, t2, t_t2, dst, t_dst,
                 rstd=None, t_rstd=None):
            if rstd is None:
                sch.copy("act", xb[0:rows, :], psf[pin][0:rows, :], [t_ps[pin]], [t_xb])
            else:
                sch.tt("dve", xb[0:rows, :], psf[pin][0:rows, :], rstd[0:rows, :], ALU.mult, [t_ps[pin], t_rstd], [t_xb])
            sch.mm(psf[pr][0:rows, :], Pm[0:rows, 0:rows], xb[0:rows, :], True, True, [t_xb, t_const], [t_ps[pr]])
            sch.tt("dve", t1[0:rows, :], xb[0:rows, :], Ctab[0:rows, :], ALU.mult, [t_xb, t_tab], [t_t1])
            sch.tt("dve", t2[0:rows, :], psf[pr][0:rows, :], Stab[0:rows, :], ALU.mult, [t_ps[pr], t_tab], [t_t2])
            if isinstance(dst, list):
                for (d_ap, r0) in dst:
                    sch.tt("dve", d_ap, t1[r0:r0 + 32, :], t2[r0:r0 + 32, :], ALU.add, [t_t1, t_t2], [t_dst])
            else:
                sch.tt("dve", dst, t1[0:rows, :], t2[0:rows, :], ALU.add, [t_t1, t_t2], [t_dst])

        def run_rope_jobs(jobs, scr):
            jobs[0]["proj"](0)
            for k, jb in enumerate(jobs):
                if k + 1 < len(jobs):
                    jobs[k + 1]["proj"]((k + 1) % 2)
                xb_, t_xb_, t1_, t_t1_, t2_, t_t2_ = scr[k % len(scr)]
                rope(jb["rows"], k % 2, 4 + k % 2, jb["C"], jb["S"], jb["t_tab"], jb["P"], xb_, t_xb_, t1_, t_t1_, t2_, t_t2_,
                     jb["dst"], jb["t_dst"], rstd=jb.get("rstd"), t_rstd=jb.get("t_rstd"))
                if jb.get("post"):
                    jb["post"]()

        for sq_i in range(nseq):
            sch.barrier()
            Lc = Carver(PHASE_BASE)
            xin = [alloc(Lc, F32, [128, D]) for _ in range(2)]
            t_xin = [Tok("xin0"), Tok("xin1")]
            for t in range(NT):
                b = t % 2
                sch.dma("sp", xin[b], dr["x"][sq_i * S + t * 128: sq_i * S + (t + 1) * 128, :], (), [t_xin[b]])
                for half in range(2):
                    for k4 in range(4):
                        kc = half * 4 + k4
                        sch.tr(psf[half][:, k4 * 128:(k4 + 1) * 128], xin[b][:, kc * 128:(kc + 1) * 128], ident_f,
                               [t_xin[b], t_const], [t_ps[half]])
                    for k4 in range(4):
                        kc = half * 4 + k4
                        sch.copy("act" if half == 0 else "dve", xT[:, kc, t * 128:(t + 1) * 128],
                                 psf[half][:, k4 * 128:(k4 + 1) * 128], [t_ps[half]], [t_xT[t // 4]])

            for l in range(nlayers):
                sch.barrier()
                A = Carver(PHASE_BASE)
                Win = alloc(A, BF16, [128, 8, 1056])
                WuqN = alloc(A, BF16, [128, 6, 512])
                WuqR = alloc(A, BF16, [128, 6, 256])
                WukvK = alloc(A, BF16, [128, 2, 512])
                WukvV = alloc(A, BF16, [128, 2, 512])
                KN = alloc(A, BF16, [128, 4, S])
                KR3 = alloc(A, BF16, [128, S])
                VC = alloc(A, BF16, [128, NT, 8, 65])
                hT = alloc(A, BF16, [128, 8, CH])
                sqb = alloc(A, BF16, [128, 2, CH])
                rb = alloc(A, F32, [128, CH])
                rq_b = alloc(A, F32, [128, CH])
                rkv_b = alloc(A, F32, [128, CH])
                cqn = alloc(A, BF16, [128, 6, CH])
                ckvn = alloc(A, BF16, [128, 2, CH])
                tabC = alloc(A, BF16, [128, CH])
                tabS = alloc(A, BF16, [128, CH])
                xb = alloc(A, BF16, [128, CH])
                t1 = alloc(A, BF16, [128, CH])
                t2 = alloc(A, BF16, [128, CH])
                xb2 = alloc(A, BF16, [128, CH])
                t12 = alloc(A, BF16, [128, CH])
                t22 = alloc(A, BF16, [128, CH])
                t_xb2, t_t12, t_t22 = Tok(), Tok(), Tok()
                QN = alloc(A, BF16, [128, 8, CH])
                QR = alloc(A, BF16, [128, 8, CH])
                PT = [alloc(A, BF16, [128, CH]) for _ in range(3)]
                cmask = alloc(A, BF16, [128, 4, CH])
                rec = rb
                pbs = alloc(A, F32, [128, CH])
                rtok = alloc(A, F32, [128, 8])
                t_W = Tok("W1")
                t_KN = [Tok() for _ in range(NCH)]
                t_KR = [Tok() for _ in range(NCH)]
                t_VC = [Tok() for _ in range(NCH)]
                t_h, t_rb, t_rq, t_rkv, t_cqn, t_ckvn = Tok(), Tok(), Tok(), Tok(), Tok(), Tok()
                t_sq = [Tok(), Tok()]
                t_tab, t_xb, t_t1, t_t2, t_QN, t_QR = Tok(), Tok(), Tok(), Tok(), Tok(), Tok()
                t_PT = [Tok(), Tok(), Tok()]
                t_cm, t_pbs, t_rtok = Tok(), Tok(), Tok()
                t_rec = t_rb

                for kc in range(8):
                    sch.dma("pool", Win[:, kc, :], dr["w_in"][l, kc * 128:(kc + 1) * 128, 0:1056], (), [t_W])
                for kc in range(6):
                    srcq = dr["w_uq"][l, kc * 128:(kc + 1) * 128, :].rearrange("p (h d) -> p h d", h=8)
                    sch.dma("pool", WuqN[:, kc, :].rearrange("p (h d) -> p h d", h=8), srcq[:, :, 0:64], (), [t_W])
                    sch.dma("pool", WuqR[:, kc, :].rearrange("p (h d) -> p h d", h=8), srcq[:, :, 64:96], (), [t_W])
                for kc in range(2):
                    srck = dr["w_ukv"][l, kc * 128:(kc + 1) * 128, :].rearrange("p (h d) -> p h d", h=8)
                    sch.dma("pool", WukvK[:, kc, :].rearrange("p (h d) -> p h d", h=8), srck[:, :, 0:64], (), [t_W])
                    sch.dma("pool", WukvV[:, kc, :].rearrange("p (h d) -> p h d", h=8), srck[:, :, 64:128], (), [t_W])
                sch.dma("pool", cmask, dr["cmask"], (), [t_cm])
                sch.memset("pool", QN[:, :, :], 0.0, [t_QN])
                sch.memset("pool", QR[:, :, :], 0.0, [t_QR])
                sch.memset("pool", VC[:, :, :, 64:65], 1.0, [t_VC[c] for c in range(NCH)])

                for c in range(NCH):
                    cols = slice(c * CH, (c + 1) * CH)
                    sch.dma("pool", tabC, dr["C32"][:, cols], (), [t_tab])
                    sch.dma("pool", tabS, dr["S32"][:, cols], (), [t_tab])
                    chunk_norm(c, lambda kc: gvec(l, "mix", kc), hT, t_h, sqb, t_sq, rb, t_rb, 0)
                    for j in range(6):
                        pi = j % 2
                        for kc in range(8):
                            sch.mm(psf[pi], Win[:, kc, j * 128:(j + 1) * 128], hT[:, kc, :], kc == 0, kc == 7,
                                   [t_W, t_h], [t_ps[pi]])
                        sch.act(cqn[:, j, :], psf[pi], AF.Copy, [t_ps[pi], t_const], [t_cqn], scale=gvec(l, "q", j))
                        sch.act(sqb[:, j % 2, :], psf[pi], AF.Square, [t_ps[pi]], [t_sq[j % 2]])
                        sch.mm(psf[2], ones_b, sqb[:, j % 2, :], j == 0, j == 5, [t_sq[j % 2], t_const], [t_ps[2]])
                    sch.act(rq_b, psf[2], AF.Ln, [t_ps[2]], [t_rq], scale=1.0 / 768, bias=EPS)
                    sch.act(rq_b, rq_b, AF.Exp, [t_rq], [t_rq], scale=-0.5)
                    for j in range(2):
                        pi = j % 2
                        for kc in range(8):
                            sch.mm(psf[pi], Win[:, kc, 768 + j * 128:768 + (j + 1) * 128], hT[:, kc, :], kc == 0, kc == 7,
                                   [t_W, t_h], [t_ps[pi]])
                        sch.act(ckvn[:, j, :], psf[pi], AF.Copy, [t_ps[pi], t_const], [t_ckvn], scale=gvec(l, "kv", j))
                        sch.act(sqb[:, j % 2, :], psf[pi], AF.Square, [t_ps[pi]], [t_sq[j % 2]])
                        sch.mm(psf[2], ones_b, sqb[:, j % 2, :], j == 0, j == 1, [t_sq[j % 2], t_const], [t_ps[2]])
                    for t in range(4):
                        for j in range(2):
                            sch.mm(psf[3][:, 2 * t:2 * t + 2], sqb[:, j, t * 128:(t + 1) * 128], ones_b[:, 0:2], j == 0, j == 1,
                                   [t_sq[j], t_const], [t_ps[3]])
                    sch.act(rkv_b, psf[2], AF.Ln, [t_ps[2]], [t_rkv], scale=1.0 / 256, bias=EPS)
                    sch.act(rkv_b, rkv_b, AF.Exp, [t_rkv], [t_rkv], scale=-0.5)
                    sch.act(rtok[:, 0:4], psf[3][:, 0:8].rearrange("p (t two) -> p t two", two=2)[:, :, 0], AF.Ln, [t_ps[3]], [t_rtok], scale=1.0 / 256, bias=EPS)
                    sch.act(rtok[:, 0:4], rtok[:, 0:4], AF.Exp, [t_rtok], [t_rtok], scale=-0.5)
                    for i in range(4):
                        pi = i % 2
                        for kc in range(6):
                            sch.mm(psf[pi], WuqN[:, kc, i * 128:(i + 1) * 128], cqn[:, kc, :], kc == 0, kc == 5,
                                   [t_W, t_cqn], [t_ps[pi]])
                        sch.tt("dve", QN[0:64, 2 * i, :], psf[pi][0:64, :], rq_b[0:64, :], ALU.mult, [t_ps[pi], t_rq], [t_QN])
                        sch.tt("dve", QN[64:128, 2 * i + 1, :], psf[pi][64:128, :], rq_b[64:128, :], ALU.mult, [t_ps[pi], t_rq], [t_QN])
                    def kr_proj(pi):
                        for kc in range(8):
                            sch.mm(psf[pi][0:32, :], Win[:, kc, 1024:1056], hT[:, kc, :], kc == 0, kc == 7, [t_W, t_h], [t_ps[pi]])

                    def kr_post():
                        sch.copy("pool", KR3[32:64, cols], KR3[0:32, cols], [t_KR[c]], [t_KR[c]])
                        sch.copy("pool", KR3[64:96, cols], KR3[0:32, cols], [t_KR[c]], [t_KR[c]])

                    def mk_qr_proj(g, rows):
                        def f(pi):
                            for kc in range(6):
                                sch.mm(psf[pi][0:rows, :], WuqR[:, kc, g * 96:g * 96 + rows], cqn[:, kc, :], kc == 0, kc == 5,
                                       [t_W, t_cqn], [t_ps[pi]])
                        return f
                    jobs = [dict(proj=kr_proj, rows=32, C=tabC, S=tabS, t_tab=t_tab, P=P32, dst=KR3[0:32, cols], t_dst=t_KR[c],
                                 post=kr_post)]
                    for g in range(3):
                        nh = 3 if g < 2 else 2
                        jobs.append(dict(proj=mk_qr_proj(g, nh * 32), rows=nh * 32, C=tabC, S=tabS, t_tab=t_tab, P=P32,
                                         dst=[(QR[32 * k:32 * k + 32, 3 * g + k, :], 32 * k) for k in range(nh)], t_dst=t_QR,
                                         rstd=rq_b, t_rstd=t_rq))
                    run_rope_jobs(jobs, [(xb, t_xb, t1, t_t1, t2, t_t2), (xb2, t_xb2, t12, t_t12, t22, t_t22)])
                    for i in range(4):
                        pi = i % 2
                        for kc in range(2):
                            sch.mm(psf[pi], WukvK[:, kc, i * 128:(i + 1) * 128], ckvn[:, kc, :], kc == 0, kc == 1,
                                   [t_W, t_ckvn], [t_ps[pi]])
                        sch.tt("dve", KN[:, i, cols], psf[pi], rkv_b, ALU.mult, [t_ps[pi], t_rkv], [t_KN[c]])
                    for t in range(4):
                        pi = t % 2
                        for kc in range(2):
                            sch.mm(psf[pi], ckvn[:, kc, t * 128:(t + 1) * 128], WukvV[:, kc, :], kc == 0, kc == 1,
                                   [t_W, t_ckvn], [t_ps[pi]])
                        sch.ts("dve", VC[:, c * 4 + t, :, 0:64], psf[pi].rearrange("p (h d) -> p h d", h=8),
                               rtok[:, t:t + 1], None, ALU.mult, None, [t_ps[pi], t_rtok], [t_VC[c]])
                    nkb = 4 * (c + 1)
                    steps = [(h, kb) for h in range(8) for kb in range(nkb)]

                    def mla_qk(si):
                        h, kb = steps[si]
                        i, off = h // 2, (h % 2) * 64
                        g, goff = h // 3, (h % 3) * 32
                        pi = 2 + (si % 2)
                        kc_ = kb // 4
                        kcols = slice(kb * 128, (kb + 1) * 128)
                        diag = kb >= 4 * c
                        sch.mm(psf[pi], KN[:, i, kcols], QN[:, h, :], True, False,
                               [t_KN[kc_], t_QN], [t_ps[pi]])
                        sch.mm(psf[pi], KR3[0:96, kcols], QR[0:96, h, :], False, not diag,
                               [t_KR[kc_], t_QR], [t_ps[pi]])
                        if diag:
                            sch.mm(psf[pi], ident_b, cmask[:, kb - 4 * c, :], False, True, [t_cm, t_const], [t_ps[pi]])
                        pt = si % 3
                        sch.act(PT[pt], psf[pi], AF.Exp, [t_ps[pi]], [t_PT[pt]], scale=MLA_SCALE)

                    def mla_pv(si):
                        h, kb = steps[si]
                        po = 4 + (h % 2)
                        pt = si % 3
                        sch.mm(psf[po][0:65, :], VC[:, kb, h, 0:65], PT[pt], kb == 0, kb == nkb - 1,
                               [t_VC[kb // 4], t_PT[pt]], [t_ps[po]])

                    def mla_norm_a(h):
                        po = 4 + (h % 2)
                        sch.op("dve", lambda e, po=po: e.reciprocal(out=rec[64:65, :], in_=psf[po][64:65, :]),
                               [t_ps[po]], [t_rec])

                    def mla_norm_b(h):
                        i, off = h // 2, (h % 2) * 64
                        po = 4 + (h % 2)
                        sch.mm(psf[6][0:64, :], ones_f[64:65, 0:64], rec[64:65, :], True, True, [t_rec, t_const], [t_ps[6]])
                        sch.copy("act", pbs[0:64, :], psf[6][0:64, :], [t_ps[6]], [t_pbs])
                        sch.tt("dve", OA[off:off + 64, i, cols], psf[po][0:64, :], pbs[0:64, :], ALU.mult,
                               [t_ps[po], t_pbs], [t_OA[c]])

                    mla_qk(0)
                    pend = None
                    for si in range(len(steps)):
                        if si + 1 < len(steps):
                            mla_qk(si + 1)
                        mla_pv(si)
                        if pend is not None:
                            pend[1] -= 1
                            if pend[1] == 0:
                                mla_norm_b(pend[0])
                                pend = None
                        h, kb = steps[si]
                        if kb == nkb - 1:
                            if pend is not None:
                                mla_norm_b(pend[0])
                            mla_norm_a(h)
                            pend = [h, 2]
                    if pend is not None:
                        mla_norm_b(pend[0])

                if dbg == "p1":
                    break
                sch.barrier()
                A = Carver(PHASE_BASE)
                Vbf = alloc(A, BF16, [128, 256])
                Vz = alloc(A, BF16, [128, 256])
                Gs = alloc(A, F32, [128, 256])
                Ktok = alloc(A, BF16, [128, 256])
                AT = alloc(A, BF16, [128, 4, 128])
                rsb = alloc(A, F32, [128, 256])
                oct_ = alloc(A, BF16, [128, 256])
                bst = alloc(A, F32, [128, 4, 6])
                bag = alloc(A, F32, [128, 4, 2])
                Win = alloc(A, BF16, [128, 8, 1704])
                Wout = alloc(A, BF16, [128, 8, D])
                KB2 = alloc(A, BF16, [128, S])
                KI3 = alloc(A, BF16, [128, S])
                VB = alloc(A, BF16, [128, NT, 65])
                hT = alloc(A, BF16, [128, 8, CH])
                rb = alloc(A, F32, [128, CH])
                tC32 = alloc(A, BF16, [128, CH])
                tS32 = alloc(A, BF16, [128, CH])
                tC64 = alloc(A, BF16, [128, CH])
                tS64 = alloc(A, BF16, [128, CH])
                xb = alloc(A, BF16, [128, CH])
                t1 = alloc(A, BF16, [128, CH])
                t2 = alloc(A, BF16, [128, CH])
                QB = alloc(A, BF16, [128, 2, CH])
                QI = alloc(A, BF16, [128, 3, CH])
                WI = alloc(A, F32, [128, 4, 8])
                RQ = alloc(A, BF16, [128, 2, CH])
                RQX = alloc(A, BF16, [128, 2, CH])
                RK = alloc(A, BF16, [128, 2, CH])
                xiT = alloc(A, F32, [128, 2, 128])
                decT = alloc(A, F32, [128, 4, 128])
                zetab = alloc(A, F32, [128, 256])
                cdb = alloc(A, F32, [128, 256])
                cb = alloc(A, F32, [128, 128])
                ctab = alloc(A, F32, [128, NIT + 1])
                gret = gp[:, l, 24:280]
                OT = alloc(A, BF16, [128, 4, CH])
                Ibuf = alloc(A, F32, [128, S])
                Rtmp2 = alloc(A, BF16, [128, 2, CH])
                Rtmp = [Rtmp2[:, 0, :], Rtmp2[:, 1, :]]
                sqb = Rtmp2
                Mb = alloc(A, BF16, [128, S])
                junk = Mb
                MbT = [alloc(A, BF16, [128, NT, 128]) for _ in range(2)]
                PTd = [alloc(A, BF16, [128, 4, 128]) for _ in range(2)]
                smA = alloc(A, F32, [128, 2])
                smR = alloc(A, F32, [128, 4])
                OBt = alloc(A, BF16, [128, 256])
                sm = alloc(A, F32, [128, 64])
                wtab = alloc(A, F32, [128, NIT + 1])
                Rst = alloc(A, F32, [128, 256])
                Rbf = alloc(A, BF16, [128, 256])
                t_W, t_Wo, t_cst = Tok(), Tok(), Tok()
                t_KB = [Tok() for _ in range(NCH)]
                t_KI = [Tok() for _ in range(NCH)]
                t_VB = [Tok() for _ in range(NCH)]
                t_h, t_rb = Tok(), Tok()
                t_tab, t_xb, t_t1, t_t2 = Tok(), Tok(), Tok(), Tok()
                t_QB, t_QI, t_WI, t_RQ, t_RQX, t_RK, t_OT = Tok(), Tok(), Tok(), Tok(), Tok(), Tok(), Tok()
                t_I, t_junk, t_Mb, t_OBt, t_sm, t_wtab = Tok(), Tok(), Tok(), Tok(), Tok(), Tok()
                t_MbT = [Tok(), Tok()]
                t_smA, t_smR = Tok(), Tok()
                t_Rtmp = [Tok(), Tok()]
                t_sq = t_Rtmp
                t_junk = t_Mb
                t_PTd = [Tok(), Tok()]
                t_Rst, t_Rbf, t_Vbf, t_Vz, t_Gs, t_Ktok, t_AT, t_rsb, t_oct, t_bst = (Tok() for _ in range(10))

                for kc in range(8):
                    sch.dma("pool", Win[:, kc, :], dr["w_in"][l, kc * 128:(kc + 1) * 128, 1056:2760], (), [t_W])
                for kc in range(8):
                    sch.dma("pool", Wout[:, kc, :], dr["w_out"][l, kc * 128:(kc + 1) * 128, :], (), [t_Wo])
                sch.dma("sp", xiT, dr["xiT"][:, :, 0:128], (), [t_cst])
                sch.dma("sp", decT, dr["decT"], (), [t_cst])
                sch.dma("sp", zetab, dr["zetab"], (), [t_cst])
                sch.dma("sp", cdb[0:64, :], dr["cdb"], (), [t_cst])
                sch.dma("sp", cb, dr["cb"], (), [t_cst])
                sch.dma("sp", ctab, dr["ctab"], (), [t_cst])
                sch.memset("pool", VB[:, :, 64:65], 1.0, [t_VB[c] for c in range(NCH)])
                sch.memset("pool", Rst[:, :], 0.0, [t_Rst])
                sch.memset("pool", Rbf[:, :], 0.0, [t_Rbf])

                def wc(a, b):
                    return slice(a - 1056, b - 1056)

                for c in range(NCH):
                    cols = slice(c * CH, (c + 1) * CH)
                    sch.dma("pool", tC32, dr["C32"][:, cols], (), [t_tab])
                    sch.dma("pool", tS32, dr["S32"][:, cols], (), [t_tab])
                    sch.dma("pool", tC64, dr["C64"][:, cols], (), [t_tab])
                    sch.dma("pool", tS64, dr["S64"][:, cols], (), [t_tab])
                    chunk_norm(c, lambda kc: gvec(l, "mix", kc), hT, t_h, sqb, t_sq, rb, t_rb, 0)

                    def proj(pi, c0, c1, rows):
                        for kc in range(8):
                            sch.mm(psf[pi][0:rows, :], Win[:, kc, wc(c0, c1)], hT[:, kc, :], kc == 0, kc == 7,
                                   [t_W, t_h], [t_ps[pi]])
                    def mkproj(c0, c1, rows):
                        def f(pi):
                            for kc in range(8):
                                sch.mm(psf[pi][0:rows, :], Win[:, kc, wc(c0, c1)], hT[:, kc, :], kc == 0, kc == 7,
                                       [t_W, t_h], [t_ps[pi]])
                        return f
                    jobs = []
                    for i in range(2):
                        jobs.append(dict(proj=mkproj(1056 + 128 * i, 1056 + 128 * (i + 1), 128), rows=128, C=tC64, S=tS64, t_tab=t_tab,
                                         P=P64, dst=QB[:, i, :], t_dst=t_QB))
                    jobs.append(dict(proj=mkproj(1312, 1376, 64), rows=64, C=tC64, S=tS64, t_tab=t_tab, P=P64,
                                     dst=KB2[0:64, cols], t_dst=t_KB[c],
                                     post=lambda: sch.copy("pool", KB2[64:128, cols], KB2[0:64, cols], [t_KB[c]], [t_KB[c]])))
                    for g in range(3):
                        nh = 3 if g < 2 else 2
                        jobs.append(dict(proj=mkproj(1440 + 96 * g, 1440 + 96 * g + 32 * nh, 32 * nh), rows=32 * nh, C=tC32, S=tS32,
                                         t_tab=t_tab, P=P32, dst=QI[0:32 * nh, g, :], t_dst=t_QI))

                    def ki_post():
                        sch.copy("pool", KI3[32:64, cols], KI3[0:32, cols], [t_KI[c]], [t_KI[c]])
                        sch.copy("pool", KI3[64:96, cols], KI3[0:32, cols], [t_KI[c]], [t_KI[c]])
                    jobs.append(dict(proj=mkproj(1696, 1728, 32), rows=32, C=tC32, S=tS32, t_tab=t_tab, P=P32,
                                     dst=KI3[0:32, cols], t_dst=t_KI[c], post=ki_post))
                    for i in range(2):
                        jobs.append(dict(proj=mkproj(1736 + 128 * i, 1736 + 128 * (i + 1), 128), rows=128, C=tC64, S=tS64, t_tab=t_tab,
                                         P=P64, dst=RQ[:, i, :], t_dst=t_RQ))
                        jobs.append(dict(proj=mkproj(1992 + 128 * i, 1992 + 128 * (i + 1), 128), rows=128, C=tC64, S=tS64, t_tab=t_tab,
                                         P=P64, dst=RK[:, i, :], t_dst=t_RK))
                    run_rope_jobs(jobs, [(xb, t_xb, t1, t_t1, t2, t_t2)])
                    if stop == "proj2":
                        break
                    for t in range(4):
                        tcols = slice(t * 128, (t + 1) * 128)
                        for kc in range(8):
                            sch.mm(psf[2][:, 0:64], hT[:, kc, tcols], Win[:, kc, wc(1376, 1440)], kc == 0, kc == 7,
                                   [t_W, t_h], [t_ps[2]])
                        for kc in range(8):
                            sch.mm(psf[2][:, 64:128], hT[:, kc, tcols], Win[:, kc, wc(1728, 1792)], kc == 0, kc == 7,
                                   [t_W, t_h], [t_ps[2]])
                        sch.copy("act", VB[:, c * 4 + t, 0:64], psf[2][:, 0:64], [t_ps[2]], [t_VB[c]])
                        sch.act(WI[:, t, :], psf[2][:, 64:72], AF.Copy, [t_ps[2]], [t_WI], scale=IDX_SCALE)

                    if stop == "proj":
                        break
                    def dsa_idx(j):
                        jt = c * 4 + j
                        qcols = slice(j * 128, (j + 1) * 128)
                        W = 128 * (jt + 1)
                        ngrp = (W + 511) // 512
                        n = 0
                        for h in range(8):
                            g, goff = h // 3, (h % 3) * 32
                            for kg in range(ngrp):
                                k0, k1 = kg * 512, min(W, kg * 512 + 512)
                                pi = 2 + n % 2
                                rt = n % 2
                                n += 1
                                sch.mm(psf[pi][:, 0:k1 - k0], QI[goff:goff + 32, g, qcols], KI3[goff:goff + 32, k0:k1], True, True,
                                       [t_QI] + [t_KI[k0 // 512]], [t_ps[pi]])
                                sch.act(Rtmp[rt][:, 0:k1 - k0], psf[pi][:, 0:k1 - k0], AF.Relu, [t_ps[pi]], [t_Rtmp[rt]])
                                if h == 0:
                                    sch.ts("dve", Ibuf[:, k0:k1], Rtmp[rt][:, 0:k1 - k0], WI[:, j, 0:1], None, ALU.mult, None,
                                           [t_Rtmp[rt], t_WI], [t_I])
                                else:
                                    sch.stt(Ibuf[:, k0:k1], Rtmp[rt][:, 0:k1 - k0], WI[:, j, h:h + 1], Ibuf[:, k0:k1],
                                            ALU.mult, ALU.add, [t_Rtmp[rt], t_WI, t_I], [t_I])

                    def dsa_bis(j):
                        jt = c * 4 + j
                        W = 128 * (jt + 1)
                        sch.op("dve", lambda e, W=W: e.tensor_reduce(out=sm[:, 0:1], in_=Ibuf[:, 0:W], axis=AX.X, op=ALU.max,
                                                                      apply_absolute_value=True), [t_I], [t_sm])
                        sch.ts("dve", sm[:, 1:2], sm[:, 0:1], 2.0, 2.0, ALU.mult, ALU.add, [t_sm], [t_sm])
                        sch.ts("dve", wtab[:, :], ctab[:, :], sm[:, 1:2], None, ALU.mult, None, [t_sm, t_cst], [t_wtab])
                        sch.tt("dve", Ibuf[:, W - 128:W], Ibuf[:, W - 128:W], cb[:, :], ALU.add, [t_I, t_cst], [t_I])
                        sch.memset("dve", sm[:, 2:3], 0.0, [t_sm])
                        for it in range(NIT):
                            sch.ts("dve", junk[:, 0:W], Ibuf[:, 0:W], sm[:, 2:3], 0.0, ALU.is_ge, ALU.add, [t_I, t_sm], [t_junk, t_sm],
                                   accum=sm[:, 3:4])
                            sch.ts("dve", sm[:, 4:5], sm[:, 3:4], 255.5, 0.5, ALU.is_ge, ALU.subtract, [t_sm], [t_sm])
                            sch.stt(sm[:, 2:3], sm[:, 4:5], wtab[:, it:it + 1], sm[:, 2:3], ALU.mult, ALU.add, [t_sm, t_wtab], [t_sm])
                        sch.tt("dve", sm[:, 5:6], sm[:, 2:3], wtab[:, NIT:NIT + 1], ALU.subtract, [t_sm, t_wtab], [t_sm])
                        sch.ts("dve", Mb[:, 0:W], Ibuf[:, 0:W], sm[:, 5:6], NEG, ALU.is_lt, ALU.mult, [t_I, t_sm], [t_Mb])

                    def dsa_tr(j):
                        jt = c * 4 + j
                        mb = jt % 2
                        for kb0 in range(0, jt + 1, 8):
                            nb = min(8, jt + 1 - kb0)
                            for ii in range(nb):
                                kb = kb0 + ii
                                sch.tr(psb[:, ii * 128:(ii + 1) * 128], Mb[:, kb * 128:(kb + 1) * 128], ident_b,
                                       [t_Mb, t_const], [t_psb])
                            sch.copy("act", MbT[mb][:, kb0:kb0 + nb, :], psb[:, 0:nb * 128].rearrange("p (a b) -> p a b", a=nb),
                                     [t_psb], [t_MbT[mb]])

                    def dsa_attn(j):
                        jt = c * 4 + j
                        mb = jt % 2
                        qcols = slice(j * 128, (j + 1) * 128)
                        steps = [(h, kb0) for h in range(4) for kb0 in range(0, jt + 1, 4)]

                        def qk(si):
                            h, kb0 = steps[si]
                            i, off = h // 2, (h % 2) * 64
                            nb = min(4, jt + 1 - kb0)
                            pi = si % 2
                            for ii in range(nb):
                                kb = kb0 + ii
                                sch.mm(psf[pi][:, ii * 128:(ii + 1) * 128], KB2[off:off + 64, kb * 128:(kb + 1) * 128],
                                       QB[off:off + 64, i, qcols], True, False, [t_KB[kb // 4], t_QB], [t_ps[pi]])
                                sch.mm(psf[pi][:, ii * 128:(ii + 1) * 128], ident_b, MbT[mb][:, kb, :], False, True,
                                       [t_MbT[mb], t_const], [t_ps[pi]])
                            sch.act(PTd[pi][:, 0:nb, :], psf[pi][:, 0:nb * 128].rearrange("p (a b) -> p a b", a=nb), AF.Exp,
                                    [t_ps[pi]], [t_PTd[pi]], scale=DSA_SCALE)

                        def pv(si):
                            h, kb0 = steps[si]
                            nb = min(4, jt + 1 - kb0)
                            pi = si % 2
                            po = 4 + (h % 2)
                            for ii in range(nb):
                                kb = kb0 + ii
                                sch.mm(psf[po][:, 0:65], PTd[pi][:, ii, :], VB[:, kb, 0:65], kb == 0, kb == jt,
                                       [t_PTd[pi], t_VB[kb // 4]], [t_ps[po]])
                            if kb0 + nb == jt + 1:
                                sch.op("dve", lambda e, po=po: e.reciprocal(out=smA[:, 0:1], in_=psf[po][:, 64:65]), [t_ps[po]], [t_smA])
                                sch.ts("dve", OBt[:, h * 64:(h + 1) * 64], psf[po][:, 0:64], smA[:, 0:1], None, ALU.mult, None,
                                       [t_ps[po], t_smA], [t_OBt])

                        qk(0)
                        for si in range(len(steps)):
                            if si + 1 < len(steps):
                                qk(si + 1)
                            pv(si)
                        for i in range(2):
                            sch.tr(psb[:, i * 128:(i + 1) * 128], OBt[:, i * 128:(i + 1) * 128], ident_b, [t_OBt, t_const], [t_psb])
                        sch.copy("act", OT[:, 0:2, qcols], psb[:, 0:256].rearrange("p (a b) -> p a b", a=2), [t_psb], [t_OT])

                    def ret_tile(j):
                        qcols = slice(j * 128, (j + 1) * 128)
                        for i in range(2):
                            sch.tt("pool", RQX[:, i, qcols], RQ[:, i, qcols], xiT[:, i, :], ALU.mult, [t_RQ, t_cst], [t_RQX])
                        for kc in range(8):
                            sch.mm(psf[6], hT[:, kc, qcols], Win[:, kc, wc(2248, 2760)], kc == 0, kc == 7, [t_W, t_h], [t_ps[6]])
                        sch.copy("act", Vbf[:, :], psf[6][:, 0:256], [t_ps[6]], [t_Vbf])
                        sch.tt("pool", Vz[:, :], Vbf[:, :], zetab[:, :], ALU.mult, [t_Vbf, t_cst], [t_Vz])
                        sch.act(Gs[:, :], psf[6][:, 256:512], AF.Exp, [t_ps[6]], [t_Gs], scale=-1.0)
                        sch.ts("pool", Gs[:, :], Gs[:, :], 1.0, None, ALU.add, None, [t_Gs], [t_Gs])
                        sch.op("dve", lambda e: e.reciprocal(out=Gs[:, :], in_=Gs[:, :]), [t_Gs], [t_Gs])
                        sch.tt("dve", Gs[:, :], Gs[:, :], psf[6][:, 256:512], ALU.mult, [t_Gs, t_ps[6]], [t_Gs])
                        for i in range(2):
                            sch.tr(psb[:, i * 128:(i + 1) * 128], RK[:, i, qcols], ident_b, [t_RK, t_const], [t_psb])
                        sch.copy("act", Ktok[:, :], psb[:, 0:256], [t_psb], [t_Ktok])
                        for h in range(4):
                            i, off = h // 2, (h % 2) * 64
                            sch.mm(psf[2 + h % 2][:, (h // 2) * 128:(h // 2 + 1) * 128], RK[off:off + 64, i, qcols],
                                   RQ[off:off + 64, i, qcols], True, True, [t_RK, t_RQ], [t_ps[2 + h % 2]])
                        for par in range(2):
                            sch.tt("dve", AT[:, 2 * par:2 * par + 2, :], psf[2 + par][:, 0:256].rearrange("p (a b) -> p a b", a=2),
                                   decT[:, 2 * par:2 * par + 2, :], ALU.mult, [t_ps[2 + par], t_cst], [t_AT])
                        for h in range(4):
                            i, off = h // 2, (h % 2) * 64
                            sch.mm(psf[3][:, h * 64:(h + 1) * 64], AT[:, (h % 2) * 2 + h // 2, :], Vbf[:, h * 64:(h + 1) * 64], True, False,
                                   [t_AT, t_Vbf], [t_ps[3]])
                            sch.mm(psf[3][:, h * 64:(h + 1) * 64], RQX[off:off + 64, i, qcols], Rbf[off:off + 64, h * 64:(h + 1) * 64],
                                   False, True, [t_RQX, t_Rbf], [t_ps[3]])
                        for h in range(4):
                            sch.mm(psf[6][0:64, h * 64:(h + 1) * 64], Ktok[:, h * 64:(h + 1) * 64], Vz[:, h * 64:(h + 1) * 64], True, True,
                                   [t_Ktok, t_Vz], [t_ps[6]])
                        sch.tt("pool", Rst[0:64, :], Rst[0:64, :], cdb[0:64, :], ALU.mult, [t_Rst, t_cst], [t_Rst])
                        sch.tt("dve", Rst[0:64, :], Rst[0:64, :], psf[6][0:64, 0:256], ALU.add, [t_Rst, t_ps[6]], [t_Rst])
                        sch.copy("act", Rbf[0:64, :], Rst[0:64, :], [t_Rst], [t_Rbf])
                        sch.copy("act", Rbf[64:128, :], Rst[0:64, :], [t_Rst], [t_Rbf])
                        sch.copy("act", rsb[:, :], psf[3][:, 0:256], [t_ps[3]], [t_rsb])
                        for h in range(4):
                            sch.op("dve", lambda e, h=h: e.bn_stats(out=bst[:, h, :], in_=rsb[:, h * 64:(h + 1) * 64]), [t_rsb], [t_bst])
                            sch.op("dve", lambda e, h=h: e.bn_aggr(out=bag[:, h, :], in_=bst[:, h, :]), [t_bst], [t_bst])
                        sch.act(smR[:, 0:4], bag[:, :, 1], AF.Sqrt, [t_bst], [t_smR], bias=EPS)
                        sch.op("dve", lambda e: e.reciprocal(out=smR[:, 0:4], in_=smR[:, 0:4]), [t_smR], [t_smR])
                        for h in range(4):
                            sch.ts("dve", rsb[:, h * 64:(h + 1) * 64], rsb[:, h * 64:(h + 1) * 64], bag[:, h, 0:1], smR[:, h:h + 1],
                                   ALU.subtract, ALU.mult, [t_rsb, t_bst, t_smR], [t_rsb])
                        sch.tt("pool", rsb[:, :], rsb[:, :], gret, ALU.mult, [t_rsb, t_const], [t_rsb])
                        sch.tt("pool", oct_[:, :], rsb[:, :], Gs[:, :], ALU.mult, [t_rsb, t_Gs], [t_oct])
                        for i in range(2):
                            sch.tr(psb[:, i * 128:(i + 1) * 128], oct_[:, i * 128:(i + 1) * 128], ident_b, [t_oct, t_const], [t_psb])
                        sch.copy("act", OT[:, 2:4, qcols], psb[:, 0:256].rearrange("p (a b) -> p a b", a=2), [t_psb], [t_OT])

                    prev = None
                    for j in range(4):
                        dsa_idx(j)
                        dsa_bis(j)
                        if prev is not None:
                            dsa_attn(prev)
                        ret_tile(j)
                        dsa_tr(j)
                        prev = j
                    dsa_attn(prev)

                    for oc in range(8):
                        pi = oc % 2
                        for mc in range(8):
                            if mc < 4:
                                sch.mm(psf[pi], Wout[:, mc, oc * 128:(oc + 1) * 128], OA[:, mc, cols], mc == 0, False,
                                       [t_Wo, t_OA[c]], [t_ps[pi]])
                            else:
                                sch.mm(psf[pi], Wout[:, mc, oc * 128:(oc + 1) * 128], OT[:, mc - 4, :], False, mc == 7,
                                       [t_Wo, t_OT], [t_ps[pi]])
                        sch.tt("dve", xT[:, oc, cols], xT[:, oc, cols], psf[pi], ALU.add, [t_xT[c], t_ps[pi]], [t_xT[c]])

                if dbg == "p2":
                    break
                sch.barrier()
                A = Carver(PHASE_BASE)
                hfT = alloc(A, BF16, [128, 8, S])
                W1 = [alloc(A, BF16, [128, 8, 1024]) for _ in range(2)]
                W2 = [alloc(A, BF16, [128, 8, D]) for _ in range(2)]
                hid = [alloc(A, BF16, [128, 8, CH]) for _ in range(2)]
                rl = [alloc(A, BF16, [128, CH]) for _ in range(2)]
                sqb = alloc(A, BF16, [128, 2, CH])
                rb = alloc(A, F32, [128, CH])
                t_hf = [Tok() for _ in range(NCH)]
                t_W1 = [Tok(), Tok()]
                t_W2 = [Tok(), Tok()]
                t_hid = [Tok(), Tok()]
                t_rl = [Tok(), Tok()]
                t_sq = [Tok(), Tok()]
                t_rb = Tok()

                def load_q(qr):
                    b = qr % 2
                    for kc in range(8):
                        sch.dma("pool", W1[b][:, kc, :], dr["w_ff1"][l, kc * 128:(kc + 1) * 128, qr * 1024:(qr + 1) * 1024], (), [t_W1[b]])
                    for fc in range(8):
                        sch.dma("pool", W2[b][:, fc, :], dr["w_ff2"][l, qr * 1024 + fc * 128: qr * 1024 + (fc + 1) * 128, :], (), [t_W2[b]])

                load_q(0)
                for c in range(NCH):
                    chunk_norm(c, lambda kc: gvec(l, "mlp", kc), hfT[:, :, c * CH:(c + 1) * CH], t_hf[c], sqb, t_sq, rb, t_rb, 0)
                for qr in range(4):
                    b = qr % 2
                    if qr + 1 < 4:
                        load_q(qr + 1)
                    for c in range(NCH):
                        cols = slice(c * CH, (c + 1) * CH)
                        hb = c % 2
                        for fc in range(8):
                            pi = fc % 2
                            for kc in range(8):
                                sch.mm(psf[pi], W1[b][:, kc, fc * 128:(fc + 1) * 128], hfT[:, kc, cols], kc == 0, kc == 7,
                                       [t_W1[b], t_hf[c]], [t_ps[pi]])
                            sch.act(rl[pi], psf[pi], AF.Relu, [t_ps[pi]], [t_rl[pi]])
                            sch.tt("pool", hid[hb][:, fc, :], rl[pi], rl[pi], ALU.mult, [t_rl[pi]], [t_hid[hb]])
                        for oc in range(8):
                            pi = 2 + oc % 2
                            for fc in range(8):
                                sch.mm(psf[pi], W2[b][:, fc, oc * 128:(oc + 1) * 128], hid[hb][:, fc, :], fc == 0, fc == 7,
                                       [t_W2[b], t_hid[hb]], [t_ps[pi]])
                            sch.tt("dve", xT[:, oc, cols], xT[:, oc, cols], psf[pi], ALU.add, [t_xT[c], t_ps[pi]], [t_xT[c]])

            sch.barrier()
            Lc = Carver(PHASE_BASE)
            yT = alloc(Lc, F32, [128, 8, CH])
            yo = [alloc(Lc, F32, [128, D]) for _ in range(2)]
            sqb = alloc(Lc, BF16, [128, 2, CH])
            rb = alloc(Lc, F32, [128, CH])
            t_y, t_rb = Tok(), Tok()
            t_sq = [Tok(), Tok()]
            t_yo = [Tok(), Tok()]
            for c in range(NCH):
                cols = slice(c * CH, (c + 1) * CH)
                if dbg:
                    src = None
                if final_norm:
                    for kc in range(8):
                        sch.act(sqb[:, kc % 2, :], xT[:, kc, cols], AF.Square, [t_xT[c]], [t_sq[kc % 2]])
                        sch.mm(psf[0], ones_b, sqb[:, kc % 2, :], kc == 0, kc == 7, [t_sq[kc % 2], t_const], [t_ps[0]])
                    sch.act(rb, psf[0], AF.Ln, [t_ps[0]], [t_rb], scale=1.0 / D, bias=EPS)
                    sch.act(rb, rb, AF.Exp, [t_rb], [t_rb], scale=-0.5)
                    for kc in range(8):
                        sch.stt(yT[:, kc, :], xT[:, kc, cols], gfin[:, kc:kc + 1], rb, ALU.mult, ALU.mult,
                                [t_xT[c], t_rb, t_const], [t_y])
                    srcT = lambda kc, t: yT[:, kc, t * 128:(t + 1) * 128]
                    t_src = t_y
                else:
                    srcT = lambda kc, t, c=c: xT[:, kc, c * CH + t * 128: c * CH + (t + 1) * 128]
                    t_src = t_xT[c]
                for t in range(4):
                    tt_ = c * 4 + t
                    b = tt_ % 2
                    for half in range(2):
                        pi = 1 + half
                        for k4 in range(4):
                            kc = half * 4 + k4
                            sch.tr(psf[pi][:, k4 * 128:(k4 + 1) * 128], srcT(kc, t), ident_f, [t_src, t_const], [t_ps[pi]])
                        sch.copy("act" if half == 0 else "dve", yo[b][:, half * 512:(half + 1) * 512], psf[pi], [t_ps[pi]], [t_yo[b]])
                    sch.dma("sp", dr["y"][sq_i * S + tt_ * 128: sq_i * S + (tt_ + 1) * 128, :], yo[b], [t_yo[b]], [t_yo[b]])

        if dbg:
            sch.barrier()
            if dbg == "p1":
                Lc = Carver(PHASE_BASE)
                tmp = alloc(Lc, F32, [128, 4, S])
                tk = Tok()
                for i in range(4):
                    sch.copy("dve", tmp[:, i, :], OA[:, i, :], [t_OA[c] for c in range(NCH)], [tk])
                sch.dma("sp", dr["dbg"][:, 0:4, :], tmp, [tk], [tk])
            else:
                sch.dma("sp", dr["dbg"], xT, [t_xT[c] for c in range(NCH)], [Tok()])
        final_waits = [(k, sch.dma_cnt[k]) for k in range(sch.n_dma_sems) if sch.dma_cnt[k] > 0]
        sch.emit(nc, block, sems, dsems, final_waits)
    return nc


NCORES = 8
_CACHE = {}


def _gpack(g_mix, g_q, g_kv, g_mlp, g_ret):
    L = g_mix.shape[0]
    out = np.zeros((L, 128, NG), np.float32)
    for l in range(L):
        out[l, :, 0:8] = np.asarray(g_mix[l], np.float32).reshape(8, 128).T
        out[l, :, 8:14] = np.asarray(g_q[l], np.float32).reshape(6, 128).T
        out[l, :, 14:16] = np.asarray(g_kv[l], np.float32).reshape(2, 128).T
        out[l, :, 16:24] = np.asarray(g_mlp[l], np.float32).reshape(8, 128).T
        out[l, :, 24:280] = np.broadcast_to(np.asarray(g_ret[l], np.float32)[None, :], (128, 256))
    return out


def run_layers(x, layers, final_norm, weights, g_final, ncores=NCORES, nseq=None, dbg=None):
    B = x.shape[0]
    nseq = B // ncores if nseq is None else nseq
    L = len(layers)
    key = (nseq, L, final_norm, dbg)
    if key not in _CACHE:
        _CACHE[key] = build_nc(nseq, L, final_norm, dbg)
    nc = _CACHE[key]
    consts = _consts()
    common = {"c_" + k: np.ascontiguousarray(v, dtype=np.float32) for k, v in consts.items()}
    for nm in ("w_in", "w_uq", "w_ukv", "w_out", "w_ff1", "w_ff2"):
        common[nm] = np.ascontiguousarray(np.asarray(weights[nm], np.float32)[layers])
    common["gpack"] = _gpack(*[np.asarray(weights[n])[layers] for n in ("g_mix", "g_q", "g_kv", "g_mlp", "g_ret")])
    common["gfin"] = np.ascontiguousarray(np.asarray(g_final, np.float32).reshape(8, 128).T)
    in_maps = []
    for ci in range(ncores):
        m = dict(common)
        m["x"] = np.ascontiguousarray(np.asarray(x[ci * nseq:(ci + 1) * nseq], np.float32).reshape(nseq * S, D))
        in_maps.append(m)
    res = run_bass_kernel_spmd(nc, in_maps, core_ids=list(range(ncores)))
    y = np.concatenate([r["y"].reshape(nseq, S, D) for r in res.results], axis=0)
    if dbg:
        return y, [r["dbg"] for r in res.results]
    return y


FUSED = True


def kernel(x, g_mix, w_in, g_q, w_uq, g_kv, w_ukv, g_ret, w_out, g_mlp, w_ff1, w_ff2, g_final):
    weights = dict(g_mix=g_mix, w_in=w_in, g_q=g_q, w_uq=w_uq, g_kv=g_kv, w_ukv=w_ukv, g_ret=g_ret, w_out=w_out,
                   g_mlp=g_mlp, w_ff1=w_ff1, w_ff2=w_ff2)
    x = np.asarray(x, np.float32)
    depth = np.asarray(w_in).shape[0]
    if FUSED:
        return run_layers(x, list(range(depth)), True, weights, g_final).astype(np.float32)
    y = x
    for l in range(depth):
        y = run_layers(y, [l], l == depth - 1, weights, g_final)
    return y.astype(np.float32)
```

```python
import numpy as np
import concourse.bass as bass
import concourse.mybir as mybir
from concourse.bass_utils import run_bass_kernel_spmd

F32 = mybir.dt.float32
BF16 = mybir.dt.bfloat16
AF = mybir.ActivationFunctionType
ALU = mybir.AluOpType
AX = mybir.AxisListType

D = 1024
S = 2048
NCH = 4
CH = 512
NT = 16
INW = 2760
DFF = 4096
EPS = 1e-6
NIT = 16
MLA_SCALE = 96 ** -0.5
DSA_SCALE = 64 ** -0.5
IDX_SCALE = (8 ** -0.5) * (32 ** -0.5)
NEG = -30000.0
NG = 8 + 6 + 2 + 8 + 256


class Tok:
    __slots__ = ("name", "lw", "rds")

    def __init__(self, name=""):
        self.name = name
        self.lw = None
        self.rds = []


class Op:
    __slots__ = ("eng", "fn", "deps", "inc", "val", "dsem", "dval", "is_dma", "ep")


class Sched:
    ENGS = ("pe", "act", "dve", "pool", "sp")

    def __init__(self, n_dma_sems=24):
        self.ops = {e: [] for e in self.ENGS}
        self.n_dma_sems = n_dma_sems
        self.dma_last = [None] * n_dma_sems
        self.dma_cnt = [0] * n_dma_sems
        self.dma_rr = 0
        self.pending = {e: [] for e in self.ENGS}
        self.epoch = 0
        self.nsets = 12

    def barrier(self):
        lasts = []
        for e in self.ENGS:
            for op in reversed(self.ops[e]):
                if not op.is_dma:
                    lasts.append(op)
                    break
        for d in self.dma_last:
            if d is not None:
                lasts.append(d)
        for e in self.ENGS:
            self.pending[e] = list(lasts)
        self.epoch += 1

    def _rec(self, eng, fn, rd, wr, is_dma=False):
        op = Op()
        op.eng = eng
        op.fn = fn
        op.inc = False
        op.val = None
        op.is_dma = is_dma
        op.ep = self.epoch % self.nsets
        op.dsem = None
        op.dval = None
        deps = list(self.pending[eng])
        self.pending[eng] = []
        for t in rd:
            if t.lw is not None:
                deps.append(t.lw)
        for t in wr:
            if t.lw is not None:
                deps.append(t.lw)
            deps.extend(t.rds)
        if is_dma:
            k = self.dma_rr % self.n_dma_sems
            self.dma_rr += 1
            if self.dma_last[k] is not None:
                deps.append(self.dma_last[k])
            self.dma_last[k] = op
            self.dma_cnt[k] += 16
            op.dsem = k
            op.dval = self.dma_cnt[k]
        fd = []
        seen = set()
        for d in deps:
            if id(d) in seen or d is op:
                continue
            seen.add(id(d))
            if (not d.is_dma) and d.eng == "pe" and eng == "pe":
                continue
            if not d.is_dma:
                d.inc = True
            fd.append(d)
        op.deps = fd
        for t in rd:
            t.rds.append(op)
        for t in wr:
            t.lw = op
            t.rds = []
        self.ops[eng].append(op)
        return op

    def op(self, eng, fn, rd=(), wr=()):
        return self._rec(eng, fn, rd, wr)

    def dma(self, eng, out, in_, rd=(), wr=()):
        return self._rec(eng, lambda e: e.dma_start(out=out, in_=in_), rd, wr, is_dma=True)

    def mm(self, out, lhsT, rhs, start, stop, rd, wr):
        return self.op("pe", lambda e: e.matmul(out, lhsT, rhs, start=start, stop=stop), rd, wr)

    def tr(self, out, in_, ident, rd, wr):
        return self.op("pe", lambda e: e.transpose(out, in_, ident), rd, wr)

    def act(self, out, in_, func, rd, wr, scale=None, bias=None, accum=None):
        kw = {}
        if scale is not None:
            kw["scale"] = scale
        if bias is not None:
            kw["bias"] = bias
        if accum is not None:
            kw["accum_out"] = accum
        return self.op("act", lambda e: e.activation(out=out, in_=in_, func=func, **kw), rd, wr)

    def tt(self, eng, out, in0, in1, op, rd, wr):
        return self.op(eng, lambda e: e.tensor_tensor(out=out, in0=in0, in1=in1, op=op), rd, wr)

    def ts(self, eng, out, in0, s1, s2, op0, op1, rd, wr, accum=None):
        if op1 is None:
            return self.op(eng, lambda e: e.tensor_scalar(out=out, in0=in0, scalar1=s1, scalar2=None, op0=op0), rd, wr)
        if accum is not None:
            return self.op(eng, lambda e: e.tensor_scalar(out=out, in0=in0, scalar1=s1, scalar2=s2, op0=op0,
                                                          op1=op1, accum_out=accum), rd, wr)
        return self.op(eng, lambda e: e.tensor_scalar(out=out, in0=in0, scalar1=s1, scalar2=s2, op0=op0, op1=op1),
                       rd, wr)

    def stt(self, out, in0, scalar, in1, op0, op1, rd, wr):
        return self.op("dve", lambda e: e.scalar_tensor_tensor(out=out, in0=in0, scalar=scalar, in1=in1,
                                                               op0=op0, op1=op1), rd, wr)

    def copy(self, eng, out, in_, rd, wr):
        if eng == "act":
            return self.op("act", lambda e: e.activation(out=out, in_=in_, func=AF.Copy), rd, wr)
        return self.op(eng, lambda e: e.tensor_copy(out=out, in_=in_), rd, wr)

    def memset(self, eng, out, val, wr):
        return self.op(eng, lambda e: e.memset(out, val), (), wr)

    def emit(self, nc, block, sems, dsems, final_waits):
        for eng in self.ENGS:
            c = [0] * self.nsets
            for op in self.ops[eng]:
                if op.inc and not op.is_dma:
                    c[op.ep] += 1
                    op.val = c[op.ep]
        engobj = {"pe": block.tensor, "act": block.scalar, "dve": block.vector, "pool": block.gpsimd,
                  "sp": block.sync}

        def make(eng):
            ops = self.ops[eng]

            def body(e):
                seen = {}
                for op in ops:
                    for d in op.deps:
                        if d.is_dma:
                            key = ("d", d.dsem)
                            sem = dsems[d.dsem]
                            val = d.dval
                        else:
                            key = ("c", d.eng, d.ep)
                            sem = sems[d.ep][d.eng]
                            val = d.val
                        if seen.get(key, 0) >= val:
                            continue
                        seen[key] = val
                        e.wait_ge(sem, val)
                    ins = op.fn(e)
                    if op.is_dma:
                        ins.then_inc(dsems[op.dsem], 16)
                    elif op.inc:
                        ins.then_inc(sems[op.ep][eng], 1)
                if eng == "sp":
                    for (k, v) in final_waits:
                        e.wait_ge(dsems[k], v)
            return body

        for eng in self.ENGS:
            engobj[eng](make(eng))


def _consts():
    c = {}
    pos = np.arange(S, dtype=np.float64)

    def tab(dim):
        inv = 10000.0 ** (-np.arange(0, dim, 2, dtype=np.float64) / dim)
        C = np.zeros((128, S), np.float32)
        Sg = np.zeros((128, S), np.float32)
        P = np.zeros((128, 128), np.float32)
        for p in range(128):
            i = p % dim
            j = i % (dim // 2)
            ang = (pos.astype(np.float32) * np.float32(inv[j])).astype(np.float32)
            C[p] = np.cos(ang)
            Sg[p] = (-np.sin(ang)) if i < dim // 2 else np.sin(ang)
            src = (p // dim) * dim + (i + dim // 2) % dim
            P[src, p] = 1.0
        return C, Sg, P
    c["C32"], c["S32"], c["P32"] = tab(32)
    c["C64"], c["S64"], c["P64"] = tab(64)
    cm = np.zeros((128, 4, 512), np.float32)
    k = np.arange(128)[:, None]
    q = np.arange(512)[None, :]
    for i in range(4):
        cm[:, i, :] = np.where(i * 128 + k > q, NEG, 0.0)
    c["cmask"] = cm
    qq = np.arange(128)[:, None]
    kk = np.arange(128)[None, :]
    c["cb"] = np.where(kk <= qq, 0.0, -1e30).astype(np.float32)
    lg = np.log1p(-np.exp2(-5.0 - np.arange(4, dtype=np.float64)))
    dec = np.zeros((128, 4, 128), np.float32)
    cc = np.arange(128)[:, None]
    q1 = np.arange(128)[None, :]
    for h in range(4):
        dec[:, (h % 2) * 2 + h // 2, :] = np.where(q1 >= cc, 0.125 * np.exp(np.maximum(q1 - cc, 0) * lg[h]), 0.0)
    c["decT"] = dec
    xi = np.zeros((128, 2, 512), np.float32)
    for i in range(2):
        for r in range(128):
            h = 2 * i + r // 64
            xi[r, i, :] = np.tile(np.exp((np.arange(128) + 1.0) * lg[h]), 4)
    c["xiT"] = xi
    zb = np.zeros((128, 256), np.float32)
    cdb = np.zeros((64, 256), np.float32)
    for h in range(4):
        zb[:, h * 64:(h + 1) * 64] = (0.125 * np.exp((127.0 - np.arange(128)) * lg[h]))[:, None]
        cdb[:, h * 64:(h + 1) * 64] = np.exp(128.0 * lg[h])
    c["zetab"] = zb
    c["cdb"] = cdb
    c["ctab"] = np.broadcast_to((2.0 ** -(np.arange(NIT + 1) + 1.0)).astype(np.float32)[None, :], (128, NIT + 1)).copy()
    c["ident"] = np.eye(128, dtype=np.float32)
    c["ones"] = np.ones((128, 128), np.float32)
    return c


CONST_SHAPES = {
    "C32": [128, S], "S32": [128, S], "P32": [128, 128], "C64": [128, S], "S64": [128, S], "P64": [128, 128],
    "cmask": [128, 4, 512], "cb": [128, 128], "decT": [128, 4, 128], "xiT": [128, 2, 512], "zetab": [128, 256],
    "cdb": [64, 256], "ctab": [128, NIT + 1], "ident": [128, 128], "ones": [128, 128],
}


def build_nc(nseq, nlayers, final_norm, dbg=None):
    from contextlib import ExitStack
    nc = bass.Bass("TRN2", target_bir_lowering=False)
    dr = {}
    dr["x"] = nc.dram_tensor("x", [nseq * S, D], F32, kind="ExternalInput").ap()
    dr["y"] = nc.dram_tensor("y", [nseq * S, D], F32, kind="ExternalOutput").ap()
    dr["w_in"] = nc.dram_tensor("w_in", [nlayers, D, INW], F32, kind="ExternalInput").ap()
    dr["w_uq"] = nc.dram_tensor("w_uq", [nlayers, 768, 768], F32, kind="ExternalInput").ap()
    dr["w_ukv"] = nc.dram_tensor("w_ukv", [nlayers, 256, 1024], F32, kind="ExternalInput").ap()
    dr["w_out"] = nc.dram_tensor("w_out", [nlayers, D, D], F32, kind="ExternalInput").ap()
    dr["w_ff1"] = nc.dram_tensor("w_ff1", [nlayers, D, DFF], F32, kind="ExternalInput").ap()
    dr["w_ff2"] = nc.dram_tensor("w_ff2", [nlayers, DFF, D], F32, kind="ExternalInput").ap()
    dr["gpack"] = nc.dram_tensor("gpack", [nlayers, 128, NG], F32, kind="ExternalInput").ap()
    dr["gfin"] = nc.dram_tensor("gfin", [128, 8], F32, kind="ExternalInput").ap()
    for k, shp in CONST_SHAPES.items():
        dr[k] = nc.dram_tensor("c_" + k, shp, F32, kind="ExternalInput").ap()
    if dbg:
        dr["dbg"] = nc.dram_tensor("dbg", [128, 8, S], F32, kind="ExternalOutput").ap()

    stop = None
    if dbg and ":" in dbg:
        dbg, stop = dbg.split(":")
    sch = Sched()
    with ExitStack() as st:
        ARW = 53000
        arena = st.enter_context(nc.sbuf_tensor("arena", [128, ARW], F32))
        psf = [st.enter_context(nc.psum_tensor("ps%d" % i, [128, 512], F32))[:, :] for i in range(7)]
        psb = st.enter_context(nc.psum_tensor("psb", [128, 1024], BF16))[:, :]
        sems = [{e: st.enter_context(nc.semaphore("sem%d_%s" % (k, e))) for e in Sched.ENGS} for k in range(sch.nsets)]
        dsems = [st.enter_context(nc.semaphore("dsem%d" % i)) for i in range(sch.n_dma_sems)]
        block = st.enter_context(nc.Block())

        class Carver:
            def __init__(self, base=0):
                self.off = base

            def take(self, nbytes):
                o = self.off
                self.off += (nbytes + 3) // 4 * 4
                assert self.off <= ARW * 4, ("SBUF arena overflow", self.off, ARW * 4)
                self.peak = max(getattr(self, "peak", 0), self.off)
                return o

        def view(off, dtype, shape):
            n = 1
            for s_ in shape[1:]:
                n *= s_
            esz = 4 if dtype == F32 else 2
            w0 = off // 4
            nw = (n * esz + 3) // 4
            ap = arena[0:shape[0], w0:w0 + nw]
            if dtype != F32:
                ap = ap.bitcast(dtype)
            if len(shape) == 3:
                ap = ap.rearrange("p (a b) -> p a b", a=shape[1])
            elif len(shape) == 4:
                ap = ap.rearrange("p (a b c) -> p a b c", a=shape[1], b=shape[2])
            return ap

        def alloc(C, dtype, shape):
            n = 1
            for s_ in shape[1:]:
                n *= s_
            return view(C.take(n * (4 if dtype == F32 else 2)), dtype, shape)

        G = Carver()
        xT = alloc(G, F32, [128, 8, S])
        OA = alloc(G, BF16, [128, 4, S])
        ident_b = alloc(G, BF16, [128, 128])
        ident_f = alloc(G, F32, [128, 128])
        ones_b = alloc(G, BF16, [128, 128])
        ones_f = alloc(G, F32, [128, 128])
        P32 = alloc(G, BF16, [128, 128])
        P64 = alloc(G, BF16, [128, 128])
        gp = alloc(G, F32, [128, nlayers, NG])
        gfin = alloc(G, F32, [128, 8])
        PHASE_BASE = G.off

        t_xT = [Tok("xT%d" % c) for c in range(NCH)]
        t_OA = [Tok("OA%d" % c) for c in range(NCH)]
        t_const = Tok("const")
        t_ps = [Tok("ps%d" % i) for i in range(7)]
        t_psb = Tok("psb")

        sch.dma("pool", ident_b, dr["ident"], (), [t_const])
        sch.dma("sp", ident_f, dr["ident"], (), [t_const])
        sch.dma("pool", ones_b, dr["ones"], (), [t_const])
        sch.dma("sp", ones_f, dr["ones"], (), [t_const])
        sch.dma("pool", P32, dr["P32"], (), [t_const])
        sch.dma("pool", P64, dr["P64"], (), [t_const])
        for l in range(nlayers):
            sch.dma("sp", gp[:, l, :], dr["gpack"][l], (), [t_const])
        sch.dma("sp", gfin, dr["gfin"], (), [t_const])

        def gvec(l, which, j):
            base = {"mix": 0, "q": 8, "kv": 14, "mlp": 16}[which]
            return gp[:, l, base + j:base + j + 1]

        def chunk_norm(c, gfun, hT_dst, t_h, sq, t_sq, rb, t_rb, pi):
            cols = slice(c * CH, (c + 1) * CH)
            for kc in range(8):
                sch.act(sq[:, kc % 2, :], xT[:, kc, cols], AF.Square, [t_xT[c]], [t_sq[kc % 2]])
                sch.mm(psf[pi], ones_b, sq[:, kc % 2, :], kc == 0, kc == 7, [t_sq[kc % 2], t_const], [t_ps[pi]])
            sch.act(rb, psf[pi], AF.Ln, [t_ps[pi]], [t_rb], scale=1.0 / D, bias=EPS)
            sch.act(rb, rb, AF.Exp, [t_rb], [t_rb], scale=-0.5)
            for kc in range(8):
                sch.stt(hT_dst[:, kc, :], xT[:, kc, cols], gfun(kc), rb, ALU.mult, ALU.mult,
                        [t_xT[c], t_rb, t_const], [t_h])

        def rope(rows, pin, pr, Ctab, Stab, t_tab, Pm, xb, t_xb, t1, t_t1, t2, t_t2, dst, t_dst,
                 rstd=None, t_rstd=None):
            if rstd is None:
                sch.copy("act", xb[0:rows, :], psf[pin][0:rows, :], [t_ps[pin]], [t_xb])
            else:
                sch.tt("dve", xb[0:rows, :], psf[pin][0:rows, :], rstd[0:rows, :], ALU.mult, [t_ps[pin], t_rstd], [t_xb])
            sch.mm(psf[pr][0:rows, :], Pm[0:rows, 0:rows], xb[0:rows, :], True, True, [t_xb, t_const], [t_ps[pr]])
            sch.tt("dve", t1[0:rows, :], xb[0:rows, :], Ctab[0:rows, :], ALU.mult, [t_xb, t_tab], [t_t1])
            sch.tt("dve", t2[0:rows, :], psf[pr][0:rows, :], Stab[0:rows, :], ALU.mult, [t_ps[pr], t_tab], [t_t2])
            if isinstance(dst, list):
                for (d_ap, r0) in dst:
                    sch.tt("dve", d_ap, t1[r0:r0 + 32, :], t2[r0:r0 + 32, :], ALU.add, [t_t1, t_t2], [t_dst])
            else:
                sch.tt("dve", dst, t1[0:rows, :], t2[0:rows, :], ALU.add, [t_t1, t_t2], [t_dst])

        def run_rope_jobs(jobs, scr):
            jobs[0]["proj"](0)
            for k, jb in enumerate(jobs):
                if k + 1 < len(jobs):
                    jobs[k + 1]["proj"]((k + 1) % 2)
                xb_, t_xb_, t1_, t_t1_, t2_, t_t2_ = scr[k % len(scr)]
                rope(jb["rows"], k % 2, 4 + k % 2, jb["C"], jb["S"], jb["t_tab"], jb["P"], xb_, t_xb_, t1_, t_t1_, t2_, t_t2_,
                     jb["dst"], jb["t_dst"], rstd=jb.get("rstd"), t_rstd=jb.get("t_rstd"))
                if jb.get("post"):
                    jb["post"]()

        for sq_i in range(nseq):
            sch.barrier()
            Lc = Carver(PHASE_BASE)
            xin = [alloc(Lc, F32, [128, D]) for _ in range(2)]
            t_xin = [Tok("xin0"), Tok("xin1")]
            for t in range(NT):
                b = t % 2
                sch.dma("sp", xin[b], dr["x"][sq_i * S + t * 128: sq_i * S + (t + 1) * 128, :], (), [t_xin[b]])
                for half in range(2):
                    for k4 in range(4):
                        kc = half * 4 + k4
                        sch.tr(psf[half][:, k4 * 128:(k4 + 1) * 128], xin[b][:, kc * 128:(kc + 1) * 128], ident_f,
                               [t_xin[b], t_const], [t_ps[half]])
                    for k4 in range(4):
                        kc = half * 4 + k4
                        sch.copy("act" if half == 0 else "dve", xT[:, kc, t * 128:(t + 1) * 128],
                                 psf[half][:, k4 * 128:(k4 + 1) * 128], [t_ps[half]], [t_xT[t // 4]])

            for l in range(nlayers):
                sch.barrier()
                A = Carver(PHASE_BASE)
                Win = alloc(A, BF16, [128, 8, 1056])
                WuqN = alloc(A, BF16, [128, 6, 512])
                WuqR = alloc(A, BF16, [128, 6, 256])
                WukvK = alloc(A, BF16, [128, 2, 512])
                WukvV = alloc(A, BF16, [128, 2, 512])
                KN = alloc(A, BF16, [128, 4, S])
                KR3 = alloc(A, BF16, [128, S])
                VC = alloc(A, BF16, [128, NT, 8, 65])
                hT = alloc(A, BF16, [128, 8, CH])
                sqb = alloc(A, BF16, [128, 2, CH])
                rb = alloc(A, F32, [128, CH])
                rq_b = alloc(A, F32, [128, CH])
                rkv_b = alloc(A, F32, [128, CH])
                cqn = alloc(A, BF16, [128, 6, CH])
                ckvn = alloc(A, BF16, [128, 2, CH])
                tabC = alloc(A, BF16, [128, CH])
                tabS = alloc(A, BF16, [128, CH])
                xb = alloc(A, BF16, [128, CH])
                t1 = alloc(A, BF16, [128, CH])
                t2 = alloc(A, BF16, [128, CH])
                xb2 = alloc(A, BF16, [128, CH])
                t12 = alloc(A, BF16, [128, CH])
                t22 = alloc(A, BF16, [128, CH])
                t_xb2, t_t12, t_t22 = Tok(), Tok(), Tok()
                QN = alloc(A, BF16, [128, 8, CH])
                QR = alloc(A, BF16, [128, 8, CH])
                PT = [alloc(A, BF16, [128, CH]) for _ in range(3)]
                cmask = alloc(A, BF16, [128, 4, CH])
                rec = rb
                pbs = alloc(A, F32, [128, CH])
                rtok = alloc(A, F32, [128, 8])
                t_W = [Tok() for _ in range(8)]; t_WqN = [Tok() for _ in range(6)]; t_WqR = [Tok() for _ in range(6)]; t_WkK = [Tok(), Tok()]; t_WkV = [Tok(), Tok()]
                t_KN = [Tok() for _ in range(NCH)]
                t_KR = [Tok() for _ in range(NCH)]
                t_VC = [Tok() for _ in range(NCH)]
                t_h, t_rb, t_rq, t_rkv, t_cqn, t_ckvn = Tok(), Tok(), Tok(), Tok(), Tok(), Tok()
                t_sq = [Tok(), Tok()]
                t_tab, t_xb, t_t1, t_t2, t_QN, t_QR = Tok(), Tok(), Tok(), Tok(), Tok(), Tok()
                t_PT = [Tok(), Tok(), Tok()]
                t_cm, t_pbs, t_rtok = Tok(), Tok(), Tok()
                t_rec = t_rb

                for kc in range(8):
                    sch.dma("pool", Win[:, kc, :], dr["w_in"][l, kc * 128:(kc + 1) * 128, 0:1056], (), [t_W[kc]])
                for kc in range(6):
                    srcq = dr["w_uq"][l, kc * 128:(kc + 1) * 128, :].rearrange("p (h d) -> p h d", h=8)
                    sch.dma("pool", WuqN[:, kc, :].rearrange("p (h d) -> p h d", h=8), srcq[:, :, 0:64], (), [t_WqN[kc]])
                    sch.dma("pool", WuqR[:, kc, :].rearrange("p (h d) -> p h d", h=8), srcq[:, :, 64:96], (), [t_WqR[kc]])
                for kc in range(2):
                    srck = dr["w_ukv"][l, kc * 128:(kc + 1) * 128, :].rearrange("p (h d) -> p h d", h=8)
                    sch.dma("pool", WukvK[:, kc, :].rearrange("p (h d) -> p h d", h=8), srck[:, :, 0:64], (), [t_WkK[kc]])
                    sch.dma("pool", WukvV[:, kc, :].rearrange("p (h d) -> p h d", h=8), srck[:, :, 64:128], (), [t_WkV[kc]])
                sch.dma("pool", cmask, dr["cmask"], (), [t_cm])
                sch.memset("pool", QN[:, :, :], 0.0, [t_QN])
                sch.memset("pool", QR[:, :, :], 0.0, [t_QR])
                sch.memset("pool", VC[:, :, :, 64:65], 1.0, [t_VC[c] for c in range(NCH)])

                for c in range(NCH):
                    cols = slice(c * CH, (c + 1) * CH)
                    sch.dma("pool", tabC, dr["C32"][:, cols], (), [t_tab])
                    sch.dma("pool", tabS, dr["S32"][:, cols], (), [t_tab])
                    chunk_norm(c, lambda kc: gvec(l, "mix", kc), hT, t_h, sqb, t_sq, rb, t_rb, 0)
                    for j in range(6):
                        pi = j % 2
                        for kc in range(8):
                            sch.mm(psf[pi], Win[:, kc, j * 128:(j + 1) * 128], hT[:, kc, :], kc == 0, kc == 7,
                                   [t_W[kc], t_h], [t_ps[pi]])
                        sch.act(cqn[:, j, :], psf[pi], AF.Copy, [t_ps[pi], t_const], [t_cqn], scale=gvec(l, "q", j))
                        sch.act(sqb[:, j % 2, :], psf[pi], AF.Square, [t_ps[pi]], [t_sq[j % 2]])
                        sch.mm(psf[2], ones_b, sqb[:, j % 2, :], j == 0, j == 5, [t_sq[j % 2], t_const], [t_ps[2]])
                    sch.act(rq_b, psf[2], AF.Ln, [t_ps[2]], [t_rq], scale=1.0 / 768, bias=EPS)
                    sch.act(rq_b, rq_b, AF.Exp, [t_rq], [t_rq], scale=-0.5)
                    for j in range(2):
                        pi = j % 2
                        for kc in range(8):
                            sch.mm(psf[pi], Win[:, kc, 768 + j * 128:768 + (j + 1) * 128], hT[:, kc, :], kc == 0, kc == 7,
                                   [t_W[kc], t_h], [t_ps[pi]])
                        sch.act(ckvn[:, j, :], psf[pi], AF.Copy, [t_ps[pi], t_const], [t_ckvn], scale=gvec(l, "kv", j))
                        sch.act(sqb[:, j % 2, :], psf[pi], AF.Square, [t_ps[pi]], [t_sq[j % 2]])
                        sch.mm(psf[2], ones_b, sqb[:, j % 2, :], j == 0, j == 1, [t_sq[j % 2], t_const], [t_ps[2]])
                    for t in range(4):
                        for j in range(2):
                            sch.mm(psf[3][:, 2 * t:2 * t + 2], sqb[:, j, t * 128:(t + 1) * 128], ones_b[:, 0:2], j == 0, j == 1,
                                   [t_sq[j], t_const], [t_ps[3]])
                    sch.act(rkv_b, psf[2], AF.Ln, [t_ps[2]], [t_rkv], scale=1.0 / 256, bias=EPS)
                    sch.act(rkv_b, rkv_b, AF.Exp, [t_rkv], [t_rkv], scale=-0.5)
                    sch.act(rtok[:, 0:4], psf[3][:, 0:8].rearrange("p (t two) -> p t two", two=2)[:, :, 0], AF.Ln, [t_ps[3]], [t_rtok], scale=1.0 / 256, bias=EPS)
                    sch.act(rtok[:, 0:4], rtok[:, 0:4], AF.Exp, [t_rtok], [t_rtok], scale=-0.5)
                    for i in range(4):
                        pi = i % 2
                        for kc in range(6):
                            sch.mm(psf[pi], WuqN[:, kc, i * 128:(i + 1) * 128], cqn[:, kc, :], kc == 0, kc == 5,
                                   [t_WqN[kc], t_cqn], [t_ps[pi]])
                        sch.tt("dve", QN[0:64, 2 * i, :], psf[pi][0:64, :], rq_b[0:64, :], ALU.mult, [t_ps[pi], t_rq], [t_QN])
                        sch.tt("dve", QN[64:128, 2 * i + 1, :], psf[pi][64:128, :], rq_b[64:128, :], ALU.mult, [t_ps[pi], t_rq], [t_QN])
                    def kr_proj(pi):
                        for kc in range(8):
                            sch.mm(psf[pi][0:32, :], Win[:, kc, 1024:1056], hT[:, kc, :], kc == 0, kc == 7, [t_W[kc], t_h], [t_ps[pi]])

                    def kr_post():
                        sch.copy("pool", KR3[32:64, cols], KR3[0:32, cols], [t_KR[c]], [t_KR[c]])
                        sch.copy("pool", KR3[64:96, cols], KR3[0:32, cols], [t_KR[c]], [t_KR[c]])

                    def mk_qr_proj(g, rows):
                        def f(pi):
                            for kc in range(6):
                                sch.mm(psf[pi][0:rows, :], WuqR[:, kc, g * 96:g * 96 + rows], cqn[:, kc, :], kc == 0, kc == 5,
                                       [t_WqR[kc], t_cqn], [t_ps[pi]])
                        return f
                    jobs = [dict(proj=kr_proj, rows=32, C=tabC, S=tabS, t_tab=t_tab, P=P32, dst=KR3[0:32, cols], t_dst=t_KR[c],
                                 post=kr_post)]
                    for g in range(3):
                        nh = 3 if g < 2 else 2
                        jobs.append(dict(proj=mk_qr_proj(g, nh * 32), rows=nh * 32, C=tabC, S=tabS, t_tab=t_tab, P=P32,
                                         dst=[(QR[32 * k:32 * k + 32, 3 * g + k, :], 32 * k) for k in range(nh)], t_dst=t_QR,
                                         rstd=rq_b, t_rstd=t_rq))
                    run_rope_jobs(jobs, [(xb, t_xb, t1, t_t1, t2, t_t2), (xb2, t_xb2, t12, t_t12, t22, t_t22)])
                    for i in range(4):
                        pi = i % 2
                        for kc in range(2):
                            sch.mm(psf[pi], WukvK[:, kc, i * 128:(i + 1) * 128], ckvn[:, kc, :], kc == 0, kc == 1,
                                   [t_WkK[kc], t_ckvn], [t_ps[pi]])
                        sch.tt("dve", KN[:, i, cols], psf[pi], rkv_b, ALU.mult, [t_ps[pi], t_rkv], [t_KN[c]])
                    for t in range(4):
                        pi = t % 2
                        for kc in range(2):
                            sch.mm(psf[pi], ckvn[:, kc, t * 128:(t + 1) * 128], WukvV[:, kc, :], kc == 0, kc == 1,
                                   [t_WkV[kc], t_ckvn], [t_ps[pi]])
                        sch.ts("dve", VC[:, c * 4 + t, :, 0:64], psf[pi].rearrange("p (h d) -> p h d", h=8),
                               rtok[:, t:t + 1], None, ALU.mult, None, [t_ps[pi], t_rtok], [t_VC[c]])
                    nkb = 4 * (c + 1)
                    steps = [(h, kb) for h in range(8) for kb in range(nkb)]

                    def mla_qk(si):
                        h, kb = steps[si]
                        i, off = h // 2, (h % 2) * 64
                        g, goff = h // 3, (h % 3) * 32
                        pi = 2 + (si % 2)
                        kc_ = kb // 4
                        kcols = slice(kb * 128, (kb + 1) * 128)
                        diag = kb >= 4 * c
                        sch.mm(psf[pi], KN[:, i, kcols], QN[:, h, :], True, False,
                               [t_KN[kc_], t_QN], [t_ps[pi]])
                        sch.mm(psf[pi], KR3[0:96, kcols], QR[0:96, h, :], False, not diag,
                               [t_KR[kc_], t_QR], [t_ps[pi]])
                        if diag:
                            sch.mm(psf[pi], ident_b, cmask[:, kb - 4 * c, :], False, True, [t_cm, t_const], [t_ps[pi]])
                        pt = si % 3
                        sch.act(PT[pt], psf[pi], AF.Exp, [t_ps[pi]], [t_PT[pt]], scale=MLA_SCALE)

                    def mla_pv(si):
                        h, kb = steps[si]
                        po = 4 + (h % 2)
                        pt = si % 3
                        sch.mm(psf[po][0:65, :], VC[:, kb, h, 0:65], PT[pt], kb == 0, kb == nkb - 1,
                               [t_VC[kb // 4], t_PT[pt]], [t_ps[po]])

                    def mla_norm_a(h):
                        po = 4 + (h % 2)
                        sch.op("dve", lambda e, po=po: e.reciprocal(out=rec[64:65, :], in_=psf[po][64:65, :]),
                               [t_ps[po]], [t_rec])

                    def mla_norm_b(h):
                        i, off = h // 2, (h % 2) * 64
                        po = 4 + (h % 2)
                        sch.mm(psf[6][0:64, :], ones_f[64:65, 0:64], rec[64:65, :], True, True, [t_rec, t_const], [t_ps[6]])
                        sch.copy("act", pbs[0:64, :], psf[6][0:64, :], [t_ps[6]], [t_pbs])
                        sch.tt("dve", OA[off:off + 64, i, cols], psf[po][0:64, :], pbs[0:64, :], ALU.mult,
                               [t_ps[po], t_pbs], [t_OA[c]])

                    mla_qk(0)
                    pend = None
                    for si in range(len(steps)):
                        if si + 1 < len(steps):
                            mla_qk(si + 1)
                        mla_pv(si)
                        if pend is not None:
                            pend[1] -= 1
                            if pend[1] == 0:
                                mla_norm_b(pend[0])
                                pend = None
                        h, kb = steps[si]
                        if kb == nkb - 1:
                            if pend is not None:
                                mla_norm_b(pend[0])
                            mla_norm_a(h)
                            pend = [h, 2]
                    if pend is not None:
                        mla_norm_b(pend[0])

                if dbg == "p1":
                    break
                sch.barrier()
                A = Carver(PHASE_BASE)
                Vbf = alloc(A, BF16, [128, 256])
                Vz = alloc(A, BF16, [128, 256])
                Gs = alloc(A, F32, [128, 256])
                Ktok = alloc(A, BF16, [128, 256])
                AT = alloc(A, BF16, [128, 4, 128])
                rsb = alloc(A, F32, [128, 256])
                oct_ = alloc(A, BF16, [128, 256])
                bst = alloc(A, F32, [128, 4, 6])
                bag = alloc(A, F32, [128, 4, 2])
                Win = alloc(A, BF16, [128, 8, 1704])
                Wout = alloc(A, BF16, [128, 8, D])
                KB2 = alloc(A, BF16, [128, S])
                KI3 = alloc(A, BF16, [128, S])
                VB = alloc(A, BF16, [128, NT, 65])
                hT = alloc(A, BF16, [128, 8, CH])
                rb = alloc(A, F32, [128, CH])
                tC32 = alloc(A, BF16, [128, CH])
                tS32 = alloc(A, BF16, [128, CH])
                tC64 = alloc(A, BF16, [128, CH])
                tS64 = alloc(A, BF16, [128, CH])
                xb = alloc(A, BF16, [128, CH])
                t1 = alloc(A, BF16, [128, CH])
                t2 = alloc(A, BF16, [128, CH])
                QB = alloc(A, BF16, [128, 2, CH])
                QI = alloc(A, BF16, [128, 3, CH])
                WI = alloc(A, F32, [128, 4, 8])
                RQ = alloc(A, BF16, [128, 2, CH])
                RQX = alloc(A, BF16, [128, 2, CH])
                RK = alloc(A, BF16, [128, 2, CH])
                xiT = alloc(A, F32, [128, 2, 128])
                decT = alloc(A, F32, [128, 4, 128])
                zetab = alloc(A, F32, [128, 256])
                cdb = alloc(A, F32, [128, 256])
                cb = alloc(A, F32, [128, 128])
                ctab = alloc(A, F32, [128, NIT + 1])
                gret = gp[:, l, 24:280]
                OT = alloc(A, BF16, [128, 4, CH])
                Ibuf = alloc(A, F32, [128, S])
                Rtmp2 = alloc(A, BF16, [128, 2, CH])
                Rtmp = [Rtmp2[:, 0, :], Rtmp2[:, 1, :]]
                sqb = Rtmp2
                Mb = alloc(A, BF16, [128, S])
                junk = Mb
                MbT = [alloc(A, BF16, [128, NT, 128]) for _ in range(2)]
                PTd = [alloc(A, BF16, [128, 4, 128]) for _ in range(2)]
                smA = alloc(A, F32, [128, 2])
                smR = alloc(A, F32, [128, 4])
                OBt = alloc(A, BF16, [128, 256])
                sm = alloc(A, F32, [128, 64])
                wtab = alloc(A, F32, [128, NIT + 1])
                Rst = alloc(A, F32, [128, 256])
                Rbf = alloc(A, BF16, [128, 256])
                t_W = [Tok() for _ in range(8)]; t_Wo = [Tok() for _ in range(8)]; t_cst = Tok()
                t_KB = [Tok() for _ in range(NCH)]
                t_KI = [Tok() for _ in range(NCH)]
                t_VB = [Tok() for _ in range(NCH)]
                t_h, t_rb = Tok(), Tok()
                t_tab, t_xb, t_t1, t_t2 = Tok(), Tok(), Tok(), Tok()
                t_QB, t_QI, t_WI, t_RQ, t_RQX, t_RK, t_OT = Tok(), Tok(), Tok(), Tok(), Tok(), Tok(), Tok()
                t_I, t_junk, t_Mb, t_OBt, t_sm, t_wtab = Tok(), Tok(), Tok(), Tok(), Tok(), Tok()
                t_MbT = [Tok(), Tok()]
                t_smA, t_smR = Tok(), Tok()
                t_Rtmp = [Tok(), Tok()]
                t_sq = t_Rtmp
                t_junk = t_Mb
                t_PTd = [Tok(), Tok()]
                t_Rst, t_Rbf, t_Vbf, t_Vz, t_Gs, t_Ktok, t_AT, t_rsb, t_oct, t_bst = (Tok() for _ in range(10))

                for kc in range(8):
                    sch.dma("pool", Win[:, kc, :], dr["w_in"][l, kc * 128:(kc + 1) * 128, 1056:2760], (), [t_W[kc]])
                for kc in range(8):
                    sch.dma("pool", Wout[:, kc, :], dr["w_out"][l, kc * 128:(kc + 1) * 128, :], (), [t_Wo[kc]])
                sch.dma("sp", xiT, dr["xiT"][:, :, 0:128], (), [t_cst])
                sch.dma("sp", decT, dr["decT"], (), [t_cst])
                sch.dma("sp", zetab, dr["zetab"], (), [t_cst])
                sch.dma("sp", cdb[0:64, :], dr["cdb"], (), [t_cst])
                sch.dma("sp", cb, dr["cb"], (), [t_cst])
                sch.dma("sp", ctab, dr["ctab"], (), [t_cst])
                sch.memset("pool", VB[:, :, 64:65], 1.0, [t_VB[c] for c in range(NCH)])
                sch.memset("pool", Rst[:, :], 0.0, [t_Rst])
                sch.memset("pool", Rbf[:, :], 0.0, [t_Rbf])

                def wc(a, b):
                    return slice(a - 1056, b - 1056)

                for c in range(NCH):
                    cols = slice(c * CH, (c + 1) * CH)
                    sch.dma("pool", tC32, dr["C32"][:, cols], (), [t_tab])
                    sch.dma("pool", tS32, dr["S32"][:, cols], (), [t_tab])
                    sch.dma("pool", tC64, dr["C64"][:, cols], (), [t_tab])
                    sch.dma("pool", tS64, dr["S64"][:, cols], (), [t_tab])
                    chunk_norm(c, lambda kc: gvec(l, "mix", kc), hT, t_h, sqb, t_sq, rb, t_rb, 0)

                    def proj(pi, c0, c1, rows):
                        for kc in range(8):
                            sch.mm(psf[pi][0:rows, :], Win[:, kc, wc(c0, c1)], hT[:, kc, :], kc == 0, kc == 7,
                                   [t_W[kc], t_h], [t_ps[pi]])
                    def mkproj(c0, c1, rows):
                        def f(pi):
                            for kc in range(8):
                                sch.mm(psf[pi][0:rows, :], Win[:, kc, wc(c0, c1)], hT[:, kc, :], kc == 0, kc == 7,
                                       [t_W[kc], t_h], [t_ps[pi]])
                        return f
                    jobs = []
                    for i in range(2):
                        jobs.append(dict(proj=mkproj(1056 + 128 * i, 1056 + 128 * (i + 1), 128), rows=128, C=tC64, S=tS64, t_tab=t_tab,
                                         P=P64, dst=QB[:, i, :], t_dst=t_QB))
                    jobs.append(dict(proj=mkproj(1312, 1376, 64), rows=64, C=tC64, S=tS64, t_tab=t_tab, P=P64,
                                     dst=KB2[0:64, cols], t_dst=t_KB[c],
                                     post=lambda: sch.copy("pool", KB2[64:128, cols], KB2[0:64, cols], [t_KB[c]], [t_KB[c]])))
                    for g in range(3):
                        nh = 3 if g < 2 else 2
                        jobs.append(dict(proj=mkproj(1440 + 96 * g, 1440 + 96 * g + 32 * nh, 32 * nh), rows=32 * nh, C=tC32, S=tS32,
                                         t_tab=t_tab, P=P32, dst=QI[0:32 * nh, g, :], t_dst=t_QI))

                    def ki_post():
                        sch.copy("pool", KI3[32:64, cols], KI3[0:32, cols], [t_KI[c]], [t_KI[c]])
                        sch.copy("pool", KI3[64:96, cols], KI3[0:32, cols], [t_KI[c]], [t_KI[c]])
                    jobs.append(dict(proj=mkproj(1696, 1728, 32), rows=32, C=tC32, S=tS32, t_tab=t_tab, P=P32,
                                     dst=KI3[0:32, cols], t_dst=t_KI[c], post=ki_post))
                    for i in range(2):
                        jobs.append(dict(proj=mkproj(1736 + 128 * i, 1736 + 128 * (i + 1), 128), rows=128, C=tC64, S=tS64, t_tab=t_tab,
                                         P=P64, dst=RQ[:, i, :], t_dst=t_RQ))
                        jobs.append(dict(proj=mkproj(1992 + 128 * i, 1992 + 128 * (i + 1), 128), rows=128, C=tC64, S=tS64, t_tab=t_tab,
                                         P=P64, dst=RK[:, i, :], t_dst=t_RK))
                    run_rope_jobs(jobs, [(xb, t_xb, t1, t_t1, t2, t_t2)])
                    if stop == "proj2":
                        break
                    for t in range(4):
                        tcols = slice(t * 128, (t + 1) * 128)
                        for kc in range(8):
                            sch.mm(psf[2][:, 0:64], hT[:, kc, tcols], Win[:, kc, wc(1376, 1440)], kc == 0, kc == 7,
                                   [t_W[kc], t_h], [t_ps[2]])
                        for kc in range(8):
                            sch.mm(psf[2][:, 64:128], hT[:, kc, tcols], Win[:, kc, wc(1728, 1792)], kc == 0, kc == 7,
                                   [t_W[kc], t_h], [t_ps[2]])
                        sch.copy("act", VB[:, c * 4 + t, 0:64], psf[2][:, 0:64], [t_ps[2]], [t_VB[c]])
                        sch.act(WI[:, t, :], psf[2][:, 64:72], AF.Copy, [t_ps[2]], [t_WI], scale=IDX_SCALE)

                    if stop == "proj":
                        break
                    def dsa_idx(j):
                        jt = c * 4 + j
                        qcols = slice(j * 128, (j + 1) * 128)
                        W = 128 * (jt + 1)
                        ngrp = (W + 511) // 512
                        n = 0
                        for h in range(8):
                            g, goff = h // 3, (h % 3) * 32
                            for kg in range(ngrp):
                                k0, k1 = kg * 512, min(W, kg * 512 + 512)
                                pi = 2 + n % 2
                                rt = n % 2
                                n += 1
                                sch.mm(psf[pi][:, 0:k1 - k0], QI[goff:goff + 32, g, qcols], KI3[goff:goff + 32, k0:k1], True, True,
                                       [t_QI] + [t_KI[k0 // 512]], [t_ps[pi]])
                                sch.act(Rtmp[rt][:, 0:k1 - k0], psf[pi][:, 0:k1 - k0], AF.Relu, [t_ps[pi]], [t_Rtmp[rt]])
                                if h == 0:
                                    sch.ts("dve", Ibuf[:, k0:k1], Rtmp[rt][:, 0:k1 - k0], WI[:, j, 0:1], None, ALU.mult, None,
                                           [t_Rtmp[rt], t_WI], [t_I])
                                else:
                                    sch.stt(Ibuf[:, k0:k1], Rtmp[rt][:, 0:k1 - k0], WI[:, j, h:h + 1], Ibuf[:, k0:k1],
                                            ALU.mult, ALU.add, [t_Rtmp[rt], t_WI, t_I], [t_I])

                    def dsa_bis(j):
                        jt = c * 4 + j
                        W = 128 * (jt + 1)
                        sch.op("dve", lambda e, W=W: e.tensor_reduce(out=sm[:, 0:1], in_=Ibuf[:, 0:W], axis=AX.X, op=ALU.max,
                                                                      apply_absolute_value=True), [t_I], [t_sm])
                        sch.ts("dve", sm[:, 1:2], sm[:, 0:1], 2.0, 2.0, ALU.mult, ALU.add, [t_sm], [t_sm])
                        sch.ts("dve", wtab[:, :], ctab[:, :], sm[:, 1:2], None, ALU.mult, None, [t_sm, t_cst], [t_wtab])
                        sch.tt("dve", Ibuf[:, W - 128:W], Ibuf[:, W - 128:W], cb[:, :], ALU.add, [t_I, t_cst], [t_I])
                        sch.memset("dve", sm[:, 2:3], 0.0, [t_sm])
                        for it in range(NIT):
                            sch.ts("dve", junk[:, 0:W], Ibuf[:, 0:W], sm[:, 2:3], 0.0, ALU.is_ge, ALU.add, [t_I, t_sm], [t_junk, t_sm],
                                   accum=sm[:, 3:4])
                            sch.ts("dve", sm[:, 4:5], sm[:, 3:4], 255.5, 0.5, ALU.is_ge, ALU.subtract, [t_sm], [t_sm])
                            sch.stt(sm[:, 2:3], sm[:, 4:5], wtab[:, it:it + 1], sm[:, 2:3], ALU.mult, ALU.add, [t_sm, t_wtab], [t_sm])
                        sch.tt("dve", sm[:, 5:6], sm[:, 2:3], wtab[:, NIT:NIT + 1], ALU.subtract, [t_sm, t_wtab], [t_sm])
                        sch.ts("dve", Mb[:, 0:W], Ibuf[:, 0:W], sm[:, 5:6], NEG, ALU.is_lt, ALU.mult, [t_I, t_sm], [t_Mb])

                    def dsa_tr(j):
                        jt = c * 4 + j
                        mb = jt % 2
                        for kb0 in range(0, jt + 1, 8):
                            nb = min(8, jt + 1 - kb0)
                            for ii in range(nb):
                                kb = kb0 + ii
                                sch.tr(psb[:, ii * 128:(ii + 1) * 128], Mb[:, kb * 128:(kb + 1) * 128], ident_b,
                                       [t_Mb, t_const], [t_psb])
                            sch.copy("act", MbT[mb][:, kb0:kb0 + nb, :], psb[:, 0:nb * 128].rearrange("p (a b) -> p a b", a=nb),
                                     [t_psb], [t_MbT[mb]])

                    def dsa_attn(j):
                        jt = c * 4 + j
                        mb = jt % 2
                        qcols = slice(j * 128, (j + 1) * 128)
                        steps = [(h, kb0) for h in range(4) for kb0 in range(0, jt + 1, 4)]

                        def qk(si):
                            h, kb0 = steps[si]
                            i, off = h // 2, (h % 2) * 64
                            nb = min(4, jt + 1 - kb0)
                            pi = si % 2
                            for ii in range(nb):
                                kb = kb0 + ii
                                sch.mm(psf[pi][:, ii * 128:(ii + 1) * 128], KB2[off:off + 64, kb * 128:(kb + 1) * 128],
                                       QB[off:off + 64, i, qcols], True, False, [t_KB[kb // 4], t_QB], [t_ps[pi]])
                                sch.mm(psf[pi][:, ii * 128:(ii + 1) * 128], ident_b, MbT[mb][:, kb, :], False, True,
                                       [t_MbT[mb], t_const], [t_ps[pi]])
                            sch.act(PTd[pi][:, 0:nb, :], psf[pi][:, 0:nb * 128].rearrange("p (a b) -> p a b", a=nb), AF.Exp,
                                    [t_ps[pi]], [t_PTd[pi]], scale=DSA_SCALE)

                        def pv(si):
                            h, kb0 = steps[si]
                            nb = min(4, jt + 1 - kb0)
                            pi = si % 2
                            po = 4 + (h % 2)
                            for ii in range(nb):
                                kb = kb0 + ii
                                sch.mm(psf[po][:, 0:65], PTd[pi][:, ii, :], VB[:, kb, 0:65], kb == 0, kb == jt,
                                       [t_PTd[pi], t_VB[kb // 4]], [t_ps[po]])
                            if kb0 + nb == jt + 1:
                                sch.op("dve", lambda e, po=po: e.reciprocal(out=smA[:, 0:1], in_=psf[po][:, 64:65]), [t_ps[po]], [t_smA])
                                sch.ts("dve", OBt[:, h * 64:(h + 1) * 64], psf[po][:, 0:64], smA[:, 0:1], None, ALU.mult, None,
                                       [t_ps[po], t_smA], [t_OBt])

                        qk(0)
                        for si in range(len(steps)):
                            if si + 1 < len(steps):
                                qk(si + 1)
                            pv(si)
                        for i in range(2):
                            sch.tr(psb[:, i * 128:(i + 1) * 128], OBt[:, i * 128:(i + 1) * 128], ident_b, [t_OBt, t_const], [t_psb])
                        sch.copy("act", OT[:, 0:2, qcols], psb[:, 0:256].rearrange("p (a b) -> p a b", a=2), [t_psb], [t_OT])

                    def ret_tile(j):
                        qcols = slice(j * 128, (j + 1) * 128)
                        for i in range(2):
                            sch.tt("pool", RQX[:, i, qcols], RQ[:, i, qcols], xiT[:, i, :], ALU.mult, [t_RQ, t_cst], [t_RQX])
                        for kc in range(8):
                            sch.mm(psf[6], hT[:, kc, qcols], Win[:, kc, wc(2248, 2760)], kc == 0, kc == 7, [t_W[kc], t_h], [t_ps[6]])
                        sch.copy("act", Vbf[:, :], psf[6][:, 0:256], [t_ps[6]], [t_Vbf])
                        sch.tt("pool", Vz[:, :], Vbf[:, :], zetab[:, :], ALU.mult, [t_Vbf, t_cst], [t_Vz])
                        sch.act(Gs[:, :], psf[6][:, 256:512], AF.Exp, [t_ps[6]], [t_Gs], scale=-1.0)
                        sch.ts("pool", Gs[:, :], Gs[:, :], 1.0, None, ALU.add, None, [t_Gs], [t_Gs])
                        sch.op("dve", lambda e: e.reciprocal(out=Gs[:, :], in_=Gs[:, :]), [t_Gs], [t_Gs])
                        sch.tt("dve", Gs[:, :], Gs[:, :], psf[6][:, 256:512], ALU.mult, [t_Gs, t_ps[6]], [t_Gs])
                        for i in range(2):
                            sch.tr(psb[:, i * 128:(i + 1) * 128], RK[:, i, qcols], ident_b, [t_RK, t_const], [t_psb])
                        sch.copy("act", Ktok[:, :], psb[:, 0:256], [t_psb], [t_Ktok])
                        for h in range(4):
                            i, off = h // 2, (h % 2) * 64
                            sch.mm(psf[2 + h % 2][:, (h // 2) * 128:(h // 2 + 1) * 128], RK[off:off + 64, i, qcols],
                                   RQ[off:off + 64, i, qcols], True, True, [t_RK, t_RQ], [t_ps[2 + h % 2]])
                        for par in range(2):
                            sch.tt("dve", AT[:, 2 * par:2 * par + 2, :], psf[2 + par][:, 0:256].rearrange("p (a b) -> p a b", a=2),
                                   decT[:, 2 * par:2 * par + 2, :], ALU.mult, [t_ps[2 + par], t_cst], [t_AT])
                        for h in range(4):
                            i, off = h // 2, (h % 2) * 64
                            sch.mm(psf[3][:, h * 64:(h + 1) * 64], AT[:, (h % 2) * 2 + h // 2, :], Vbf[:, h * 64:(h + 1) * 64], True, False,
                                   [t_AT, t_Vbf], [t_ps[3]])
                            sch.mm(psf[3][:, h * 64:(h + 1) * 64], RQX[off:off + 64, i, qcols], Rbf[off:off + 64, h * 64:(h + 1) * 64],
                                   False, True, [t_RQX, t_Rbf], [t_ps[3]])
                        for h in range(4):
                            sch.mm(psf[6][0:64, h * 64:(h + 1) * 64], Ktok[:, h * 64:(h + 1) * 64], Vz[:, h * 64:(h + 1) * 64], True, True,
                                   [t_Ktok, t_Vz], [t_ps[6]])
                        sch.tt("pool", Rst[0:64, :], Rst[0:64, :], cdb[0:64, :], ALU.mult, [t_Rst, t_cst], [t_Rst])
                        sch.tt("dve", Rst[0:64, :], Rst[0:64, :], psf[6][0:64, 0:256], ALU.add, [t_Rst, t_ps[6]], [t_Rst])
                        sch.copy("act", Rbf[0:64, :], Rst[0:64, :], [t_Rst], [t_Rbf])
                        sch.copy("act", Rbf[64:128, :], Rst[0:64, :], [t_Rst], [t_Rbf])
                        sch.copy("act", rsb[:, :], psf[3][:, 0:256], [t_ps[3]], [t_rsb])
                        for h in range(4):
                            sch.op("dve", lambda e, h=h: e.bn_stats(out=bst[:, h, :], in_=rsb[:, h * 64:(h + 1) * 64]), [t_rsb], [t_bst])
                            sch.op("dve", lambda e, h=h: e.bn_aggr(out=bag[:, h, :], in_=bst[:, h, :]), [t_bst], [t_bst])
                        sch.act(smR[:, 0:4], bag[:, :, 1], AF.Sqrt, [t_bst], [t_smR], bias=EPS)
                        sch.op("dve", lambda e: e.reciprocal(out=smR[:, 0:4], in_=smR[:, 0:4]), [t_smR], [t_smR])
                        for h in range(4):
                            sch.ts("dve", rsb[:, h * 64:(h + 1) * 64], rsb[:, h * 64:(h + 1) * 64], bag[:, h, 0:1], smR[:, h:h + 1],
                                   ALU.subtract, ALU.mult, [t_rsb, t_bst, t_smR], [t_rsb])
                        sch.tt("pool", rsb[:, :], rsb[:, :], gret, ALU.mult, [t_rsb, t_const], [t_rsb])
                        sch.tt("pool", oct_[:, :], rsb[:, :], Gs[:, :], ALU.mult, [t_rsb, t_Gs], [t_oct])
                        for i in range(2):
                            sch.tr(psb[:, i * 128:(i + 1) * 128], oct_[:, i * 128:(i + 1) * 128], ident_b, [t_oct, t_const], [t_psb])
                        sch.copy("act", OT[:, 2:4, qcols], psb[:, 0:256].rearrange("p (a b) -> p a b", a=2), [t_psb], [t_OT])

                    prev = None
                    for j in range(4):
                        dsa_idx(j)
                        dsa_bis(j)
                        if prev is not None:
                            dsa_attn(prev)
                        ret_tile(j)
                        dsa_tr(j)
                        prev = j
                    dsa_attn(prev)

                    for oc in range(8):
                        pi = oc % 2
                        for mc in range(8):
                            if mc < 4:
                                sch.mm(psf[pi], Wout[:, mc, oc * 128:(oc + 1) * 128], OA[:, mc, cols], mc == 0, False,
                                       [t_Wo[mc], t_OA[c]], [t_ps[pi]])
                            else:
                                sch.mm(psf[pi], Wout[:, mc, oc * 128:(oc + 1) * 128], OT[:, mc - 4, :], False, mc == 7,
                                       [t_Wo[mc], t_OT], [t_ps[pi]])
                        sch.tt("dve", xT[:, oc, cols], xT[:, oc, cols], psf[pi], ALU.add, [t_xT[c], t_ps[pi]], [t_xT[c]])

                if dbg == "p2":
                    break
                sch.barrier()
                A = Carver(PHASE_BASE)
                hfT = alloc(A, BF16, [128, 8, S])
                W1 = [alloc(A, BF16, [128, 8, 1024]) for _ in range(2)]
                W2 = [alloc(A, BF16, [128, 8, D]) for _ in range(2)]
                hid = [alloc(A, BF16, [128, 8, CH]) for _ in range(2)]
                rl = [alloc(A, BF16, [128, CH]) for _ in range(2)]
                sqb = alloc(A, BF16, [128, 2, CH])
                rb = alloc(A, F32, [128, CH])
                t_hf = [Tok() for _ in range(NCH)]
                t_W1 = [[Tok() for _ in range(8)] for _ in range(2)]
                t_W2 = [[Tok() for _ in range(8)] for _ in range(2)]
                t_hid = [Tok(), Tok()]
                t_rl = [Tok(), Tok()]
                t_sq = [Tok(), Tok()]
                t_rb = Tok()

                def load_q(qr):
                    b = qr % 2
                    for kc in range(8):
                        sch.dma("pool", W1[b][:, kc, :], dr["w_ff1"][l, kc * 128:(kc + 1) * 128, qr * 1024:(qr + 1) * 1024], (), [t_W1[b][kc]])
                    for fc in range(8):
                        sch.dma("pool", W2[b][:, fc, :], dr["w_ff2"][l, qr * 1024 + fc * 128: qr * 1024 + (fc + 1) * 128, :], (), [t_W2[b][fc]])

                load_q(0)
                for c in range(NCH):
                    chunk_norm(c, lambda kc: gvec(l, "mlp", kc), hfT[:, :, c * CH:(c + 1) * CH], t_hf[c], sqb, t_sq, rb, t_rb, 0)
                for qr in range(4):
                    b = qr % 2
                    if qr + 1 < 4:
                        load_q(qr + 1)
                    for c in range(NCH):
                        cols = slice(c * CH, (c + 1) * CH)
                        hb = c % 2
                        for fc in range(8):
                            pi = fc % 2
                            for kc in range(8):
                                sch.mm(psf[pi], W1[b][:, kc, fc * 128:(fc + 1) * 128], hfT[:, kc, cols], kc == 0, kc == 7,
                                       [t_W1[b][kc], t_hf[c]], [t_ps[pi]])
                            sch.act(rl[pi], psf[pi], AF.Relu, [t_ps[pi]], [t_rl[pi]])
                            sch.tt("pool", hid[hb][:, fc, :], rl[pi], rl[pi], ALU.mult, [t_rl[pi]], [t_hid[hb]])
                        for oc in range(8):
                            pi = 2 + oc % 2
                            for fc in range(8):
                                sch.mm(psf[pi], W2[b][:, fc, oc * 128:(oc + 1) * 128], hid[hb][:, fc, :], fc == 0, fc == 7,
                                       [t_W2[b][fc], t_hid[hb]], [t_ps[pi]])
                            sch.tt("dve", xT[:, oc, cols], xT[:, oc, cols], psf[pi], ALU.add, [t_xT[c], t_ps[pi]], [t_xT[c]])

            sch.barrier()
            Lc = Carver(PHASE_BASE)
            yT = alloc(Lc, F32, [128, 8, CH])
            yo = [alloc(Lc, F32, [128, D]) for _ in range(2)]
            sqb = alloc(Lc, BF16, [128, 2, CH])
            rb = alloc(Lc, F32, [128, CH])
            t_y, t_rb = Tok(), Tok()
            t_sq = [Tok(), Tok()]
            t_yo = [Tok(), Tok()]
            for c in range(NCH):
                cols = slice(c * CH, (c + 1) * CH)
                if dbg:
                    src = None
                if final_norm:
                    for kc in range(8):
                        sch.act(sqb[:, kc % 2, :], xT[:, kc, cols], AF.Square, [t_xT[c]], [t_sq[kc % 2]])
                        sch.mm(psf[0], ones_b, sqb[:, kc % 2, :], kc == 0, kc == 7, [t_sq[kc % 2], t_const], [t_ps[0]])
                    sch.act(rb, psf[0], AF.Ln, [t_ps[0]], [t_rb], scale=1.0 / D, bias=EPS)
                    sch.act(rb, rb, AF.Exp, [t_rb], [t_rb], scale=-0.5)
                    for kc in range(8):
                        sch.stt(yT[:, kc, :], xT[:, kc, cols], gfin[:, kc:kc + 1], rb, ALU.mult, ALU.mult,
                                [t_xT[c], t_rb, t_const], [t_y])
                    srcT = lambda kc, t: yT[:, kc, t * 128:(t + 1) * 128]
                    t_src = t_y
                else:
                    srcT = lambda kc, t, c=c: xT[:, kc, c * CH + t * 128: c * CH + (t + 1) * 128]
                    t_src = t_xT[c]
                for t in range(4):
                    tt_ = c * 4 + t
                    b = tt_ % 2
                    for half in range(2):
                        pi = 1 + half
                        for k4 in range(4):
                            kc = half * 4 + k4
                            sch.tr(psf[pi][:, k4 * 128:(k4 + 1) * 128], srcT(kc, t), ident_f, [t_src, t_const], [t_ps[pi]])
                        sch.copy("act" if half == 0 else "dve", yo[b][:, half * 512:(half + 1) * 512], psf[pi], [t_ps[pi]], [t_yo[b]])
                    sch.dma("sp", dr["y"][sq_i * S + tt_ * 128: sq_i * S + (tt_ + 1) * 128, :], yo[b], [t_yo[b]], [t_yo[b]])

        if dbg:
            sch.barrier()
            if dbg == "p1":
                Lc = Carver(PHASE_BASE)
                tmp = alloc(Lc, F32, [128, 4, S])
                tk = Tok()
                for i in range(4):
                    sch.copy("dve", tmp[:, i, :], OA[:, i, :], [t_OA[c] for c in range(NCH)], [tk])
                sch.dma("sp", dr["dbg"][:, 0:4, :], tmp, [tk], [tk])
            else:
                sch.dma("sp", dr["dbg"], xT, [t_xT[c] for c in range(NCH)], [Tok()])
        final_waits = [(k, sch.dma_cnt[k]) for k in range(sch.n_dma_sems) if sch.dma_cnt[k] > 0]
        sch.emit(nc, block, sems, dsems, final_waits)
    return nc


NCORES = 8
_CACHE = {}


def _gpack(g_mix, g_q, g_kv, g_mlp, g_ret):
    L = g_mix.shape[0]
    out = np.zeros((L, 128, NG), np.float32)
    for l in range(L):
        out[l, :, 0:8] = np.asarray(g_mix[l], np.float32).reshape(8, 128).T
        out[l, :, 8:14] = np.asarray(g_q[l], np.float32).reshape(6, 128).T
        out[l, :, 14:16] = np.asarray(g_kv[l], np.float32).reshape(2, 128).T
        out[l, :, 16:24] = np.asarray(g_mlp[l], np.float32).reshape(8, 128).T
        out[l, :, 24:280] = np.broadcast_to(np.asarray(g_ret[l], np.float32)[None, :], (128, 256))
    return out


def run_layers(x, layers, final_norm, weights, g_final, ncores=NCORES, nseq=None, dbg=None):
    B = x.shape[0]
    nseq = B // ncores if nseq is None else nseq
    L = len(layers)
    key = (nseq, L, final_norm, dbg)
    if key not in _CACHE:
        _CACHE[key] = build_nc(nseq, L, final_norm, dbg)
    nc = _CACHE[key]
    consts = _consts()
    common = {"c_" + k: np.ascontiguousarray(v, dtype=np.float32) for k, v in consts.items()}
    for nm in ("w_in", "w_uq", "w_ukv", "w_out", "w_ff1", "w_ff2"):
        common[nm] = np.ascontiguousarray(np.asarray(weights[nm], np.float32)[layers])
    common["gpack"] = _gpack(*[np.asarray(weights[n])[layers] for n in ("g_mix", "g_q", "g_kv", "g_mlp", "g_ret")])
    common["gfin"] = np.ascontiguousarray(np.asarray(g_final, np.float32).reshape(8, 128).T)
    in_maps = []
    for ci in range(ncores):
        m = dict(common)
        m["x"] = np.ascontiguousarray(np.asarray(x[ci * nseq:(ci + 1) * nseq], np.float32).reshape(nseq * S, D))
        in_maps.append(m)
    res = run_bass_kernel_spmd(nc, in_maps, core_ids=list(range(ncores)))
    y = np.concatenate([r["y"].reshape(nseq, S, D) for r in res.results], axis=0)
    if dbg:
        return y, [r["dbg"] for r in res.results]
    return y


FUSED = True


def kernel(x, g_mix, w_in, g_q, w_uq, g_kv, w_ukv, g_ret, w_out, g_mlp, w_ff1, w_ff2, g_final):
    weights = dict(g_mix=g_mix, w_in=w_in, g_q=g_q, w_uq=w_uq, g_kv=g_kv, w_ukv=w_ukv, g_ret=g_ret, w_out=w_out,
                   g_mlp=g_mlp, w_ff1=w_ff1, w_ff2=w_ff2)
    x = np.asarray(x, np.float32)
    depth = np.asarray(w_in).shape[0]
    if FUSED:
        return run_layers(x, list(range(depth)), True, weights, g_final).astype(np.float32)
    y = x
    for l in range(depth):
        y = run_layers(y, [l], l == depth - 1, weights, g_final)
    return y.astype(np.float32)
```

```python
import numpy as np
import concourse.bass as bass
import concourse.mybir as mybir
from concourse.bass_utils import run_bass_kernel_spmd

F32 = mybir.dt.float32
BF16 = mybir.dt.bfloat16
AF = mybir.ActivationFunctionType
ALU = mybir.AluOpType
AX = mybir.AxisListType

D = 1024
S = 2048
NCH = 4
CH = 512
NT = 16
INW = 2760
DFF = 4096
EPS = 1e-6
NIT = 13
MLA_SCALE = 96 ** -0.5
DSA_SCALE = 64 ** -0.5
IDX_SCALE = (8 ** -0.5) * (32 ** -0.5)
NEG = -30000.0
NG = 8 + 6 + 2 + 8 + 256


class Tok:
    __slots__ = ("name", "lw", "rds")

    def __init__(self, name=""):
        self.name = name
        self.lw = None
        self.rds = []


class Op:
    __slots__ = ("eng", "fn", "deps", "inc", "val", "dsem", "dval", "is_dma", "ep")


class Sched:
    ENGS = ("pe", "act", "dve", "pool", "sp")

    def __init__(self, n_dma_sems=24):
        self.ops = {e: [] for e in self.ENGS}
        self.n_dma_sems = n_dma_sems
        self.dma_last = [None] * n_dma_sems
        self.dma_cnt = [0] * n_dma_sems
        self.dma_rr = 0
        self.pending = {e: [] for e in self.ENGS}
        self.epoch = 0
        self.nsets = 12

    def barrier(self):
        lasts = []
        for e in self.ENGS:
            for op in reversed(self.ops[e]):
                if not op.is_dma:
                    lasts.append(op)
                    break
        for d in self.dma_last:
            if d is not None:
                lasts.append(d)
        for e in self.ENGS:
            self.pending[e] = list(lasts)
        self.epoch += 1

    def _rec(self, eng, fn, rd, wr, is_dma=False):
        op = Op()
        op.eng = eng
        op.fn = fn
        op.inc = False
        op.val = None
        op.is_dma = is_dma
        op.ep = self.epoch % self.nsets
        op.dsem = None
        op.dval = None
        deps = list(self.pending[eng])
        self.pending[eng] = []
        for t in rd:
            if t.lw is not None:
                deps.append(t.lw)
        for t in wr:
            if t.lw is not None:
                deps.append(t.lw)
            deps.extend(t.rds)
        if is_dma:
            k = self.dma_rr % self.n_dma_sems
            self.dma_rr += 1
            if self.dma_last[k] is not None:
                deps.append(self.dma_last[k])
            self.dma_last[k] = op
            self.dma_cnt[k] += 16
            op.dsem = k
            op.dval = self.dma_cnt[k]
        fd = []
        seen = set()
        for d in deps:
            if id(d) in seen or d is op:
                continue
            seen.add(id(d))
            if (not d.is_dma) and d.eng == "pe" and eng == "pe":
                continue
            if not d.is_dma:
                d.inc = True
            fd.append(d)
        op.deps = fd
        for t in rd:
            t.rds.append(op)
        for t in wr:
            t.lw = op
            t.rds = []
        self.ops[eng].append(op)
        return op

    def op(self, eng, fn, rd=(), wr=()):
        return self._rec(eng, fn, rd, wr)

    def dma(self, eng, out, in_, rd=(), wr=()):
        return self._rec(eng, lambda e: e.dma_start(out=out, in_=in_), rd, wr, is_dma=True)

    def mm(self, out, lhsT, rhs, start, stop, rd, wr):
        return self.op("pe", lambda e: e.matmul(out, lhsT, rhs, start=start, stop=stop), rd, wr)

    def tr(self, out, in_, ident, rd, wr):
        return self.op("pe", lambda e: e.transpose(out, in_, ident), rd, wr)

    def act(self, out, in_, func, rd, wr, scale=None, bias=None, accum=None):
        kw = {}
        if scale is not None:
            kw["scale"] = scale
        if bias is not None:
            kw["bias"] = bias
        if accum is not None:
            kw["accum_out"] = accum
        return self.op("act", lambda e: e.activation(out=out, in_=in_, func=func, **kw), rd, wr)

    def tt(self, eng, out, in0, in1, op, rd, wr):
        return self.op(eng, lambda e: e.tensor_tensor(out=out, in0=in0, in1=in1, op=op), rd, wr)

    def ts(self, eng, out, in0, s1, s2, op0, op1, rd, wr, accum=None):
        if op1 is None:
            return self.op(eng, lambda e: e.tensor_scalar(out=out, in0=in0, scalar1=s1, scalar2=None, op0=op0), rd, wr)
        if accum is not None:
            return self.op(eng, lambda e: e.tensor_scalar(out=out, in0=in0, scalar1=s1, scalar2=s2, op0=op0,
                                                          op1=op1, accum_out=accum), rd, wr)
        return self.op(eng, lambda e: e.tensor_scalar(out=out, in0=in0, scalar1=s1, scalar2=s2, op0=op0, op1=op1),
                       rd, wr)

    def stt(self, out, in0, scalar, in1, op0, op1, rd, wr):
        return self.op("dve", lambda e: e.scalar_tensor_tensor(out=out, in0=in0, scalar=scalar, in1=in1,
                                                               op0=op0, op1=op1), rd, wr)

    def copy(self, eng, out, in_, rd, wr):
        if eng == "act":
            return self.op("act", lambda e: e.activation(out=out, in_=in_, func=AF.Copy), rd, wr)
        return self.op(eng, lambda e: e.tensor_copy(out=out, in_=in_), rd, wr)

    def memset(self, eng, out, val, wr):
        return self.op(eng, lambda e: e.memset(out, val), (), wr)

    def emit(self, nc, block, sems, dsems, final_waits):
        for eng in self.ENGS:
            c = [0] * self.nsets
            for op in self.ops[eng]:
                if op.inc and not op.is_dma:
                    c[op.ep] += 1
                    op.val = c[op.ep]
        engobj = {"pe": block.tensor, "act": block.scalar, "dve": block.vector, "pool": block.gpsimd,
                  "sp": block.sync}

        def make(eng):
            ops = self.ops[eng]

            def body(e):
                seen = {}
                for op in ops:
                    for d in op.deps:
                        if d.is_dma:
                            key = ("d", d.dsem)
                            sem = dsems[d.dsem]
                            val = d.dval
                        else:
                            key = ("c", d.eng, d.ep)
                            sem = sems[d.ep][d.eng]
                            val = d.val
                        if seen.get(key, 0) >= val:
                            continue
                        seen[key] = val
                        e.wait_ge(sem, val)
                    ins = op.fn(e)
                    if op.is_dma:
                        ins.then_inc(dsems[op.dsem], 16)
                    elif op.inc:
                        ins.then_inc(sems[op.ep][eng], 1)
                if eng == "sp":
                    for (k, v) in final_waits:
                        e.wait_ge(dsems[k], v)
            return body

        for eng in self.ENGS:
            engobj[eng](make(eng))


def _consts():
    c = {}
    pos = np.arange(S, dtype=np.float64)

    def tab(dim):
        inv = 10000.0 ** (-np.arange(0, dim, 2, dtype=np.float64) / dim)
        C = np.zeros((128, S), np.float32)
        Sg = np.zeros((128, S), np.float32)
        P = np.zeros((128, 128), np.float32)
        for p in range(128):
            i = p % dim
            j = i % (dim // 2)
            ang = (pos.astype(np.float32) * np.float32(inv[j])).astype(np.float32)
            C[p] = np.cos(ang)
            Sg[p] = (-np.sin(ang)) if i < dim // 2 else np.sin(ang)
            src = (p // dim) * dim + (i + dim // 2) % dim
            P[src, p] = 1.0
        return C, Sg, P
    c["C32"], c["S32"], c["P32"] = tab(32)
    c["C64"], c["S64"], c["P64"] = tab(64)
    cm = np.zeros((128, 4, 512), np.float32)
    k = np.arange(128)[:, None]
    q = np.arange(512)[None, :]
    for i in range(4):
        cm[:, i, :] = np.where(i * 128 + k > q, NEG, 0.0)
    c["cmask"] = cm
    qq = np.arange(128)[:, None]
    kk = np.arange(128)[None, :]
    c["cb"] = np.where(kk <= qq, 0.0, -1e30).astype(np.float32)
    lg = np.log1p(-np.exp2(-5.0 - np.arange(4, dtype=np.float64)))
    dec = np.zeros((128, 4, 128), np.float32)
    cc = np.arange(128)[:, None]
    q1 = np.arange(128)[None, :]
    for h in range(4):
        dec[:, (h % 2) * 2 + h // 2, :] = np.where(q1 >= cc, 0.125 * np.exp(np.maximum(q1 - cc, 0) * lg[h]), 0.0)
    c["decT"] = dec
    xi = np.zeros((128, 2, 512), np.float32)
    for i in range(2):
        for r in range(128):
            h = 2 * i + r // 64
            xi[r, i, :] = np.tile(np.exp((np.arange(128) + 1.0) * lg[h]), 4)
    c["xiT"] = xi
    zb = np.zeros((128, 256), np.float32)
    cdb = np.zeros((64, 256), np.float32)
    for h in range(4):
        zb[:, h * 64:(h + 1) * 64] = (0.125 * np.exp((127.0 - np.arange(128)) * lg[h]))[:, None]
        cdb[:, h * 64:(h + 1) * 64] = np.exp(128.0 * lg[h])
    c["zetab"] = zb
    c["cdb"] = cdb
    c["ctab"] = np.broadcast_to((2.0 ** -(np.arange(NIT + 1) + 1.0)).astype(np.float32)[None, :], (128, NIT + 1)).copy()
    c["ident"] = np.eye(128, dtype=np.float32)
    c["ones"] = np.ones((128, 128), np.float32)
    return c


CONST_SHAPES = {
    "C32": [128, S], "S32": [128, S], "P32": [128, 128], "C64": [128, S], "S64": [128, S], "P64": [128, 128],
    "cmask": [128, 4, 512], "cb": [128, 128], "decT": [128, 4, 128], "xiT": [128, 2, 512], "zetab": [128, 256],
    "cdb": [64, 256], "ctab": [128, NIT + 1], "ident": [128, 128], "ones": [128, 128],
}


def build_nc(nseq, nlayers, final_norm, dbg=None):
    from contextlib import ExitStack
    nc = bass.Bass("TRN2", target_bir_lowering=False)
    dr = {}
    dr["x"] = nc.dram_tensor("x", [nseq * S, D], F32, kind="ExternalInput").ap()
    dr["y"] = nc.dram_tensor("y", [nseq * S, D], F32, kind="ExternalOutput").ap()
    dr["w_in"] = nc.dram_tensor("w_in", [nlayers, D, INW], F32, kind="ExternalInput").ap()
    dr["w_uq"] = nc.dram_tensor("w_uq", [nlayers, 768, 768], F32, kind="ExternalInput").ap()
    dr["w_ukv"] = nc.dram_tensor("w_ukv", [nlayers, 256, 1024], F32, kind="ExternalInput").ap()
    dr["w_out"] = nc.dram_tensor("w_out", [nlayers, D, D], F32, kind="ExternalInput").ap()
    dr["w_ff1"] = nc.dram_tensor("w_ff1", [nlayers, D, DFF], F32, kind="ExternalInput").ap()
    dr["w_ff2"] = nc.dram_tensor("w_ff2", [nlayers, DFF, D], F32, kind="ExternalInput").ap()
    dr["gpack"] = nc.dram_tensor("gpack", [nlayers, 128, NG], F32, kind="ExternalInput").ap()
    dr["gfin"] = nc.dram_tensor("gfin", [128, 8], F32, kind="ExternalInput").ap()
    for k, shp in CONST_SHAPES.items():
        dr[k] = nc.dram_tensor("c_" + k, shp, F32, kind="ExternalInput").ap()
    if dbg:
        dr["dbg"] = nc.dram_tensor("dbg", [128, 8, S], F32, kind="ExternalOutput").ap()

    stop = None
    if dbg and ":" in dbg:
        dbg, stop = dbg.split(":")
    sch = Sched()
    with ExitStack() as st:
        ARW = 53000
        arena = st.enter_context(nc.sbuf_tensor("arena", [128, ARW], F32))
        psf = [st.enter_context(nc.psum_tensor("ps%d" % i, [128, 512], F32))[:, :] for i in range(7)]
        psb = st.enter_context(nc.psum_tensor("psb", [128, 1024], BF16))[:, :]
        sems = [{e: st.enter_context(nc.semaphore("sem%d_%s" % (k, e))) for e in Sched.ENGS} for k in range(sch.nsets)]
        dsems = [st.enter_context(nc.semaphore("dsem%d" % i)) for i in range(sch.n_dma_sems)]
        block = st.enter_context(nc.Block())

        class Carver:
            def __init__(self, base=0):
                self.off = base

            def take(self, nbytes):
                o = self.off
                self.off += (nbytes + 3) // 4 * 4
                assert self.off <= ARW * 4, ("SBUF arena overflow", self.off, ARW * 4)
                self.peak = max(getattr(self, "peak", 0), self.off)
                return o

        def view(off, dtype, shape):
            n = 1
            for s_ in shape[1:]:
                n *= s_
            esz = 4 if dtype == F32 else 2
            w0 = off // 4
            nw = (n * esz + 3) // 4
            ap = arena[0:shape[0], w0:w0 + nw]
            if dtype != F32:
                ap = ap.bitcast(dtype)
            if len(shape) == 3:
                ap = ap.rearrange("p (a b) -> p a b", a=shape[1])
            elif len(shape) == 4:
                ap = ap.rearrange("p (a b c) -> p a b c", a=shape[1], b=shape[2])
            return ap

        def alloc(C, dtype, shape):
            n = 1
            for s_ in shape[1:]:
                n *= s_
            return view(C.take(n * (4 if dtype == F32 else 2)), dtype, shape)

        G = Carver()
        xT = alloc(G, F32, [128, 8, S])
        OA = alloc(G, BF16, [128, 4, S])
        ident_b = alloc(G, BF16, [128, 128])
        ident_f = alloc(G, F32, [128, 128])
        ones_b = alloc(G, BF16, [128, 128])
        ones_f = alloc(G, F32, [128, 128])
        P32 = alloc(G, BF16, [128, 128])
        P64 = alloc(G, BF16, [128, 128])
        gp = alloc(G, F32, [128, nlayers, NG])
        gfin = alloc(G, F32, [128, 8])
        PHASE_BASE = G.off

        t_xT = [Tok("xT%d" % c) for c in range(NCH)]
        t_OA = [Tok("OA%d" % c) for c in range(NCH)]
        t_const = Tok("const")
        t_ps = [Tok("ps%d" % i) for i in range(7)]
        t_psb = Tok("psb")

        sch.dma("pool", ident_b, dr["ident"], (), [t_const])
        sch.dma("sp", ident_f, dr["ident"], (), [t_const])
        sch.dma("pool", ones_b, dr["ones"], (), [t_const])
        sch.dma("sp", ones_f, dr["ones"], (), [t_const])
        sch.dma("pool", P32, dr["P32"], (), [t_const])
        sch.dma("pool", P64, dr["P64"], (), [t_const])
        for l in range(nlayers):
            sch.dma("sp", gp[:, l, :], dr["gpack"][l], (), [t_const])
        sch.dma("sp", gfin, dr["gfin"], (), [t_const])

        def gvec(l, which, j):
            base = {"mix": 0, "q": 8, "kv": 14, "mlp": 16}[which]
            return gp[:, l, base + j:base + j + 1]

        def chunk_norm(c, gfun, hT_dst, t_h, sq, t_sq, rb, t_rb, pi):
            cols = slice(c * CH, (c + 1) * CH)
            for kc in range(8):
                sch.act(sq[:, kc % 2, :], xT[:, kc, cols], AF.Square, [t_xT[c]], [t_sq[kc % 2]])
                sch.mm(psf[pi], ones_b, sq[:, kc % 2, :], kc == 0, kc == 7, [t_sq[kc % 2], t_const], [t_ps[pi]])
            sch.act(rb, psf[pi], AF.Ln, [t_ps[pi]], [t_rb], scale=1.0 / D, bias=EPS)
            sch.act(rb, rb, AF.Exp, [t_rb], [t_rb], scale=-0.5)
            for kc in range(8):
                sch.stt(hT_dst[:, kc, :], xT[:, kc, cols], gfun(kc), rb, ALU.mult, ALU.mult,
                        [t_xT[c], t_rb, t_const], [t_h])

        def rope(rows, pin, pr, Ctab, Stab, t_tab, Pm, xb, t_xb, t1, t_t1, t2, t_t2, dst, t_dst,
                 rstd=None, t_rstd=None):
            if rstd is None:
                sch.copy("act", xb[0:rows, :], psf[pin][0:rows, :], [t_ps[pin]], [t_xb])
            else:
                sch.tt("dve", xb[0:rows, :], psf[pin][0:rows, :], rstd[0:rows, :], ALU.mult, [t_ps[pin], t_rstd], [t_xb])
            sch.mm(psf[pr][0:rows, :], Pm[0:rows, 0:rows], xb[0:rows, :], True, True, [t_xb, t_const], [t_ps[pr]])
            sch.tt("dve", t1[0:rows, :], xb[0:rows, :], Ctab[0:rows, :], ALU.mult, [t_xb, t_tab], [t_t1])
            sch.tt("dve", t2[0:rows, :], psf[pr][0:rows, :], Stab[0:rows, :], ALU.mult, [t_ps[pr], t_tab], [t_t2])
            if isinstance(dst, list):
                for (d_ap, r0) in dst:
                    sch.tt("dve", d_ap, t1[r0:r0 + 32, :], t2[r0:r0 + 32, :], ALU.add, [t_t1, t_t2], [t_dst])
            else:
                sch.tt("dve", dst, t1[0:rows, :], t2[0:rows, :], ALU.add, [t_t1, t_t2], [t_dst])

        def run_rope_jobs(jobs, scr):
            jobs[0]["proj"](0)
            for k, jb in enumerate(jobs):
                if k + 1 < len(jobs):
                    jobs[k + 1]["proj"]((k + 1) % 2)
                xb_, t_xb_, t1_, t_t1_, t2_, t_t2_ = scr[k % len(scr)]
                rope(jb["rows"], k % 2, 4 + k % 2, jb["C"], jb["S"], jb["t_tab"], jb["P"], xb_, t_xb_, t1_, t_t1_, t2_, t_t2_,
                     jb["dst"], jb["t_dst"], rstd=jb.get("rstd"), t_rstd=jb.get("t_rstd"))
                if jb.get("post"):
                    jb["post"]()

        for sq_i in range(nseq):
            sch.barrier()
            Lc = Carver(PHASE_BASE)
            xin = [alloc(Lc, F32, [128, D]) for _ in range(2)]
            t_xin = [Tok("xin0"), Tok("xin1")]
            for t in range(NT):
                b = t % 2
                sch.dma("sp", xin[b], dr["x"][sq_i * S + t * 128: sq_i * S + (t + 1) * 128, :], (), [t_xin[b]])
                for half in range(2):
                    for k4 in range(4):
                        kc = half * 4 + k4
                        sch.tr(psf[half][:, k4 * 128:(k4 + 1) * 128], xin[b][:, kc * 128:(kc + 1) * 128], ident_f,
                               [t_xin[b], t_const], [t_ps[half]])
                    for k4 in range(4):
                        kc = half * 4 + k4
                        sch.copy("act" if half == 0 else "dve", xT[:, kc, t * 128:(t + 1) * 128],
                                 psf[half][:, k4 * 128:(k4 + 1) * 128], [t_ps[half]], [t_xT[t // 4]])

            for l in range(nlayers):
                sch.barrier()
                A = Carver(PHASE_BASE)
                Win = alloc(A, BF16, [128, 8, 1056])
                WuqN = alloc(A, BF16, [128, 6, 512])
                WuqR = alloc(A, BF16, [128, 6, 256])
                WukvK = alloc(A, BF16, [128, 2, 512])
                WukvV = alloc(A, BF16, [128, 2, 512])
                KN = alloc(A, BF16, [128, 4, S])
                KR3 = alloc(A, BF16, [128, S])
                VC = alloc(A, BF16, [128, NT, 8, 65])
                hT = alloc(A, BF16, [128, 8, CH])
                sqb = alloc(A, BF16, [128, 2, CH])
                rb = alloc(A, F32, [128, CH])
                rq_b = alloc(A, F32, [128, CH])
                rkv_b = alloc(A, F32, [128, CH])
                cqn = alloc(A, BF16, [128, 6, CH])
                ckvn = alloc(A, BF16, [128, 2, CH])
                tabC = alloc(A, BF16, [128, CH])
                tabS = alloc(A, BF16, [128, CH])
                xb = alloc(A, BF16, [128, CH])
                t1 = alloc(A, BF16, [128, CH])
                t2 = alloc(A, BF16, [128, CH])
                xb2 = alloc(A, BF16, [128, CH])
                t12 = alloc(A, BF16, [128, CH])
                t22 = alloc(A, BF16, [128, CH])
                t_xb2, t_t12, t_t22 = Tok(), Tok(), Tok()
                QN = alloc(A, BF16, [128, 8, CH])
                QR = alloc(A, BF16, [128, 8, CH])
                PT = [alloc(A, BF16, [128, CH]) for _ in range(3)]
                cmask = alloc(A, BF16, [128, 4, CH])
                rec = rb
                pbs = alloc(A, F32, [128, CH])
                rtok = alloc(A, F32, [128, 8])
                t_W = [Tok() for _ in range(8)]; t_WqN = [Tok() for _ in range(6)]; t_WqR = [Tok() for _ in range(6)]; t_WkK = [Tok(), Tok()]; t_WkV = [Tok(), Tok()]
                t_KN = [Tok() for _ in range(NCH)]
                t_KR = [Tok() for _ in range(NCH)]
                t_VC = [Tok() for _ in range(NCH)]
                t_h, t_rb, t_rq, t_rkv, t_cqn, t_ckvn = Tok(), Tok(), Tok(), Tok(), Tok(), Tok()
                t_sq = [Tok(), Tok()]
                t_tab, t_xb, t_t1, t_t2, t_QN, t_QR = Tok(), Tok(), Tok(), Tok(), Tok(), Tok()
                t_PT = [Tok(), Tok(), Tok()]
                t_cm, t_pbs, t_rtok = Tok(), Tok(), Tok()
                t_rec = t_rb

                for kc in range(8):
                    sch.dma("pool", Win[:, kc, :], dr["w_in"][l, kc * 128:(kc + 1) * 128, 0:1056], (), [t_W[kc]])
                for kc in range(6):
                    srcq = dr["w_uq"][l, kc * 128:(kc + 1) * 128, :].rearrange("p (h d) -> p h d", h=8)
                    sch.dma("pool", WuqN[:, kc, :].rearrange("p (h d) -> p h d", h=8), srcq[:, :, 0:64], (), [t_WqN[kc]])
                    sch.dma("pool", WuqR[:, kc, :].rearrange("p (h d) -> p h d", h=8), srcq[:, :, 64:96], (), [t_WqR[kc]])
                for kc in range(2):
                    srck = dr["w_ukv"][l, kc * 128:(kc + 1) * 128, :].rearrange("p (h d) -> p h d", h=8)
                    sch.dma("pool", WukvK[:, kc, :].rearrange("p (h d) -> p h d", h=8), srck[:, :, 0:64], (), [t_WkK[kc]])
                    sch.dma("pool", WukvV[:, kc, :].rearrange("p (h d) -> p h d", h=8), srck[:, :, 64:128], (), [t_WkV[kc]])
                sch.dma("pool", cmask, dr["cmask"], (), [t_cm])
                sch.memset("pool", QN[:, :, :], 0.0, [t_QN])
                sch.memset("pool", QR[:, :, :], 0.0, [t_QR])
                sch.memset("pool", VC[:, :, :, 64:65], 1.0, [t_VC[c] for c in range(NCH)])

                for c in range(NCH):
                    cols = slice(c * CH, (c + 1) * CH)
                    sch.dma("pool", tabC, dr["C32"][:, cols], (), [t_tab])
                    sch.dma("pool", tabS, dr["S32"][:, cols], (), [t_tab])
                    chunk_norm(c, lambda kc: gvec(l, "mix", kc), hT, t_h, sqb, t_sq, rb, t_rb, 0)
                    for j in range(6):
                        pi = j % 2
                        for kc in range(8):
                            sch.mm(psf[pi], Win[:, kc, j * 128:(j + 1) * 128], hT[:, kc, :], kc == 0, kc == 7,
                                   [t_W[kc], t_h], [t_ps[pi]])
                        sch.act(cqn[:, j, :], psf[pi], AF.Copy, [t_ps[pi], t_const], [t_cqn], scale=gvec(l, "q", j))
                        sch.act(sqb[:, j % 2, :], psf[pi], AF.Square, [t_ps[pi]], [t_sq[j % 2]])
                        sch.mm(psf[2], ones_b, sqb[:, j % 2, :], j == 0, j == 5, [t_sq[j % 2], t_const], [t_ps[2]])
                    sch.act(rq_b, psf[2], AF.Ln, [t_ps[2]], [t_rq], scale=1.0 / 768, bias=EPS)
                    sch.act(rq_b, rq_b, AF.Exp, [t_rq], [t_rq], scale=-0.5)
                    for j in range(2):
                        pi = j % 2
                        for kc in range(8):
                            sch.mm(psf[pi], Win[:, kc, 768 + j * 128:768 + (j + 1) * 128], hT[:, kc, :], kc == 0, kc == 7,
                                   [t_W[kc], t_h], [t_ps[pi]])
                        sch.act(ckvn[:, j, :], psf[pi], AF.Copy, [t_ps[pi], t_const], [t_ckvn], scale=gvec(l, "kv", j))
                        sch.act(sqb[:, j % 2, :], psf[pi], AF.Square, [t_ps[pi]], [t_sq[j % 2]])
                        sch.mm(psf[2], ones_b, sqb[:, j % 2, :], j == 0, j == 1, [t_sq[j % 2], t_const], [t_ps[2]])
                    for t in range(4):
                        for j in range(2):
                            sch.mm(psf[3][:, 2 * t:2 * t + 2], sqb[:, j, t * 128:(t + 1) * 128], ones_b[:, 0:2], j == 0, j == 1,
                                   [t_sq[j], t_const], [t_ps[3]])
                    sch.act(rkv_b, psf[2], AF.Ln, [t_ps[2]], [t_rkv], scale=1.0 / 256, bias=EPS)
                    sch.act(rkv_b, rkv_b, AF.Exp, [t_rkv], [t_rkv], scale=-0.5)
                    sch.act(rtok[:, 0:4], psf[3][:, 0:8].rearrange("p (t two) -> p t two", two=2)[:, :, 0], AF.Ln, [t_ps[3]], [t_rtok], scale=1.0 / 256, bias=EPS)
                    sch.act(rtok[:, 0:4], rtok[:, 0:4], AF.Exp, [t_rtok], [t_rtok], scale=-0.5)
                    for i in range(4):
                        pi = i % 2
                        for kc in range(6):
                            sch.mm(psf[pi], WuqN[:, kc, i * 128:(i + 1) * 128], cqn[:, kc, :], kc == 0, kc == 5,
                                   [t_WqN[kc], t_cqn], [t_ps[pi]])
                        sch.tt("dve", QN[0:64, 2 * i, :], psf[pi][0:64, :], rq_b[0:64, :], ALU.mult, [t_ps[pi], t_rq], [t_QN])
                        sch.tt("dve", QN[64:128, 2 * i + 1, :], psf[pi][64:128, :], rq_b[64:128, :], ALU.mult, [t_ps[pi], t_rq], [t_QN])
                    def kr_proj(pi):
                        for kc in range(8):
                            sch.mm(psf[pi][0:32, :], Win[:, kc, 1024:1056], hT[:, kc, :], kc == 0, kc == 7, [t_W[kc], t_h], [t_ps[pi]])

                    def kr_post():
                        sch.copy("pool", KR3[32:64, cols], KR3[0:32, cols], [t_KR[c]], [t_KR[c]])
                        sch.copy("pool", KR3[64:96, cols], KR3[0:32, cols], [t_KR[c]], [t_KR[c]])

                    def mk_qr_proj(g, rows):
                        def f(pi):
                            for kc in range(6):
                                sch.mm(psf[pi][0:rows, :], WuqR[:, kc, g * 96:g * 96 + rows], cqn[:, kc, :], kc == 0, kc == 5,
                                       [t_WqR[kc], t_cqn], [t_ps[pi]])
                        return f
                    jobs = [dict(proj=kr_proj, rows=32, C=tabC, S=tabS, t_tab=t_tab, P=P32, dst=KR3[0:32, cols], t_dst=t_KR[c],
                                 post=kr_post)]
                    for g in range(3):
                        nh = 3 if g < 2 else 2
                        jobs.append(dict(proj=mk_qr_proj(g, nh * 32), rows=nh * 32, C=tabC, S=tabS, t_tab=t_tab, P=P32,
                                         dst=[(QR[32 * k:32 * k + 32, 3 * g + k, :], 32 * k) for k in range(nh)], t_dst=t_QR,
                                         rstd=rq_b, t_rstd=t_rq))
                    run_rope_jobs(jobs, [(xb, t_xb, t1, t_t1, t2, t_t2), (xb2, t_xb2, t12, t_t12, t22, t_t22)])
                    for i in range(4):
                        pi = i % 2
                        for kc in range(2):
                            sch.mm(psf[pi], WukvK[:, kc, i * 128:(i + 1) * 128], ckvn[:, kc, :], kc == 0, kc == 1,
                                   [t_WkK[kc], t_ckvn], [t_ps[pi]])
                        sch.tt("dve", KN[:, i, cols], psf[pi], rkv_b, ALU.mult, [t_ps[pi], t_rkv], [t_KN[c]])
                    for t in range(4):
                        pi = t % 2
                        for kc in range(2):
                            sch.mm(psf[pi], ckvn[:, kc, t * 128:(t + 1) * 128], WukvV[:, kc, :], kc == 0, kc == 1,
                                   [t_WkV[kc], t_ckvn], [t_ps[pi]])
                        sch.ts("dve", VC[:, c * 4 + t, :, 0:64], psf[pi].rearrange("p (h d) -> p h d", h=8),
                               rtok[:, t:t + 1], None, ALU.mult, None, [t_ps[pi], t_rtok], [t_VC[c]])
                    nkb = 4 * (c + 1)
                    steps = [(h, kb) for h in range(8) for kb in range(nkb)]

                    def mla_qk(si):
                        h, kb = steps[si]
                        i, off = h // 2, (h % 2) * 64
                        g, goff = h // 3, (h % 3) * 32
                        pi = 2 + (si % 2)
                        kc_ = kb // 4
                        kcols = slice(kb * 128, (kb + 1) * 128)
                        diag = kb >= 4 * c
                        sch.mm(psf[pi], KN[:, i, kcols], QN[:, h, :], True, False,
                               [t_KN[kc_], t_QN], [t_ps[pi]])
                        sch.mm(psf[pi], KR3[0:96, kcols], QR[0:96, h, :], False, not diag,
                               [t_KR[kc_], t_QR], [t_ps[pi]])
                        if diag:
                            sch.mm(psf[pi], ident_b, cmask[:, kb - 4 * c, :], False, True, [t_cm, t_const], [t_ps[pi]])
                        pt = si % 3
                        sch.act(PT[pt], psf[pi], AF.Exp, [t_ps[pi]], [t_PT[pt]], scale=MLA_SCALE)

                    def mla_pv(si):
                        h, kb = steps[si]
                        po = 4 + (h % 2)
                        pt = si % 3
                        sch.mm(psf[po][0:65, :], VC[:, kb, h, 0:65], PT[pt], kb == 0, kb == nkb - 1,
                               [t_VC[kb // 4], t_PT[pt]], [t_ps[po]])

                    def mla_norm_a(h):
                        po = 4 + (h % 2)
                        sch.op("dve", lambda e, po=po: e.reciprocal(out=rec[64:65, :], in_=psf[po][64:65, :]),
                               [t_ps[po]], [t_rec])

                    def mla_norm_b(h):
                        i, off = h // 2, (h % 2) * 64
                        po = 4 + (h % 2)
                        sch.mm(psf[6][0:64, :], ones_f[64:65, 0:64], rec[64:65, :], True, True, [t_rec, t_const], [t_ps[6]])
                        sch.copy("act", pbs[0:64, :], psf[6][0:64, :], [t_ps[6]], [t_pbs])
                        sch.tt("dve", OA[off:off + 64, i, cols], psf[po][0:64, :], pbs[0:64, :], ALU.mult,
                               [t_ps[po], t_pbs], [t_OA[c]])

                    mla_qk(0)
                    pend = None
                    for si in range(len(steps)):
                        if si + 1 < len(steps):
                            mla_qk(si + 1)
                        mla_pv(si)
                        if pend is not None:
                            pend[1] -= 1
                            if pend[1] == 0:
                                mla_norm_b(pend[0])
                                pend = None
                        h, kb = steps[si]
                        if kb == nkb - 1:
                            if pend is not None:
                                mla_norm_b(pend[0])
                            mla_norm_a(h)
                            pend = [h, 2]
                    if pend is not None:
                        mla_norm_b(pend[0])

                if dbg == "p1":
                    break
                sch.barrier()
                A = Carver(PHASE_BASE)
                Vbf = alloc(A, BF16, [128, 256])
                Vz = alloc(A, BF16, [128, 256])
                Gs = alloc(A, F32, [128, 256])
                Ktok = alloc(A, BF16, [128, 256])
                AT = alloc(A, BF16, [128, 4, 128])
                rsb = alloc(A, F32, [128, 256])
                oct_ = alloc(A, BF16, [128, 256])
                bst = alloc(A, F32, [128, 4, 6])
                bag = alloc(A, F32, [128, 4, 2])
                Win = alloc(A, BF16, [128, 8, 1704])
                Wout = alloc(A, BF16, [128, 8, D])
                KB2 = alloc(A, BF16, [128, S])
                KI3 = alloc(A, BF16, [128, S])
                VB = alloc(A, BF16, [128, NT, 65])
                hT = alloc(A, BF16, [128, 8, CH])
                rb = alloc(A, F32, [128, CH])
                tC32 = alloc(A, BF16, [128, CH])
                tS32 = alloc(A, BF16, [128, CH])
                tC64 = alloc(A, BF16, [128, CH])
                tS64 = alloc(A, BF16, [128, CH])
                xb = alloc(A, BF16, [128, CH])
                t1 = alloc(A, BF16, [128, CH])
                t2 = alloc(A, BF16, [128, CH])
                QB = alloc(A, BF16, [128, 2, CH])
                QI = alloc(A, BF16, [128, 3, CH])
                WI = alloc(A, F32, [128, 4, 8])
                RQ = alloc(A, BF16, [128, 2, CH])
                RQX = alloc(A, BF16, [128, 2, CH])
                RK = alloc(A, BF16, [128, 2, CH])
                xiT = alloc(A, F32, [128, 2, 128])
                decT = alloc(A, F32, [128, 4, 128])
                zetab = alloc(A, F32, [128, 256])
                cdb = alloc(A, F32, [128, 256])
                cb = alloc(A, F32, [128, 128])
                ctab = alloc(A, F32, [128, NIT + 1])
                gret = gp[:, l, 24:280]
                OT = alloc(A, BF16, [128, 4, CH])
                Ibuf = alloc(A, F32, [128, S])
                Rtmp2 = alloc(A, BF16, [128, 2, CH])
                Rtmp = [Rtmp2[:, 0, :], Rtmp2[:, 1, :]]
                sqb = Rtmp2
                Mb = alloc(A, BF16, [128, S])
                junk = Mb
                MbT = [alloc(A, BF16, [128, NT, 128]) for _ in range(2)]
                PTd = [alloc(A, BF16, [128, 4, 128]) for _ in range(2)]
                smA = alloc(A, F32, [128, 2])
                smR = alloc(A, F32, [128, 4])
                OBt = alloc(A, BF16, [128, 256])
                sm = alloc(A, F32, [128, 64])
                wtab = alloc(A, F32, [128, NIT + 1])
                Rst = alloc(A, F32, [128, 256])
                Rbf = alloc(A, BF16, [128, 256])
                t_W = [Tok() for _ in range(8)]; t_Wo = [Tok() for _ in range(8)]; t_cst = Tok()
                t_KB = [Tok() for _ in range(NCH)]
                t_KI = [Tok() for _ in range(NCH)]
                t_VB = [Tok() for _ in range(NCH)]
                t_h, t_rb = Tok(), Tok()
                t_tab, t_xb, t_t1, t_t2 = Tok(), Tok(), Tok(), Tok()
                t_QB, t_QI, t_WI, t_RQ, t_RQX, t_RK, t_OT = Tok(), Tok(), Tok(), Tok(), Tok(), Tok(), Tok()
                t_I, t_junk, t_Mb, t_OBt, t_sm, t_wtab = Tok(), Tok(), Tok(), Tok(), Tok(), Tok()
                t_MbT = [Tok(), Tok()]
                t_smA, t_smR = Tok(), Tok()
                t_Rtmp = [Tok(), Tok()]
                t_sq = t_Rtmp
                t_junk = t_Mb
                t_PTd = [Tok(), Tok()]
                t_Rst, t_Rbf, t_Vbf, t_Vz, t_Gs, t_Ktok, t_AT, t_rsb, t_oct, t_bst = (Tok() for _ in range(10))

                for kc in range(8):
                    sch.dma("pool", Win[:, kc, :], dr["w_in"][l, kc * 128:(kc + 1) * 128, 1056:2760], (), [t_W[kc]])
                for kc in range(8):
                    sch.dma("pool", Wout[:, kc, :], dr["w_out"][l, kc * 128:(kc + 1) * 128, :], (), [t_Wo[kc]])
                sch.dma("sp", xiT, dr["xiT"][:, :, 0:128], (), [t_cst])
                sch.dma("sp", decT, dr["decT"], (), [t_cst])
                sch.dma("sp", zetab, dr["zetab"], (), [t_cst])
                sch.dma("sp", cdb[0:64, :], dr["cdb"], (), [t_cst])
                sch.dma("sp", cb, dr["cb"], (), [t_cst])
                sch.dma("sp", ctab, dr["ctab"], (), [t_cst])
                sch.memset("pool", VB[:, :, 64:65], 1.0, [t_VB[c] for c in range(NCH)])
                sch.memset("pool", Rst[:, :], 0.0, [t_Rst])
                sch.memset("pool", Rbf[:, :], 0.0, [t_Rbf])

                def wc(a, b):
                    return slice(a - 1056, b - 1056)

                for c in range(NCH):
                    cols = slice(c * CH, (c + 1) * CH)
                    sch.dma("pool", tC32, dr["C32"][:, cols], (), [t_tab])
                    sch.dma("pool", tS32, dr["S32"][:, cols], (), [t_tab])
                    sch.dma("pool", tC64, dr["C64"][:, cols], (), [t_tab])
                    sch.dma("pool", tS64, dr["S64"][:, cols], (), [t_tab])
                    chunk_norm(c, lambda kc: gvec(l, "mix", kc), hT, t_h, sqb, t_sq, rb, t_rb, 0)

                    def proj(pi, c0, c1, rows):
                        for kc in range(8):
                            sch.mm(psf[pi][0:rows, :], Win[:, kc, wc(c0, c1)], hT[:, kc, :], kc == 0, kc == 7,
                                   [t_W[kc], t_h], [t_ps[pi]])
                    def mkproj(c0, c1, rows):
                        def f(pi):
                            for kc in range(8):
                                sch.mm(psf[pi][0:rows, :], Win[:, kc, wc(c0, c1)], hT[:, kc, :], kc == 0, kc == 7,
                                       [t_W[kc], t_h], [t_ps[pi]])
                        return f
                    jobs = []
                    for i in range(2):
                        jobs.append(dict(proj=mkproj(1056 + 128 * i, 1056 + 128 * (i + 1), 128), rows=128, C=tC64, S=tS64, t_tab=t_tab,
                                         P=P64, dst=QB[:, i, :], t_dst=t_QB))
                    jobs.append(dict(proj=mkproj(1312, 1376, 64), rows=64, C=tC64, S=tS64, t_tab=t_tab, P=P64,
                                     dst=KB2[0:64, cols], t_dst=t_KB[c],
                                     post=lambda: sch.copy("pool", KB2[64:128, cols], KB2[0:64, cols], [t_KB[c]], [t_KB[c]])))
                    for g in range(3):
                        nh = 3 if g < 2 else 2
                        jobs.append(dict(proj=mkproj(1440 + 96 * g, 1440 + 96 * g + 32 * nh, 32 * nh), rows=32 * nh, C=tC32, S=tS32,
                                         t_tab=t_tab, P=P32, dst=QI[0:32 * nh, g, :], t_dst=t_QI))

                    def ki_post():
                        sch.copy("pool", KI3[32:64, cols], KI3[0:32, cols], [t_KI[c]], [t_KI[c]])
                        sch.copy("pool", KI3[64:96, cols], KI3[0:32, cols], [t_KI[c]], [t_KI[c]])
                    jobs.append(dict(proj=mkproj(1696, 1728, 32), rows=32, C=tC32, S=tS32, t_tab=t_tab, P=P32,
                                     dst=KI3[0:32, cols], t_dst=t_KI[c], post=ki_post))
                    for i in range(2):
                        jobs.append(dict(proj=mkproj(1736 + 128 * i, 1736 + 128 * (i + 1), 128), rows=128, C=tC64, S=tS64, t_tab=t_tab,
                                         P=P64, dst=RQ[:, i, :], t_dst=t_RQ))
                        jobs.append(dict(proj=mkproj(1992 + 128 * i, 1992 + 128 * (i + 1), 128), rows=128, C=tC64, S=tS64, t_tab=t_tab,
                                         P=P64, dst=RK[:, i, :], t_dst=t_RK))
                    run_rope_jobs(jobs, [(xb, t_xb, t1, t_t1, t2, t_t2)])
                    if stop == "proj2":
                        break
                    for t in range(4):
                        tcols = slice(t * 128, (t + 1) * 128)
                        for kc in range(8):
                            sch.mm(psf[2][:, 0:64], hT[:, kc, tcols], Win[:, kc, wc(1376, 1440)], kc == 0, kc == 7,
                                   [t_W[kc], t_h], [t_ps[2]])
                        for kc in range(8):
                            sch.mm(psf[2][:, 64:128], hT[:, kc, tcols], Win[:, kc, wc(1728, 1792)], kc == 0, kc == 7,
                                   [t_W[kc], t_h], [t_ps[2]])
                        sch.copy("act", VB[:, c * 4 + t, 0:64], psf[2][:, 0:64], [t_ps[2]], [t_VB[c]])
                        sch.act(WI[:, t, :], psf[2][:, 64:72], AF.Copy, [t_ps[2]], [t_WI], scale=IDX_SCALE)

                    if stop == "proj":
                        break
                    def dsa_idx(j):
                        jt = c * 4 + j
                        qcols = slice(j * 128, (j + 1) * 128)
                        W = 128 * (jt + 1)
                        ngrp = (W + 511) // 512
                        n = 0
                        for h in range(8):
                            g, goff = h // 3, (h % 3) * 32
                            for kg in range(ngrp):
                                k0, k1 = kg * 512, min(W, kg * 512 + 512)
                                pi = 2 + n % 2
                                rt = n % 2
                                n += 1
                                sch.mm(psf[pi][:, 0:k1 - k0], QI[goff:goff + 32, g, qcols], KI3[goff:goff + 32, k0:k1], True, True,
                                       [t_QI] + [t_KI[k0 // 512]], [t_ps[pi]])
                                sch.act(Rtmp[rt][:, 0:k1 - k0], psf[pi][:, 0:k1 - k0], AF.Relu, [t_ps[pi]], [t_Rtmp[rt]])
                                if h == 0:
                                    sch.ts("dve", Ibuf[:, k0:k1], Rtmp[rt][:, 0:k1 - k0], WI[:, j, 0:1], None, ALU.mult, None,
                                           [t_Rtmp[rt], t_WI], [t_I])
                                else:
                                    sch.stt(Ibuf[:, k0:k1], Rtmp[rt][:, 0:k1 - k0], WI[:, j, h:h + 1], Ibuf[:, k0:k1],
                                            ALU.mult, ALU.add, [t_Rtmp[rt], t_WI, t_I], [t_I])

                    def dsa_bis(j):
                        jt = c * 4 + j
                        W = 128 * (jt + 1)
                        sch.op("dve", lambda e, W=W: e.tensor_reduce(out=sm[:, 0:1], in_=Ibuf[:, 0:W], axis=AX.X, op=ALU.max,
                                                                      apply_absolute_value=True), [t_I], [t_sm])
                        sch.ts("dve", sm[:, 1:2], sm[:, 0:1], 2.0, 2.0, ALU.mult, ALU.add, [t_sm], [t_sm])
                        sch.ts("dve", wtab[:, :], ctab[:, :], sm[:, 1:2], None, ALU.mult, None, [t_sm, t_cst], [t_wtab])
                        sch.tt("dve", Ibuf[:, W - 128:W], Ibuf[:, W - 128:W], cb[:, :], ALU.add, [t_I, t_cst], [t_I])
                        sch.memset("dve", sm[:, 2:3], 0.0, [t_sm])
                        for it in range(NIT):
                            sch.ts("dve", junk[:, 0:W], Ibuf[:, 0:W], sm[:, 2:3], 0.0, ALU.is_ge, ALU.add, [t_I, t_sm], [t_junk, t_sm],
                                   accum=sm[:, 3:4])
                            sch.ts("dve", sm[:, 4:5], sm[:, 3:4], 255.5, 0.5, ALU.is_ge, ALU.subtract, [t_sm], [t_sm])
                            sch.stt(sm[:, 2:3], sm[:, 4:5], wtab[:, it:it + 1], sm[:, 2:3], ALU.mult, ALU.add, [t_sm, t_wtab], [t_sm])
                        sch.tt("dve", sm[:, 5:6], sm[:, 2:3], wtab[:, NIT:NIT + 1], ALU.subtract, [t_sm, t_wtab], [t_sm])
                        sch.ts("dve", Mb[:, 0:W], Ibuf[:, 0:W], sm[:, 5:6], NEG, ALU.is_lt, ALU.mult, [t_I, t_sm], [t_Mb])

                    def dsa_tr(j):
                        jt = c * 4 + j
                        mb = jt % 2
                        for kb0 in range(0, jt + 1, 8):
                            nb = min(8, jt + 1 - kb0)
                            for ii in range(nb):
                                kb = kb0 + ii
                                sch.tr(psb[:, ii * 128:(ii + 1) * 128], Mb[:, kb * 128:(kb + 1) * 128], ident_b,
                                       [t_Mb, t_const], [t_psb])
                            sch.copy("act", MbT[mb][:, kb0:kb0 + nb, :], psb[:, 0:nb * 128].rearrange("p (a b) -> p a b", a=nb),
                                     [t_psb], [t_MbT[mb]])

                    def dsa_attn(j):
                        jt = c * 4 + j
                        mb = jt % 2
                        qcols = slice(j * 128, (j + 1) * 128)
                        steps = [(h, kb0) for h in range(4) for kb0 in range(0, jt + 1, 4)]

                        def qk(si):
                            h, kb0 = steps[si]
                            i, off = h // 2, (h % 2) * 64
                            nb = min(4, jt + 1 - kb0)
                            pi = si % 2
                            for ii in range(nb):
                                kb = kb0 + ii
                                sch.mm(psf[pi][:, ii * 128:(ii + 1) * 128], KB2[off:off + 64, kb * 128:(kb + 1) * 128],
                                       QB[off:off + 64, i, qcols], True, False, [t_KB[kb // 4], t_QB], [t_ps[pi]])
                                sch.mm(psf[pi][:, ii * 128:(ii + 1) * 128], ident_b, MbT[mb][:, kb, :], False, True,
                                       [t_MbT[mb], t_const], [t_ps[pi]])
                            sch.act(PTd[pi][:, 0:nb, :], psf[pi][:, 0:nb * 128].rearrange("p (a b) -> p a b", a=nb), AF.Exp,
                                    [t_ps[pi]], [t_PTd[pi]], scale=DSA_SCALE)

                        def pv(si):
                            h, kb0 = steps[si]
                            nb = min(4, jt + 1 - kb0)
                            pi = si % 2
                            po = 4 + (h % 2)
                            for ii in range(nb):
                                kb = kb0 + ii
                                sch.mm(psf[po][:, 0:65], PTd[pi][:, ii, :], VB[:, kb, 0:65], kb == 0, kb == jt,
                                       [t_PTd[pi], t_VB[kb // 4]], [t_ps[po]])
                            if kb0 + nb == jt + 1:
                                sch.op("dve", lambda e, po=po: e.reciprocal(out=smA[:, 0:1], in_=psf[po][:, 64:65]), [t_ps[po]], [t_smA])
                                sch.ts("dve", OBt[:, h * 64:(h + 1) * 64], psf[po][:, 0:64], smA[:, 0:1], None, ALU.mult, None,
                                       [t_ps[po], t_smA], [t_OBt])

                        qk(0)
                        for si in range(len(steps)):
                            if si + 1 < len(steps):
                                qk(si + 1)
                            pv(si)
                        for i in range(2):
                            sch.tr(psb[:, i * 128:(i + 1) * 128], OBt[:, i * 128:(i + 1) * 128], ident_b, [t_OBt, t_const], [t_psb])
                        sch.copy("act", OT[:, 0:2, qcols], psb[:, 0:256].rearrange("p (a b) -> p a b", a=2), [t_psb], [t_OT])

                    def ret_tile(j):
                        qcols = slice(j * 128, (j + 1) * 128)
                        for i in range(2):
                            sch.tt("pool", RQX[:, i, qcols], RQ[:, i, qcols], xiT[:, i, :], ALU.mult, [t_RQ, t_cst], [t_RQX])
                        for kc in range(8):
                            sch.mm(psf[6], hT[:, kc, qcols], Win[:, kc, wc(2248, 2760)], kc == 0, kc == 7, [t_W[kc], t_h], [t_ps[6]])
                        sch.copy("act", Vbf[:, :], psf[6][:, 0:256], [t_ps[6]], [t_Vbf])
                        sch.tt("pool", Vz[:, :], Vbf[:, :], zetab[:, :], ALU.mult, [t_Vbf, t_cst], [t_Vz])
                        sch.act(Gs[:, :], psf[6][:, 256:512], AF.Exp, [t_ps[6]], [t_Gs], scale=-1.0)
                        sch.ts("pool", Gs[:, :], Gs[:, :], 1.0, None, ALU.add, None, [t_Gs], [t_Gs])
                        sch.op("dve", lambda e: e.reciprocal(out=Gs[:, :], in_=Gs[:, :]), [t_Gs], [t_Gs])
                        sch.tt("dve", Gs[:, :], Gs[:, :], psf[6][:, 256:512], ALU.mult, [t_Gs, t_ps[6]], [t_Gs])
                        for i in range(2):
                            sch.tr(psb[:, i * 128:(i + 1) * 128], RK[:, i, qcols], ident_b, [t_RK, t_const], [t_psb])
                        sch.copy("act", Ktok[:, :], psb[:, 0:256], [t_psb], [t_Ktok])
                        for h in range(4):
                            i, off = h // 2, (h % 2) * 64
                            sch.mm(psf[2 + h % 2][:, (h // 2) * 128:(h // 2 + 1) * 128], RK[off:off + 64, i, qcols],
                                   RQ[off:off + 64, i, qcols], True, True, [t_RK, t_RQ], [t_ps[2 + h % 2]])
                        for par in range(2):
                            sch.tt("dve", AT[:, 2 * par:2 * par + 2, :], psf[2 + par][:, 0:256].rearrange("p (a b) -> p a b", a=2),
                                   decT[:, 2 * par:2 * par + 2, :], ALU.mult, [t_ps[2 + par], t_cst], [t_AT])
                        for h in range(4):
                            i, off = h // 2, (h % 2) * 64
                            sch.mm(psf[3][:, h * 64:(h + 1) * 64], AT[:, (h % 2) * 2 + h // 2, :], Vbf[:, h * 64:(h + 1) * 64], True, False,
                                   [t_AT, t_Vbf], [t_ps[3]])
                            sch.mm(psf[3][:, h * 64:(h + 1) * 64], RQX[off:off + 64, i, qcols], Rbf[off:off + 64, h * 64:(h + 1) * 64],
                                   False, True, [t_RQX, t_Rbf], [t_ps[3]])
                        for h in range(4):
                            sch.mm(psf[6][0:64, h * 64:(h + 1) * 64], Ktok[:, h * 64:(h + 1) * 64], Vz[:, h * 64:(h + 1) * 64], True, True,
                                   [t_Ktok, t_Vz], [t_ps[6]])
                        sch.tt("pool", Rst[0:64, :], Rst[0:64, :], cdb[0:64, :], ALU.mult, [t_Rst, t_cst], [t_Rst])
                        sch.tt("dve", Rst[0:64, :], Rst[0:64, :], psf[6][0:64, 0:256], ALU.add, [t_Rst, t_ps[6]], [t_Rst])
                        sch.copy("act", Rbf[0:64, :], Rst[0:64, :], [t_Rst], [t_Rbf])
                        sch.copy("act", Rbf[64:128, :], Rst[0:64, :], [t_Rst], [t_Rbf])
                        sch.copy("act", rsb[:, :], psf[3][:, 0:256], [t_ps[3]], [t_rsb])
                        for h in range(4):
                            sch.op("dve", lambda e, h=h: e.bn_stats(out=bst[:, h, :], in_=rsb[:, h * 64:(h + 1) * 64]), [t_rsb], [t_bst])
                            sch.op("dve", lambda e, h=h: e.bn_aggr(out=bag[:, h, :], in_=bst[:, h, :]), [t_bst], [t_bst])
                        sch.act(smR[:, 0:4], bag[:, :, 1], AF.Sqrt, [t_bst], [t_smR], bias=EPS)
                        sch.op("dve", lambda e: e.reciprocal(out=smR[:, 0:4], in_=smR[:, 0:4]), [t_smR], [t_smR])
                        for h in range(4):
                            sch.ts("dve", rsb[:, h * 64:(h + 1) * 64], rsb[:, h * 64:(h + 1) * 64], bag[:, h, 0:1], smR[:, h:h + 1],
                                   ALU.subtract, ALU.mult, [t_rsb, t_bst, t_smR], [t_rsb])
                        sch.tt("pool", rsb[:, :], rsb[:, :], gret, ALU.mult, [t_rsb, t_const], [t_rsb])
                        sch.tt("pool", oct_[:, :], rsb[:, :], Gs[:, :], ALU.mult, [t_rsb, t_Gs], [t_oct])
                        for i in range(2):
                            sch.tr(psb[:, i * 128:(i + 1) * 128], oct_[:, i * 128:(i + 1) * 128], ident_b, [t_oct, t_const], [t_psb])
                        sch.copy("act", OT[:, 2:4, qcols], psb[:, 0:256].rearrange("p (a b) -> p a b", a=2), [t_psb], [t_OT])

                    prev = None
                    for j in range(4):
                        dsa_idx(j)
                        dsa_bis(j)
                        if prev is not None:
                            dsa_attn(prev)
                        ret_tile(j)
                        dsa_tr(j)
                        prev = j
                    dsa_attn(prev)

                    for oc in range(8):
                        pi = oc % 2
                        for mc in range(8):
                            if mc < 4:
                                sch.mm(psf[pi], Wout[:, mc, oc * 128:(oc + 1) * 128], OA[:, mc, cols], mc == 0, False,
                                       [t_Wo[mc], t_OA[c]], [t_ps[pi]])
                            else:
                                sch.mm(psf[pi], Wout[:, mc, oc * 128:(oc + 1) * 128], OT[:, mc - 4, :], False, mc == 7,
                                       [t_Wo[mc], t_OT], [t_ps[pi]])
                        sch.tt("dve", xT[:, oc, cols], xT[:, oc, cols], psf[pi], ALU.add, [t_xT[c], t_ps[pi]], [t_xT[c]])

                if dbg == "p2":
                    break
                sch.barrier()
                A = Carver(PHASE_BASE)
                hfT = alloc(A, BF16, [128, 8, S])
                W1 = [alloc(A, BF16, [128, 8, 1024]) for _ in range(2)]
                W2 = [alloc(A, BF16, [128, 8, D]) for _ in range(2)]
                hid = [alloc(A, BF16, [128, 8, CH]) for _ in range(2)]
                rl = [alloc(A, BF16, [128, CH]) for _ in range(2)]
                sqb = alloc(A, BF16, [128, 2, CH])
                rb = alloc(A, F32, [128, CH])
                t_hf = [Tok() for _ in range(NCH)]
                t_W1 = [[Tok() for _ in range(8)] for _ in range(2)]
                t_W2 = [[Tok() for _ in range(8)] for _ in range(2)]
                t_hid = [Tok(), Tok()]
                t_rl = [Tok(), Tok()]
                t_sq = [Tok(), Tok()]
                t_rb = Tok()

                def load_q(qr):
                    b = qr % 2
                    for kc in range(8):
                        sch.dma("pool", W1[b][:, kc, :], dr["w_ff1"][l, kc * 128:(kc + 1) * 128, qr * 1024:(qr + 1) * 1024], (), [t_W1[b][kc]])
                    for fc in range(8):
                        sch.dma("pool", W2[b][:, fc, :], dr["w_ff2"][l, qr * 1024 + fc * 128: qr * 1024 + (fc + 1) * 128, :], (), [t_W2[b][fc]])

                load_q(0)
                for c in range(NCH):
                    chunk_norm(c, lambda kc: gvec(l, "mlp", kc), hfT[:, :, c * CH:(c + 1) * CH], t_hf[c], sqb, t_sq, rb, t_rb, 0)
                for qr in range(4):
                    b = qr % 2
                    if qr + 1 < 4:
                        load_q(qr + 1)
                    for c in range(NCH):
                        cols = slice(c * CH, (c + 1) * CH)
                        hb = c % 2
                        for fc in range(8):
                            pi = fc % 2
                            for kc in range(8):
                                sch.mm(psf[pi], W1[b][:, kc, fc * 128:(fc + 1) * 128], hfT[:, kc, cols], kc == 0, kc == 7,
                                       [t_W1[b][kc], t_hf[c]], [t_ps[pi]])
                            sch.act(rl[pi], psf[pi], AF.Relu, [t_ps[pi]], [t_rl[pi]])
                            sch.tt("pool", hid[hb][:, fc, :], rl[pi], rl[pi], ALU.mult, [t_rl[pi]], [t_hid[hb]])
                        for oc in range(8):
                            pi = 2 + oc % 2
                            for fc in range(8):
                                sch.mm(psf[pi], W2[b][:, fc, oc * 128:(oc + 1) * 128], hid[hb][:, fc, :], fc == 0, fc == 7,
                                       [t_W2[b][fc], t_hid[hb]], [t_ps[pi]])
                            sch.tt("dve", xT[:, oc, cols], xT[:, oc, cols], psf[pi], ALU.add, [t_xT[c], t_ps[pi]], [t_xT[c]])

            sch.barrier()
            Lc = Carver(PHASE_BASE)
            yT = alloc(Lc, F32, [128, 8, CH])
            yo = [alloc(Lc, F32, [128, D]) for _ in range(2)]
            sqb = alloc(Lc, BF16, [128, 2, CH])
            rb = alloc(Lc, F32, [128, CH])
            t_y, t_rb = Tok(), Tok()
            t_sq = [Tok(), Tok()]
            t_yo = [Tok(), Tok()]
            for c in range(NCH):
                cols = slice(c * CH, (c + 1) * CH)
                if dbg:
                    src = None
                if final_norm:
                    for kc in range(8):
                        sch.act(sqb[:, kc % 2, :], xT[:, kc, cols], AF.Square, [t_xT[c]], [t_sq[kc % 2]])
                        sch.mm(psf[0], ones_b, sqb[:, kc % 2, :], kc == 0, kc == 7, [t_sq[kc % 2], t_const], [t_ps[0]])
                    sch.act(rb, psf[0], AF.Ln, [t_ps[0]], [t_rb], scale=1.0 / D, bias=EPS)
                    sch.act(rb, rb, AF.Exp, [t_rb], [t_rb], scale=-0.5)
                    for kc in range(8):
                        sch.stt(yT[:, kc, :], xT[:, kc, cols], gfin[:, kc:kc + 1], rb, ALU.mult, ALU.mult,
                                [t_xT[c], t_rb, t_const], [t_y])
                    srcT = lambda kc, t: yT[:, kc, t * 128:(t + 1) * 128]
                    t_src = t_y
                else:
                    srcT = lambda kc, t, c=c: xT[:, kc, c * CH + t * 128: c * CH + (t + 1) * 128]
                    t_src = t_xT[c]
                for t in range(4):
                    tt_ = c * 4 + t
                    b = tt_ % 2
                    for half in range(2):
                        pi = 1 + half
                        for k4 in range(4):
                            kc = half * 4 + k4
                            sch.tr(psf[pi][:, k4 * 128:(k4 + 1) * 128], srcT(kc, t), ident_f, [t_src, t_const], [t_ps[pi]])
                        sch.copy("act" if half == 0 else "dve", yo[b][:, half * 512:(half + 1) * 512], psf[pi], [t_ps[pi]], [t_yo[b]])
                    sch.dma("sp", dr["y"][sq_i * S + tt_ * 128: sq_i * S + (tt_ + 1) * 128, :], yo[b], [t_yo[b]], [t_yo[b]])

        if dbg:
            sch.barrier()
            if dbg == "p1":
                Lc = Carver(PHASE_BASE)
                tmp = alloc(Lc, F32, [128, 4, S])
                tk = Tok()
                for i in range(4):
                    sch.copy("dve", tmp[:, i, :], OA[:, i, :], [t_OA[c] for c in range(NCH)], [tk])
                sch.dma("sp", dr["dbg"][:, 0:4, :], tmp, [tk], [tk])
            else:
                sch.dma("sp", dr["dbg"], xT, [t_xT[c] for c in range(NCH)], [Tok()])
        final_waits = [(k, sch.dma_cnt[k]) for k in range(sch.n_dma_sems) if sch.dma_cnt[k] > 0]
        sch.emit(nc, block, sems, dsems, final_waits)
    return nc


NCORES = 8
_CACHE = {}


def _gpack(g_mix, g_q, g_kv, g_mlp, g_ret):
    L = g_mix.shape[0]
    out = np.zeros((L, 128, NG), np.float32)
    for l in range(L):
        out[l, :, 0:8] = np.asarray(g_mix[l], np.float32).reshape(8, 128).T
        out[l, :, 8:14] = np.asarray(g_q[l], np.float32).reshape(6, 128).T
        out[l, :, 14:16] = np.asarray(g_kv[l], np.float32).reshape(2, 128).T
        out[l, :, 16:24] = np.asarray(g_mlp[l], np.float32).reshape(8, 128).T
        out[l, :, 24:280] = np.broadcast_to(np.asarray(g_ret[l], np.float32)[None, :], (128, 256))
    return out


def run_layers(x, layers, final_norm, weights, g_final, ncores=NCORES, nseq=None, dbg=None):
    B = x.shape[0]
    nseq = B // ncores if nseq is None else nseq
    L = len(layers)
    key = (nseq, L, final_norm, dbg)
    if key not in _CACHE:
        _CACHE[key] = build_nc(nseq, L, final_norm, dbg)
    nc = _CACHE[key]
    consts = _consts()
    common = {"c_" + k: np.ascontiguousarray(v, dtype=np.float32) for k, v in consts.items()}
    for nm in ("w_in", "w_uq", "w_ukv", "w_out", "w_ff1", "w_ff2"):
        common[nm] = np.ascontiguousarray(np.asarray(weights[nm], np.float32)[layers])
    common["gpack"] = _gpack(*[np.asarray(weights[n])[layers] for n in ("g_mix", "g_q", "g_kv", "g_mlp", "g_ret")])
    common["gfin"] = np.ascontiguousarray(np.asarray(g_final, np.float32).reshape(8, 128).T)
    in_maps = []
    for ci in range(ncores):
        m = dict(common)
        m["x"] = np.ascontiguousarray(np.asarray(x[ci * nseq:(ci + 1) * nseq], np.float32).reshape(nseq * S, D))
        in_maps.append(m)
    res = run_bass_kernel_spmd(nc, in_maps, core_ids=list(range(ncores)))
    y = np.concatenate([r["y"].reshape(nseq, S, D) for r in res.results], axis=0)
    if dbg:
        return y, [r["dbg"] for r in res.results]
    return y


FUSED = True


def kernel(x, g_mix, w_in, g_q, w_uq, g_kv, w_ukv, g_ret, w_out, g_mlp, w_ff1, w_ff2, g_final):
    weights = dict(g_mix=g_mix, w_in=w_in, g_q=g_q, w_uq=w_uq, g_kv=g_kv, w_ukv=w_ukv, g_ret=g_ret, w_out=w_out,
                   g_mlp=g_mlp, w_ff1=w_ff1, w_ff2=w_ff2)
    x = np.asarray(x, np.float32)
    depth = np.asarray(w_in).shape[0]
    if FUSED:
        return run_layers(x, list(range(depth)), True, weights, g_final).astype(np.float32)
    y = x
    for l in range(depth):
        y = run_layers(y, [l], l == depth - 1, weights, g_final)
    return y.astype(np.float32)
```
